# Optimizing a Trainium2 kernel written in Bass

```python
import math
import jax, jax.numpy as jnp
from jax import lax
import numpy as np


D_MODEL = 2048
BATCH = 16
SEQ = 2048
DEPTH = 4

N_META = 16
RWKV_HEAD_DIM = 64
RWKV_WIDTH = 3 * D_MODEL // 8
RWKV_HEADS = RWKV_WIDTH // RWKV_HEAD_DIM
RWKV_DECAY_RANK = 96
RWKV_AAA_RANK = 96
RWKV_GATE_RANK = 256
RWKV_COLS = 3 * RWKV_WIDTH + RWKV_DECAY_RANK + RWKV_AAA_RANK + RWKV_GATE_RANK
RWKV_GN_EPS = 64e-5
FOX_HEAD_DIM = 64
FOX_WIDTH = 3 * D_MODEL // 8
FOX_HEADS = FOX_WIDTH // FOX_HEAD_DIM
FOX_BLOCK = 128
FOX_COLS = 3 * FOX_WIDTH + FOX_HEADS
MLSTM_WIDTH = D_MODEL // 4
MLSTM_HEADS = 4
MLSTM_HEAD_DIM = MLSTM_WIDTH // MLSTM_HEADS
MLSTM_CONV = 4
MLSTM_CHUNK = 64
MLSTM_COLS = 4 * MLSTM_WIDTH + 2 * MLSTM_HEADS
N_BRANCH = 3
GATE_COLS = N_BRANCH * D_MODEL
N_IN = RWKV_COLS + FOX_COLS + MLSTM_COLS + GATE_COLS
D_FF = 256 * math.ceil(8 * D_MODEL / 3 / 256)
N_EXPERTS = 8
TOP_K = 2
D_FF_EXPERT = D_FF
N_DENSE = (DEPTH + 1) // 2
N_MOE = DEPTH // 2
LN_EPS = 1e-5
DEEPNORM_ALPHA = (2 * DEPTH) ** 0.25
DEEPNORM_BETA = (8 * DEPTH) ** -0.25

kernel_name = 'hybrid_rwkv7_fox_mlstm_moe_deepnorm'


def layer_norm(x, g, b):
    xf = x.astype(jnp.float32)
    mu = jnp.mean(xf, axis=-1, keepdims=True)
    var = jnp.mean(jnp.square(xf - mu), axis=-1, keepdims=True)
    return ((xf - mu) * lax.rsqrt(var + LN_EPS) * g + b).astype(x.dtype)


def causal_depthwise_conv(x, w):
    K, L = w.shape[0], x.shape[1]
    xp = jnp.pad(x, ((0, 0), (K - 1, 0), (0, 0)))
    y = xp[:, 0:L] * w[0]
    for j in range(1, K):
        y = y + xp[:, j:j + L] * w[j]
    return y


def rwkv7_time_mix(z, mu, w0, w2, a0, a2, g2, k_k, k_a, r_k, gn_g, gn_b):
    f32 = jnp.float32
    B, L, _ = z.shape
    W = RWKV_WIDTH
    z_prev = jnp.pad(z, ((0, 0), (1, 0), (0, 0)))[:, :-1]
    z = z + mu * (z_prev - z)
    r, k, v, wd, ad, gd = jnp.split(
        z, [W, 2 * W, 3 * W, 3 * W + RWKV_DECAY_RANK, 3 * W + RWKV_DECAY_RANK + RWKV_AAA_RANK], axis=-1)
    w_log = -jax.nn.softplus(-(w0 + jnp.tanh(wd) @ w2).astype(f32)) - 0.5
    decay = jnp.exp(-jnp.exp(w_log))
    a = jax.nn.sigmoid((a0 + ad @ a2).astype(f32))
    g = jax.nn.sigmoid(gd) @ g2

    def heads(t):
        return t.reshape(B, L, RWKV_HEADS, RWKV_HEAD_DIM)

    kk = heads((k * k_k).astype(f32))
    kk = kk / jnp.maximum(jnp.sqrt(jnp.sum(jnp.square(kk), axis=-1, keepdims=True)), 1e-12)
    k = heads(k.astype(f32) * (1.0 + (a - 1.0) * k_a))
    r, v, decay, a = heads(r.astype(f32)), heads(v.astype(f32)), heads(decay), heads(a)

    def step(S, inp):
        r_t, w_t, k_t, v_t, kk_t, a_t = inp
        S = (S * w_t[:, :, None, :]
             - jnp.einsum('bhvk,bhk->bhv', S, kk_t)[..., None] * (kk_t * a_t)[:, :, None, :]
             + v_t[..., None] * k_t[:, :, None, :])
        return S, jnp.einsum('bhvk,bhk->bhv', S, r_t)

    xs = tuple(jnp.moveaxis(t, 1, 0) for t in (r, decay, k, v, kk, a))
    S0 = jnp.zeros((B, RWKV_HEADS, RWKV_HEAD_DIM, RWKV_HEAD_DIM), f32)
    _, o = lax.scan(step, S0, xs)
    o = jnp.moveaxis(o, 0, 1)
    mean = jnp.mean(o, axis=-1, keepdims=True)
    var = jnp.mean(jnp.square(o - mean), axis=-1, keepdims=True)
    o = ((o - mean) * lax.rsqrt(var + RWKV_GN_EPS)).reshape(B, L, W) * gn_g + gn_b
    bonus = jnp.sum(r * k * r_k, axis=-1, keepdims=True) * v
    o = (o + bonus.reshape(B, L, W)) * g
    return o.astype(z.dtype)


def forgetting_attention(z, b_f):
    B, L, _ = z.shape
    W = FOX_WIDTH
    q, k, v, fg = jnp.split(z, [W, 2 * W, 3 * W], axis=-1)
    q = q.reshape(B, L, FOX_HEADS, FOX_HEAD_DIM)
    k = k.reshape(B, L, FOX_HEADS, FOX_HEAD_DIM)
    v = v.reshape(B, L, FOX_HEADS, FOX_HEAD_DIM)
    log_f = jax.nn.log_sigmoid((fg + b_f).astype(jnp.float32))
    cum = jnp.cumsum(log_f, axis=1).transpose(0, 2, 1)
    scale = FOX_HEAD_DIM ** -0.5
    starts = [0] + list(range(N_META, L, FOX_BLOCK))
    ends = starts[1:] + [L]
    outs = []
    for s, e in zip(starts, ends):
        logits = jnp.einsum('bqhd,bkhd->bhqk', q[:, s:e], k[:, :e],
                            preferred_element_type=jnp.float32) * scale
        logits = logits + cum[:, :, s:e, None] - cum[:, :, None, :e]
        causal = jnp.arange(s, e)[:, None] >= jnp.arange(e)[None, :]
        logits = jnp.where(causal, logits, -jnp.inf)
        p = jax.nn.softmax(logits, axis=-1).astype(v.dtype)
        outs.append(jnp.einsum('bhqk,bkhd->bqhd', p, v[:, :e]))
    return jnp.concatenate(outs, axis=1).reshape(B, L, W)


def mlstm_chunk(carry, inputs):
    C, n, m = carry
    q, k, v, li, lf = inputs
    Lc = q.shape[2]
    b = jnp.cumsum(lf, axis=-1)
    causal = jnp.tril(jnp.ones((Lc, Lc), dtype=bool))
    log_d = jnp.where(causal, b[..., :, None] - b[..., None, :] + li[..., None, :], -jnp.inf)
    log_inter = b + m[..., None]
    m_q = jnp.maximum(log_inter, jnp.max(log_d, axis=-1))
    s = jnp.einsum('bhqd,bhkd->bhqk', q, k) * jnp.exp(log_d - m_q[..., None])
    w_inter = jnp.exp(log_inter - m_q)
    num = w_inter[..., None] * jnp.einsum('bhqd,bhdv->bhqv', q, C) + jnp.einsum('bhqk,bhkv->bhqv', s, v)
    den = w_inter * jnp.einsum('bhqd,bhd->bhq', q, n) + jnp.sum(s, axis=-1)
    h = num / jnp.maximum(jnp.abs(den), jnp.exp(-m_q))[..., None]
    b_end = b[..., -1]
    log_g = b_end[..., None] - b + li
    m_new = jnp.maximum(b_end + m, jnp.max(log_g, axis=-1))
    gk = jnp.exp(log_g - m_new[..., None])
    dec = jnp.exp(b_end + m - m_new)
    C = dec[..., None, None] * C + jnp.einsum('bhl,bhld,bhlv->bhdv', gk, k, v)
    n = dec[..., None] * n + jnp.einsum('bhl,bhld->bhd', gk, k)
    return (C, n, m_new), h


def mlstm_mix(z, conv_w, b_i, b_f):
    f32 = jnp.float32
    B, L, _ = z.shape
    W, H, Dh = MLSTM_WIDTH, MLSTM_HEADS, MLSTM_HEAD_DIM
    qk, v, o, ig, fg = jnp.split(z, [2 * W, 3 * W, 4 * W, 4 * W + H], axis=-1)
    qk = jax.nn.silu(causal_depthwise_conv(qk, conv_w))
    q, k = jnp.split(qk, 2, axis=-1)

    def heads(t):
        return t.astype(f32).reshape(B, L, H, Dh).transpose(0, 2, 1, 3)

    q, k, v = heads(q), heads(k) * (Dh ** -0.5), heads(v)
    li = (ig + b_i).astype(f32).transpose(0, 2, 1)
    lf = jax.nn.log_sigmoid((fg + b_f).astype(f32)).transpose(0, 2, 1)
    carry = (jnp.zeros((B, H, Dh, Dh), f32), jnp.zeros((B, H, Dh), f32), jnp.zeros((B, H), f32))
    carry, h_meta = mlstm_chunk(carry, (q[:, :, :N_META], k[:, :, :N_META], v[:, :, :N_META],
                                        li[:, :, :N_META], lf[:, :, :N_META]))
    n_chunks = (L - N_META) // MLSTM_CHUNK

    def to_chunks(t):
        t = t[:, :, N_META:]
        t = t.reshape((B, H, n_chunks, MLSTM_CHUNK) + t.shape[3:])
        return jnp.moveaxis(t, 2, 0)

    _, h_rest = lax.scan(mlstm_chunk, carry, tuple(to_chunks(t) for t in (q, k, v, li, lf)))
    h_rest = jnp.moveaxis(h_rest, 0, 2).reshape(B, H, L - N_META, Dh)
    h = jnp.concatenate([h_meta, h_rest], axis=2).transpose(0, 2, 1, 3).reshape(B, L, W)
    return (jax.nn.sigmoid(o.astype(f32)) * h).astype(z.dtype)


def swiglu(x, w1, w3, w2):
    return (jax.nn.silu(x @ w1) * (x @ w3)) @ w2


def moe_swiglu(x, router_w, w1, w3, w2):
    logits = (x @ router_w).astype(jnp.float32)
    top_v, top_i = lax.top_k(logits, TOP_K)
    top_w = jax.nn.softmax(top_v, axis=-1)
    gates = jnp.sum(jax.nn.one_hot(top_i, N_EXPERTS, dtype=jnp.float32) * top_w[..., None],
                    axis=-2).astype(x.dtype)
    y = jnp.zeros_like(x)
    for e in range(N_EXPERTS):
        y = y + gates[..., e:e + 1] * swiglu(x, w1[e], w3[e], w2[e])
    return y


def setup_inputs(seed: int = 0) -> dict:
    key = jax.random.key(seed)
    keys = iter(jax.random.split(key, 64))

    def nrm(shape, scale=1.0):
        return jax.random.normal(next(keys), shape, jnp.float32) * scale

    def unif(shape):
        return jax.random.uniform(next(keys), shape, jnp.float32)

    D, L = D_MODEL, DEPTH
    return {
        'x': nrm((BATCH, SEQ, D)),
        'meta_tokens': nrm((N_META, D)),
        'ln_emb_g': 1.0 + nrm((D,), 0.05),
        'ln_emb_b': nrm((D,), 0.02),
        'w_in': nrm((L, D, N_IN), D ** -0.5),
        'rwkv_mu': unif((L, RWKV_COLS)),
        'rwkv_w0': nrm((L, RWKV_WIDTH), 0.5),
        'rwkv_w2': nrm((L, RWKV_DECAY_RANK, RWKV_WIDTH), 0.1 * RWKV_DECAY_RANK ** -0.5),
        'rwkv_a0': nrm((L, RWKV_WIDTH), 0.1),
        'rwkv_a2': nrm((L, RWKV_AAA_RANK, RWKV_WIDTH), 0.1 * RWKV_AAA_RANK ** -0.5),
        'rwkv_g2': nrm((L, RWKV_GATE_RANK, RWKV_WIDTH), RWKV_GATE_RANK ** -0.5),
        'rwkv_k_k': 0.85 + nrm((L, RWKV_WIDTH), 0.05),
        'rwkv_k_a': 1.0 + nrm((L, RWKV_WIDTH), 0.05),
        'rwkv_r_k': nrm((L, RWKV_HEADS, RWKV_HEAD_DIM), 0.1),
        'rwkv_gn_g': 1.0 + nrm((L, RWKV_WIDTH), 0.05),
        'rwkv_gn_b': nrm((L, RWKV_WIDTH), 0.02),
        'fox_b_f': jnp.linspace(1.0, 4.0, FOX_HEADS, dtype=jnp.float32) + nrm((L, FOX_HEADS), 0.1),
        'mlstm_conv_w': nrm((L, MLSTM_CONV, 2 * MLSTM_WIDTH), MLSTM_CONV ** -0.5),
        'mlstm_b_i': nrm((L, MLSTM_HEADS), 0.1),
        'mlstm_b_f': jnp.linspace(3.0, 6.0, MLSTM_HEADS, dtype=jnp.float32) + nrm((L, MLSTM_HEADS), 0.1),
        'proj_rwkv': nrm((L, RWKV_WIDTH, D), DEEPNORM_BETA * RWKV_WIDTH ** -0.5),
        'proj_fox': nrm((L, FOX_WIDTH, D), DEEPNORM_BETA * FOX_WIDTH ** -0.5),
        'proj_mlstm': nrm((L, MLSTM_WIDTH, D), DEEPNORM_BETA * MLSTM_WIDTH ** -0.5),
        'w_out': nrm((L, D, D), DEEPNORM_BETA * D ** -0.5),
        'ln1_g': 1.0 + nrm((L, D), 0.05),
        'ln1_b': nrm((L, D), 0.02),
        'ffn_w1': nrm((N_DENSE, D, D_FF), D ** -0.5),
        'ffn_w3': nrm((N_DENSE, D, D_FF), D ** -0.5),
        'ffn_w2': nrm((N_DENSE, D_FF, D), DEEPNORM_BETA * D_FF ** -0.5),
        'router_w': nrm((N_MOE, D, N_EXPERTS), D ** -0.5),
        'moe_w1': nrm((N_MOE, N_EXPERTS, D, D_FF_EXPERT), D ** -0.5),
        'moe_w3': nrm((N_MOE, N_EXPERTS, D, D_FF_EXPERT), D ** -0.5),
        'moe_w2': nrm((N_MOE, N_EXPERTS, D_FF_EXPERT, D), DEEPNORM_BETA * D_FF_EXPERT ** -0.5),
        'ln2_g': 1.0 + nrm((L, D), 0.05),
        'ln2_b': nrm((L, D), 0.02),
    }


def reference(x, meta_tokens, ln_emb_g, ln_emb_b, w_in, rwkv_mu, rwkv_w0, rwkv_w2, rwkv_a0, rwkv_a2,
              rwkv_g2, rwkv_k_k, rwkv_k_a, rwkv_r_k, rwkv_gn_g, rwkv_gn_b, fox_b_f, mlstm_conv_w,
              mlstm_b_i, mlstm_b_f, proj_rwkv, proj_fox, proj_mlstm, w_out, ln1_g, ln1_b,
              ffn_w1, ffn_w3, ffn_w2, router_w, moe_w1, moe_w3, moe_w2, ln2_g, ln2_b):
    B = x.shape[0]
    meta = jnp.broadcast_to(meta_tokens[None].astype(x.dtype), (B, N_META, D_MODEL))
    h = layer_norm(jnp.concatenate([meta, x], axis=1), ln_emb_g, ln_emb_b)
    c1 = RWKV_COLS
    c2 = c1 + FOX_COLS
    c3 = c2 + MLSTM_COLS
    for l in range(DEPTH):
        z = h @ w_in[l]
        z_r, z_f, z_m, z_g = jnp.split(z, [c1, c2, c3], axis=-1)
        y_r = rwkv7_time_mix(z_r, rwkv_mu[l], rwkv_w0[l], rwkv_w2[l], rwkv_a0[l], rwkv_a2[l],
                             rwkv_g2[l], rwkv_k_k[l], rwkv_k_a[l], rwkv_r_k[l], rwkv_gn_g[l], rwkv_gn_b[l])
        y_f = forgetting_attention(z_f, fox_b_f[l])
        y_m = mlstm_mix(z_m, mlstm_conv_w[l], mlstm_b_i[l], mlstm_b_f[l])
        g_r, g_f, g_m = jnp.split(jax.nn.sigmoid(z_g), N_BRANCH, axis=-1)
        mixed = (g_r * (y_r @ proj_rwkv[l]) + g_f * (y_f @ proj_fox[l])
                 + g_m * (y_m @ proj_mlstm[l])) @ w_out[l]
        h = layer_norm(DEEPNORM_ALPHA * h + mixed, ln1_g[l], ln1_b[l])
        if l % 2 == 0:
            ff = swiglu(h, ffn_w1[l // 2], ffn_w3[l // 2], ffn_w2[l // 2])
        else:
            ff = moe_swiglu(h, router_w[l // 2], moe_w1[l // 2], moe_w3[l // 2], moe_w2[l // 2])
        h = layer_norm(DEEPNORM_ALPHA * h + ff, ln2_g[l], ln2_b[l])
    return h[:, N_META:]
```

```python
import numpy as np
from contextlib import ExitStack, contextmanager
import concourse.bass as bass
import concourse.mybir as mybir
from concourse.bass_utils import run_bass_kernel_spmd

F32 = mybir.dt.float32
BF16 = mybir.dt.bfloat16
AF = mybir.ActivationFunctionType
ALU = mybir.AluOpType
AX = mybir.AxisListType

D = 2048
KC = 16
NMETA = 16
RW = 768
RH = 12
FW = 768
MW = 512
MH = 4
C1 = 2752
C2 = C1 + 2316
C3 = C2 + 2056
NIN = C3 + 6144
DFF = 5632
FC = 44
NE = 8
ALPHA = 8 ** 0.25
NDS = 48


class Res:
    __slots__ = ("name", "w", "r", "sem", "base")

    ALL = []

    def __init__(self, name):
        self.name = name
        self.w = {}
        self.r = {}
        self.base = {}
        self.sem = None
        Res.ALL.append(self)


class VA:
    __slots__ = ("ap", "res")

    def __init__(self, ap, res):
        self.ap = ap
        self.res = res

    def __getitem__(self, idx):
        return VA(self.ap[idx], self.res)


class T:
    def __init__(self, t, name):
        self.t = t
        self.res = Res(name)

    def __getitem__(self, idx):
        return VA(self.t[idx], self.res)


class Prog:
    def __init__(self, nc):
        self.nc = nc
        self.es = ExitStack()
        self.eng = {"pe": nc.tensor, "act": nc.scalar, "dve": nc.vector, "pool": nc.gpsimd, "sp": nc.sync}
        self.esem = {k: self.es.enter_context(nc.semaphore(f"e_{k}")) for k in self.eng}
        self.ecnt = {k: 0 for k in self.eng}
        self.dsems = [self.es.enter_context(nc.semaphore(f"d{i}")) for i in range(NDS)]
        self.dcnt = [0] * NDS
        self.dnext = 0
        self.obs = {k: {} for k in self.eng}
        self.ninstr = 0

    def semof(self, key):
        return self.esem[key[1]] if key[0] == "e" else self.dsems[key[1]]

    def _waits(self, eng, reads, writes, disjoint):
        waits = {}
        for r in reads:
            for k, v in r.w.items():
                if waits.get(k, 0) < v:
                    waits[k] = v
        for w in writes:
            for k, v in w.r.items():
                if waits.get(k, 0) < v:
                    waits[k] = v
            for k, v in (w.base if disjoint else w.w).items():
                if waits.get(k, 0) < v:
                    waits[k] = v
        ob = self.obs[eng]
        e = self.eng[eng]
        for k, v in waits.items():
            if eng == "pe" and k == ("e", "pe"):
                continue
            if ob.get(k, 0) >= v:
                continue
            e.wait_ge(self.semof(k), v)
            ob[k] = v
            self.ninstr += 1

    def _record(self, key, val, reads, writes, disjoint):
        for r in reads:
            if r.r.get(key, 0) < val:
                r.r[key] = val
        for w in writes:
            if not disjoint:
                w.r = {}
                w.w = {key: val}
                w.base = {key: val}
            else:
                if w.w.get(key, 0) < val:
                    w.w[key] = val

    def op(self, eng, fn, reads, writes, disjoint=False):
        reads = [x.res for x in reads if x is not None]
        writes = [x.res for x in writes]
        self._waits(eng, reads, writes, disjoint)
        ins = fn(self.eng[eng])
        self.ecnt[eng] += 1
        ins.then_inc(self.esem[eng], 1)
        self.ninstr += 1
        self._record(("e", eng), self.ecnt[eng], reads, writes, disjoint)

    def dma(self, q, out, in_, tile, disjoint=True):
        res = tile.res
        if res.sem is None:
            res.sem = self.dnext % NDS
            self.dnext += 1
        idx = res.sem
        reads = [in_.res]
        writes = [out.res]
        self._waits(q, reads, writes, disjoint)
        ins = self.eng[q].dma_start(out=out.ap, in_=in_.ap)
        self.dcnt[idx] += 1
        ins.then_inc(self.dsems[idx], 16)
        self.ninstr += 1
        self._record(("d", idx), 16 * self.dcnt[idx], reads, writes, disjoint)

    def barrier(self):
        ev = {("e", k): v for k, v in self.ecnt.items() if v > 0}
        for i in range(NDS):
            if self.dcnt[i] > 0:
                ev[("d", i)] = 16 * self.dcnt[i]
        for eng, e in self.eng.items():
            ob = self.obs[eng]
            for k, v in ev.items():
                if ob.get(k, 0) >= v:
                    continue
                e.wait_ge(self.semof(k), v)
                ob[k] = v
                self.ninstr += 1

    def reset_all(self):
        nc = self.nc
        self.barrier()
        if getattr(self, "no_reset", False):
            return
        if not hasattr(self, "bsem"):
            self.bsem = [self.es.enter_context(nc.semaphore(f"bar{i}")) for i in range(4)]
        A, C, B, Dd = self.bsem
        order = ["pe", "act", "dve", "pool", "sp"]
        for k in order:
            e = self.eng[k]
            e.sem_inc(A, 1)
            e.wait_ge(A, 5)
            e.sem_inc(C, 1)
        pe = self.eng["pe"]
        pe.wait_ge(C, 5)
        for s_ in list(self.esem.values()) + list(self.dsems):
            pe.sem_clear(s_)
        pe.sem_clear(A)
        pe.sem_clear(C)
        pe.sem_inc(B, 1)
        for k in order[1:]:
            e = self.eng[k]
            e.wait_ge(B, 1)
            e.sem_inc(Dd, 1)
        pe.wait_ge(Dd, 4)
        pe.sem_clear(B)
        pe.sem_clear(Dd)
        self.ecnt = {k: 0 for k in self.eng}
        self.dcnt = [0] * NDS
        self.obs = {k: {} for k in self.eng}
        for r in Res.ALL:
            r.w = {}
            r.r = {}
            r.base = {}

    @contextmanager
    def stage(self):
        st = Stage(self)
        try:
            yield st
        finally:
            self.barrier()
            st.es.close()

    def dram(self, name, shape, dt):
        return T(self.nc.dram_tensor(name, list(shape), dt, kind="Internal"), name)

    def mm(self, out, lhsT, rhs, start=True, stop=True):
        self.op("pe", lambda e: e.matmul(out.ap, lhsT.ap, rhs.ap, start=start, stop=stop),
                [lhsT, rhs], [out], disjoint=True)

    def tr(self, out, in_, ident):
        self.op("pe", lambda e: e.transpose(out.ap, in_.ap, ident.ap), [in_, ident], [out], disjoint=True)

    def act(self, out, in_, func, bias=None, scale=1.0, eng="act"):
        def f(e):
            kw = {}
            if bias is not None:
                kw["bias"] = bias.ap if isinstance(bias, VA) else bias
            return e.activation(out=out.ap, in_=in_.ap, func=func, scale=scale, **kw)
        self.op("act", f, [in_, bias if isinstance(bias, VA) else None], [out], disjoint=True)

    def tt(self, out, a, b, op, eng="dve"):
        self.op(eng, lambda e: e.tensor_tensor(out=out.ap, in0=a.ap, in1=b.ap, op=op), [a, b], [out], disjoint=True)

    def ts(self, out, a, s1, op0, s2=None, op1=None, eng="dve"):
        def f(e):
            a1 = s1.ap if isinstance(s1, VA) else s1
            if s2 is None:
                return e.tensor_scalar(out=out.ap, in0=a.ap, scalar1=a1, scalar2=None, op0=op0)
            a2 = s2.ap if isinstance(s2, VA) else s2
            return e.tensor_scalar(out=out.ap, in0=a.ap, scalar1=a1, scalar2=a2, op0=op0, op1=op1)
        self.op(eng, f, [a, s1 if isinstance(s1, VA) else None, s2 if isinstance(s2, VA) else None], [out],
                disjoint=True)

    def stt(self, out, a, s, b, op0, op1):
        def f(e):
            sc = s.ap if isinstance(s, VA) else s
            return e.scalar_tensor_tensor(out=out.ap, in0=a.ap, scalar=sc, in1=b.ap, op0=op0, op1=op1)
        self.op("dve", f, [a, b, s if isinstance(s, VA) else None], [out], disjoint=True)

    def copy(self, out, in_, eng="dve"):
        if eng == "act":
            self.act(out, in_, AF.Copy)
        else:
            self.op(eng, lambda e: e.tensor_copy(out=out.ap, in_=in_.ap), [in_], [out], disjoint=True)

    def recip(self, out, in_):
        self.op("dve", lambda e: e.reciprocal(out=out.ap, in_=in_.ap), [in_], [out], disjoint=True)

    def memset(self, out, val, eng="dve"):
        self.op(eng, lambda e: e.memset(out.ap, val), [], [out], disjoint=False)

    def scan(self, out, d0, d1, init, op0, op1):
        self.op("dve", lambda e: e.tensor_tensor_scan(out=out.ap, data0=d0.ap, data1=d1.ap, initial=init,
                                                      op0=op0, op1=op1), [d0, d1], [out], disjoint=True)


class Stage:
    def __init__(self, p):
        self.p = p
        self.es = ExitStack()
        self.n = 0

    def tile(self, shape, dt, name=None):
        self.n += 1
        name = name or f"t{self.n}"
        self.p.uid = getattr(self.p, "uid", 0) + 1
        nm = f"{name}_{self.p.uid}"
        return T(self.es.enter_context(self.p.nc.sbuf_tensor(nm, list(shape), dt)), nm)

    def psum(self, shape=(128, 512), dt=F32, name=None):
        self.n += 1
        self.p.uid = getattr(self.p, "uid", 0) + 1
        nm = f"ps_{self.p.uid}"
        return T(self.es.enter_context(self.p.nc.psum_tensor(nm, list(shape), dt)), nm)


CO_ID = 0
CO_MEAN = 128
CO_BLK = 256
CO_MLE = 384
CO_MLT = 512
CO_MGE = 640
CO_ONE = 768
CO_SEL = 896
NCONST = CO_SEL + 12 * 128


def make_consts():
    c = np.zeros((128, NCONST), np.float32)
    i = np.arange(128)
    c[:, CO_ID:CO_ID + 128] = np.eye(128)
    c[:, CO_MEAN:CO_MEAN + 128] = 1.0 / D
    c[:64, CO_BLK:CO_BLK + 64] = 1.0
    c[64:, CO_BLK + 64:CO_BLK + 128] = 1.0
    c[:, CO_MLE:CO_MLE + 128] = (i[:, None] <= i[None, :])
    c[:, CO_MLT:CO_MLT + 128] = (i[:, None] < i[None, :])
    c[:, CO_MGE:CO_MGE + 128] = (i[:, None] > i[None, :])
    c[:, CO_ONE:CO_ONE + 128] = 1.0
    for h in range(12):
        c[h, CO_SEL + h * 128:CO_SEL + (h + 1) * 128] = 1.0
    return c


class TT(T):
    def view(self, pat, **kw):
        v = TT.__new__(TT)
        v.t = self.t.rearrange(pat, **kw)
        v.res = self.res
        return v


def va_re(va, pat, **kw):
    return VA(va.ap.rearrange(pat, **kw), va.res)


class Cfg:
    def __init__(self, NT=16, NSEQ=1, depth=4, debug=False):
        self.NT = NT
        self.NSEQ = NSEQ
        self.depth = depth
        self.L = NMETA + 128 * NT
        self.S = 128 * NT
        self.tiles = [(0, NMETA)] + [(NMETA + 128 * i, 128) for i in range(NT)]
        self.sbs = [(o, min(512, self.L - o)) for o in range(0, self.L, 512)]
        self.n_dense = (depth + 1) // 2
        self.n_moe = depth // 2
        self.debug = debug


def col_chunks():
    segs = [0, 768, 1536, 2304, 2400, 2496, C1, C1 + 768, C1 + 1536, C1 + 2304, C2, C2 + 512, C2 + 1024,
            C2 + 1536, C2 + 2048, C3, C3 + 2048, C3 + 4096, NIN]
    chunks = []
    for a, b in zip(segs[:-1], segs[1:]):
        c = a
        while c < b:
            m = min(128, b - c)
            chunks.append((c, m))
            c += m
    blocks = []
    cur = []
    for ch in chunks:
        if cur and (ch[0] + ch[1] - cur[0][0]) > 512:
            blocks.append(cur)
            cur = []
        cur.append(ch)
    blocks.append(cur)
    return blocks


def build(cfg):
    nc = bass.Bass("TRN2", target_bir_lowering=False)
    P = Prog(nc)
    L, NT, dep = cfg.L, cfg.NT, cfg.depth
    NTl = NT + 1

    def ext(name, shape, dt=F32):
        t = TT.__new__(TT)
        t.t = nc.dram_tensor(name, list(shape), dt, kind="ExternalInput").ap()
        t.res = Res(name)
        return t

    def scr(name, shape, dt=F32):
        t = TT.__new__(TT)
        t.t = nc.dram_tensor(name, list(shape), dt, kind="Internal").ap()
        t.res = Res(name)
        return t

    x_in = ext("x", [cfg.NSEQ, cfg.S, D])
    meta = ext("meta", [NMETA, D])
    consts = ext("consts", [128, NCONST])
    vecD = ext("vecD", [128, 2 + 4 * dep, KC])
    vecR = ext("vecR", [128, dep, 13, 6])
    foxbf = ext("foxbf", [dep, 12, 1])
    convw = ext("convw", [128, dep, 8, 4])
    mlb = ext("mlb", [dep, 4, 2])
    w_in = ext("w_in", [dep, D, NIN])
    rw2 = ext("rwkv_w2", [dep, 96, RW])
    ra2 = ext("rwkv_a2", [dep, 96, RW])
    rg2 = ext("rwkv_g2", [dep, 256, RW])
    p_r = ext("proj_rwkv", [dep, RW, D])
    p_f = ext("proj_fox", [dep, FW, D])
    p_m = ext("proj_mlstm", [dep, MW, D])
    w_o = ext("w_out", [dep, D, D])
    f_w1 = ext("ffn_w1", [cfg.n_dense, D, DFF])
    f_w3 = ext("ffn_w3", [cfg.n_dense, D, DFF])
    f_w2 = ext("ffn_w2", [cfg.n_dense, DFF, D])
    if cfg.n_moe:
        r_w = ext("router_w", [cfg.n_moe, D, NE])
        m_w1 = ext("moe_w1", [cfg.n_moe, NE, D, DFF])
        m_w3 = ext("moe_w3", [cfg.n_moe, NE, D, DFF])
        m_w2 = ext("moe_w2", [cfg.n_moe, NE, DFF, D])
    out_t = TT.__new__(TT)
    out_t.t = nc.dram_tensor("out", [cfg.NSEQ, cfg.S, D], F32, kind="ExternalOutput").ap()
    out_t.res = Res("out")
    dbg = {}
    if cfg.debug:
        for nm, shp in [("d_ht", [D, L]), ("d_zt", [NIN, L]), ("d_yt", [D, L]), ("d_new", [D, L])]:
            t = TT.__new__(TT)
            t.t = nc.dram_tensor(nm, shp, F32, kind="ExternalOutput").ap()
            t.res = Res(nm)
            dbg[nm] = t

    HT32 = scr("HT32", [D, L])
    HTb = scr("HTb", [D, L], BF16)
    ZT = scr("ZT", [NIN, L])
    YT = scr("YT", [D, L], BF16)
    MIXT = scr("MIXT", [D, L], BF16)
    NEWT = scr("NEWT", [D, L])
    RWS = scr("RWS", [8, RW, L])
    HT32v = HT32.view("(kc p) l -> p kc l", p=128)
    HTbv = HTb.view("(kc p) l -> p kc l", p=128)
    YTv = YT.view("(kc p) l -> p kc l", p=128)
    MIXTv = MIXT.view("(kc p) l -> p kc l", p=128)
    NEWTv = NEWT.view("(kc p) l -> p kc l", p=128)

    ges = P.es

    def gtile(name, shape, dt):
        return TT_from(ges.enter_context(nc.sbuf_tensor(name, list(shape), dt)), name)

    def TT_from(t, name):
        o = TT.__new__(TT)
        o.t = t
        o.res = Res(name)
        return o

    CON = gtile("CON", [128, NCONST], F32)
    CONB = gtile("CONB", [128, 896], BF16)
    VD = gtile("VD", [128, 2 + 4 * dep, KC], F32)
    VR = gtile("VR", [128, dep, 13, 6], F32)
    P.dma("sp", CON[:, :], consts[:, :], CON)
    P.dma("sp", VD[:, :, :], vecD[:, :, :], VD)
    P.dma("sp", VR[:, :, :, :], vecR[:, :, :, :], VR)
    P.copy(CONB[:, :], CON[:, 0:896])
    ID = CON[:, CO_ID:CO_ID + 128]

    def ident(n):
        return CON[:n, CO_ID:CO_ID + n]

    STG_N = 2816

    def mk_stg(st, n=3):
        st.stg = [st.tile([128, STG_N], F32) for _ in range(n)]
        st.stg_i = 0

    def wload(st, dst, src, np_, a, b, eng="pool", engs=None):
        step = max(1, STG_N // b)
        a0 = 0
        while a0 < a:
            a1 = min(a, a0 + step)
            stg = st.stg[st.stg_i % len(st.stg)]
            st.stg_i += 1
            view = VA(stg.t[:np_, 0:(a1 - a0) * b].rearrange("p (a b) -> p a b", a=a1 - a0), stg.res)
            P.dma("sp", view, src[:, a0:a1, :], stg, disjoint=False)
            P.copy(dst[:, a0:a1, :], view, eng=(engs[st.stg_i % len(engs)] if engs else eng))
            a0 = a1

    def pipelined(n, load, compute, depth=1):
        for i in range(min(depth, n)):
            load(i)
        for i in range(n):
            if i + depth < n:
                load(i + depth)
            compute(i)

    blocks = col_chunks()
    cache = {}
    NB_WIN = len(blocks)
    n_ffn = cfg.n_dense + cfg.n_moe * NE

    def cfam(name, nslots, elems):
        per = max(1, (96 << 20) // (128 * elems * 2))
        ts = [scr(f"C_{name}_{g}", [min(per, nslots - g * per), 128, elems], BF16)
              for g in range((nslots + per - 1) // per)]
        cache[name] = (per, ts)

    def cview(fam, slot, a, b):
        per, ts = cache[fam]
        t = ts[slot // per]
        return VA(t.t[slot % per, :, 0:a * b].rearrange("p (a b) -> p a b", a=a), t.res)

    cfam("win", dep * NB_WIN, KC * 512)
    cfam("pr", dep, 6 * D)
    cfam("pf", dep, 6 * D)
    cfam("pm", dep, 4 * D)
    cfam("wo", dep * 4, KC * 512)
    cfam("w1", n_ffn * 22, KC * 256)
    cfam("w3", n_ffn * 22, KC * 256)
    cfam("w2", n_ffn * 16, FC * 128)

    def ffn_idx(l, e_):
        return (l // 2) if l % 2 == 0 else cfg.n_dense + (l // 2) * NE + e_

    def prologue():
        with P.stage() as st:
            mk_stg(st, 4)
            WT = [st.tile([128, 6 * D], BF16) for _ in range(2)]
            k = [0]
            engs = ["pool", "act", "dve"]

            def fill(fam, slot, srcv, a, b):
                wt = WT[k[0] % 2]
                k[0] += 1
                dst = VA(wt.t[:, 0:a * b].rearrange("p (a b) -> p a b", a=a), wt.res)
                wload(st, dst, srcv, 128, a, b, engs=engs)
                P.dma("sp", cview(fam, slot, a, b), dst, wt)
            wv = w_in.view("l (kc p) n -> l p kc n", p=128)
            prv = p_r.view("l (kc p) n -> l p kc n", p=128)
            pfv = p_f.view("l (kc p) n -> l p kc n", p=128)
            pmv = p_m.view("l (kc p) n -> l p kc n", p=128)
            wov = w_o.view("l (kc p) n -> l p kc n", p=128)
            for l in range(dep):
                for bi_, blk in enumerate(blocks):
                    c0 = blk[0][0]
                    cw = blk[-1][0] + blk[-1][1] - c0
                    fill("win", l * NB_WIN + bi_, wv[l, :, :, c0:c0 + cw], KC, cw)
                fill("pr", l, prv[l], 6, D)
                fill("pf", l, pfv[l], 6, D)
                fill("pm", l, pmv[l], 4, D)
                for cb in range(4):
                    fill("wo", l * 4 + cb, wov[l, :, :, cb * 512:(cb + 1) * 512], KC, 512)
                moe = (l % 2 == 1)
                li = l // 2
                for e_ in range(NE if moe else 1):
                    if moe:
                        w1v = m_w1.view("l e (kc p) n -> l e p kc n", p=128)[li, e_]
                        w3v = m_w3.view("l e (kc p) n -> l e p kc n", p=128)[li, e_]
                        w2v = m_w2.view("l e (kc p) n -> l e p kc n", p=128)[li, e_]
                    else:
                        w1v = f_w1.view("l (kc p) n -> l p kc n", p=128)[li]
                        w3v = f_w3.view("l (kc p) n -> l p kc n", p=128)[li]
                        w2v = f_w2.view("l (kc p) n -> l p kc n", p=128)[li]
                    fi = ffn_idx(l, e_)
                    for cb in range(22):
                        fill("w1", fi * 22 + cb, w1v[:, :, cb * 256:(cb + 1) * 256], KC, 256)
                        fill("w3", fi * 22 + cb, w3v[:, :, cb * 256:(cb + 1) * 256], KC, 256)
                    for m in range(16):
                        fill("w2", fi * 16 + m, w2v[:, :, m * 128:(m + 1) * 128], FC, 128)

    def ln_stage(src_v, gi, bi, also_dbg=None):
        with P.stage() as st:
            NEW = [st.tile([128, KC, 512], F32) for _ in range(2)]
            SQ = st.tile([128, KC, 512], F32)
            OB = [st.tile([128, KC, 512], BF16) for _ in range(2)]
            mean = st.tile([128, 512], F32)
            rstd = st.tile([128, 512], F32)
            psm = st.psum()
            psq = st.psum()
            for bi_, (o, n) in enumerate(cfg.sbs):
                nw = NEW[bi_ % 2]
                ob = OB[bi_ % 2]
                P.dma("sp", nw[:, :, :n], src_v[:, :, o:o + n], nw)
                ln_core(st, nw, SQ, ob, mean, rstd, psm, psq, n, gi, bi)
                P.dma("sp", HT32v[:, :, o:o + n], nw[:, :, :n], nw)
                P.dma("sp", HTbv[:, :, o:o + n], ob[:, :, :n], ob)

    def ln_core(st, nw, SQ, ob, mean, rstd, psm, psq, n, gi, bi):
        MEANM = CON[:, CO_MEAN:CO_MEAN + 128]
        P.act(SQ[:, :, :n], nw[:, :, :n], AF.Square)
        for kc in range(KC):
            P.mm(psm[:, :n], MEANM, nw[:, kc, :n], start=(kc == 0), stop=(kc == KC - 1))
        for kc in range(KC):
            P.mm(psq[:, :n], MEANM, SQ[:, kc, :n], start=(kc == 0), stop=(kc == KC - 1))
        P.copy(mean[:, :n], psm[:, :n])
        P.tt(rstd[:, :n], mean[:, :n], mean[:, :n], ALU.mult)
        P.tt(rstd[:, :n], psq[:, :n], rstd[:, :n], ALU.subtract)
        P.ts(rstd[:, :n], rstd[:, :n], 1e-5, ALU.add)
        P.act(rstd[:, :n], rstd[:, :n], AF.Sqrt)
        P.recip(rstd[:, :n], rstd[:, :n])
        for kc in range(KC):
            P.tt(SQ[:, kc, :n], nw[:, kc, :n], mean[:, :n], ALU.subtract)
            P.tt(SQ[:, kc, :n], SQ[:, kc, :n], rstd[:, :n], ALU.mult)
            P.ts(nw[:, kc, :n], SQ[:, kc, :n], VD[:, gi, kc:kc + 1], ALU.mult, VD[:, bi, kc:kc + 1], ALU.add)
            P.copy(ob[:, kc, :n], nw[:, kc, :n], eng="pool")

    def stage_ln0(s):
        with P.stage() as st:
            XT = [st.tile([128, D], F32) for _ in range(2)]
            NEW = st.tile([128, KC, 128], F32)
            SQ = st.tile([128, KC, 128], F32)
            OB = st.tile([128, KC, 128], BF16)
            mean = st.tile([128, 128], F32)
            rstd = st.tile([128, 128], F32)
            pst = [st.psum() for _ in range(2)]
            psm = st.psum()
            psq = st.psum()
            for ti, (o, sz) in enumerate(cfg.tiles):
                xt = XT[ti % 2]
                if ti == 0:
                    P.dma("sp", xt[:sz, :], meta[:, :], xt)
                else:
                    P.dma("sp", xt[:sz, :], x_in[s, o - NMETA:o - NMETA + sz, :], xt)
                for g in range(4):
                    ps = pst[g % 2]
                    for j in range(4):
                        kc = g * 4 + j
                        P.tr(ps[:, j * 128:j * 128 + sz], xt[:sz, kc * 128:(kc + 1) * 128], ident(sz))
                    P.copy(NEW[:, g * 4:(g + 1) * 4, :sz],
                           va_re(ps[:, :], "p (a b) -> p a b", a=4)[:, :, :sz] if False else
                           VA(ps.t[:, :].rearrange("p (a b) -> p a b", a=4)[:, :, :sz], ps.res))
                ln_core(st, NEW, SQ, OB, mean, rstd, psm, psq, sz, 0, 1)
                P.dma("sp", HT32v[:, :, o:o + sz], NEW[:, :, :sz], NEW)
                P.dma("sp", HTbv[:, :, o:o + sz], OB[:, :, :sz], OB)

    blocks = col_chunks()

    def stage_win(l):
        wv = w_in.view("l (kc p) n -> l p kc n", p=128)
        with P.stage() as st:
            mk_stg(st, 3)
            XT = [st.tile([128, KC, 512], BF16) for _ in range(2)]
            W = [st.tile([128, KC, 512], BF16) for _ in range(3)]
            ZS = [st.tile([128, 512], F32) for _ in range(4)]
            PS = [st.psum() for _ in range(4)]
            jobs = [(bi_, o, n, blk, bj) for bi_, (o, n) in enumerate(cfg.sbs) for bj, blk in enumerate(blocks)]
            cnt = [0]

            def load(i):
                bi_, o, n, blk, bj = jobs[i]
                if bj == 0:
                    xt = XT[bi_ % 2]
                    P.dma("sp", xt[:, :, :n], HTbv[:, :, o:o + n], xt)
                c0 = blk[0][0]
                cw = blk[-1][0] + blk[-1][1] - c0
                w = W[i % 3]
                P.dma("sp", w[:, :, :cw], cview("win", l * NB_WIN + bj, KC, cw), w, disjoint=False)

            def compute(i):
                bi_, o, n, blk, bj = jobs[i]
                xt = XT[bi_ % 2]
                w = W[i % 3]
                c0 = blk[0][0]
                for (c, m) in blk:
                    ps = PS[cnt[0] % 4]
                    zs = ZS[cnt[0] % 4]
                    cnt[0] += 1
                    for kc in range(KC):
                        P.mm(ps[:m, :n], w[:, kc, c - c0:c - c0 + m], xt[:, kc, :n], start=(kc == 0),
                             stop=(kc == KC - 1))
                    P.act(zs[:m, :n], ps[:m, :n], AF.Sigmoid if c >= C3 else AF.Copy)
                    P.dma("sp", ZT[c:c + m, o:o + n], zs[:m, :n], zs)
            pipelined(len(jobs), load, compute, depth=2)

    def stage_m1(l):
        prv = p_r.view("l (kc p) n -> l p kc n", p=128)
        pfv = p_f.view("l (kc p) n -> l p kc n", p=128)
        pmv = p_m.view("l (kc p) n -> l p kc n", p=128)
        with P.stage() as st:
            PR = st.tile([128, 6, D], BF16)
            PF = st.tile([128, 6, D], BF16)
            PM = st.tile([128, 4, D], BF16)
            P.dma("sp", PR[:, :, :], cview("pr", l, 6, D), PR)
            P.dma("sp", PF[:, :, :], cview("pf", l, 6, D), PF)
            P.dma("sp", PM[:, :, :], cview("pm", l, 4, D), PM)
            Y = [st.tile([128, KC, 512], BF16) for _ in range(2)]
            G = [[st.tile([128, 512], F32) for _ in range(3)] for _ in range(2)]
            A1 = [st.tile([128, 512], F32) for _ in range(2)]
            A2 = [st.tile([128, 512], F32) for _ in range(2)]
            MO = [st.tile([128, 512], BF16) for _ in range(2)]
            PS = [[st.psum() for _ in range(3)] for _ in range(2)]
            it = 0
            for bi_, (o, n) in enumerate(cfg.sbs):
                y = Y[bi_ % 2]
                P.dma("sp", y[:, :, :n], YTv[:, :, o:o + n], y)
                for m in range(KC):
                    g3 = G[it % 2]
                    ps3 = PS[it % 2]
                    a1, a2, mo = A1[it % 2], A2[it % 2], MO[it % 2]
                    it += 1
                    for gi in range(3):
                        r0 = C3 + gi * D + m * 128
                        P.dma("sp", g3[gi][:, :n], ZT[r0:r0 + 128, o:o + n], g3[gi])
                    for kc in range(6):
                        P.mm(ps3[0][:, :n], PR[:, kc, m * 128:(m + 1) * 128], y[:, kc, :n], start=(kc == 0), stop=(kc == 5))
                    for kc in range(6):
                        P.mm(ps3[1][:, :n], PF[:, kc, m * 128:(m + 1) * 128], y[:, 6 + kc, :n], start=(kc == 0), stop=(kc == 5))
                    for kc in range(4):
                        P.mm(ps3[2][:, :n], PM[:, kc, m * 128:(m + 1) * 128], y[:, 12 + kc, :n], start=(kc == 0), stop=(kc == 3))
                    P.tt(a1[:, :n], ps3[0][:, :n], g3[0][:, :n], ALU.mult)
                    P.tt(a2[:, :n], ps3[1][:, :n], g3[1][:, :n], ALU.mult)
                    P.tt(a1[:, :n], a1[:, :n], a2[:, :n], ALU.add)
                    P.tt(a2[:, :n], ps3[2][:, :n], g3[2][:, :n], ALU.mult)
                    P.tt(mo[:, :n], a1[:, :n], a2[:, :n], ALU.add)
                    P.dma("sp", MIXT[m * 128:(m + 1) * 128, o:o + n], mo[:, :n], mo)

    def stage_proj_res(xv, nkc, wview, WB=512):
        with P.stage() as st:
            mk_stg(st, 3)
            XT = [st.tile([128, nkc, 512], BF16) for _ in range(2)]
            W = [st.tile([128, nkc, WB], BF16) for _ in range(2)]
            HR = [st.tile([128, 512], F32) for _ in range(3)]
            PS = [st.psum() for _ in range(3)]
            jobs = [(bi_, o, n, cb) for bi_, (o, n) in enumerate(cfg.sbs) for cb in range(D // WB)]
            it = [0]

            def load(i):
                bi_, o, n, cb = jobs[i]
                if cb == 0:
                    xt = XT[bi_ % 2]
                    P.dma("sp", xt[:, :, :n], xv[:, :, o:o + n], xt)
                w = W[i % 2]
                P.dma("sp", w[:, :, :], wview(cb), w, disjoint=False)

            def compute(i):
                bi_, o, n, cb = jobs[i]
                xt = XT[bi_ % 2]
                w = W[i % 2]
                for mm_ in range(WB // 128):
                    m = cb * (WB // 128) + mm_
                    ps = PS[it[0] % 3]
                    hr = HR[it[0] % 3]
                    it[0] += 1
                    P.dma("sp", hr[:, :n], HT32[m * 128:(m + 1) * 128, o:o + n], hr)
                    for kc in range(nkc):
                        P.mm(ps[:, :n], w[:, kc, mm_ * 128:(mm_ + 1) * 128], xt[:, kc, :n], start=(kc == 0),
                             stop=(kc == nkc - 1))
                    P.stt(hr[:, :n], hr[:, :n], ALPHA, ps[:, :n], ALU.mult, ALU.add)
                    P.dma("sp", NEWT[m * 128:(m + 1) * 128, o:o + n], hr[:, :n], hr)
            pipelined(len(jobs), load, compute, depth=1)

    def stage_m2(l):
        stage_proj_res(MIXTv, KC, lambda cb: cview("wo", l * 4 + cb, KC, 512))

    def stage_ffn(l):
        moe = (l % 2 == 1)
        li = l // 2
        nexp = NE if moe else 1
        with P.stage() as st:
            XT = st.tile([128, KC, 512], BF16)
            HID = st.tile([128, FC, 512], BF16)
            W1 = [st.tile([128, KC, 256], BF16) for _ in range(2)]
            W3 = [st.tile([128, KC, 256], BF16) for _ in range(2)]
            W2 = [st.tile([128, FC, 128], BF16) for _ in range(2)]
            SIL = [st.tile([128, 512], F32) for _ in range(2)]
            HR = [st.tile([128, 512], F32) for _ in range(2)]
            PA = [st.psum() for _ in range(2)]
            PB = [st.psum() for _ in range(2)]
            PO = [st.psum() for _ in range(2)]
            mk_stg(st, 2)
            if moe:
                ACC = st.tile([128, KC, 512], F32)
                GBC = st.tile([128, NE, 512], BF16)
                X32 = st.tile([128, KC, 128], F32)
                RWT = st.tile([128, KC, NE], F32)
                LG = st.tile([128, 8], F32)
                MX = st.tile([128, 8], F32)
                EX = st.tile([128, 8], F32)
                MK = st.tile([128, 8], F32)
                SC = st.tile([128, 4], F32)
                DG = st.tile([128, 128], F32)
                PR_ = st.psum()
                P.dma("sp", RWT[:, :, :], r_w.view("l (kc p) e -> l p kc e", p=128)[li], RWT)
            it = 0
            wi = 0
            w2i = 0
            for bi_, (o, n) in enumerate(cfg.sbs):
                P.dma("sp", XT[:, :, :n], HTbv[:, :, o:o + n], XT)
                if moe:
                    for t0 in range(0, n, 128):
                        tn = min(128, n - t0)
                        P.dma("sp", X32[:, :, :tn], HT32v[:, :, o + t0:o + t0 + tn], X32)
                        for kc in range(KC):
                            P.mm(PR_[:tn, 0:8], X32[:, kc, :tn], RWT[:, kc, :], start=(kc == 0), stop=(kc == KC - 1))
                        P.copy(LG[:tn, :], PR_[:tn, 0:8])
                        P.op("dve", lambda e: e.max(out=MX.t[:tn, :], in_=LG.t[:tn, :]), [LG], [MX], disjoint=True)
                        P.ts(MK[:tn, :], LG[:tn, :], MX[:tn, 1:2], ALU.is_ge)
                        P.ts(SC[:tn, 0:1], MX[:tn, 0:1], -1.0, ALU.mult)
                        P.act(EX[:tn, :], LG[:tn, :], AF.Exp, bias=SC[:tn, 0:1])
                        P.tt(EX[:tn, :], EX[:tn, :], MK[:tn, :], ALU.mult)
                        P.op("dve", lambda e: e.reduce_sum(out=SC.t[:tn, 1:2], in_=EX.t[:tn, :], axis=AX.X), [EX], [SC],
                             disjoint=True)
                        P.recip(SC[:tn, 2:3], SC[:tn, 1:2])
                        P.ts(EX[:tn, :], EX[:tn, :], SC[:tn, 2:3], ALU.mult)
                        for e_ in range(NE):
                            P.ts(DG[:tn, :tn], CON[:tn, CO_ID:CO_ID + tn], EX[:tn, e_:e_ + 1], ALU.mult)
                            P.mm(PR_[:, 128:128 + tn], CON[:tn, CO_ONE:CO_ONE + 128], DG[:tn, :tn])
                            P.copy(GBC[:, e_, t0:t0 + tn], PR_[:, 128:128 + tn])
                for e_ in range(nexp):
                    if moe:
                        w1v = m_w1.view("l e (kc p) n -> l e p kc n", p=128)[li, e_]
                        w3v = m_w3.view("l e (kc p) n -> l e p kc n", p=128)[li, e_]
                        w2v = m_w2.view("l e (kc p) n -> l e p kc n", p=128)[li, e_]
                    else:
                        w1v = f_w1.view("l (kc p) n -> l p kc n", p=128)[li]
                        w3v = f_w3.view("l (kc p) n -> l p kc n", p=128)[li]
                        w2v = f_w2.view("l (kc p) n -> l p kc n", p=128)[li]
                    for cb in range(DFF // 256):
                        w1 = W1[wi % 2]
                        w3 = W3[wi % 2]
                        wi += 1
                        P.dma("sp", w1[:, :, :], cview("w1", ffn_idx(l, e_) * 22 + cb, KC, 256), w1, disjoint=False)
                        P.dma("sp", w3[:, :, :], cview("w3", ffn_idx(l, e_) * 22 + cb, KC, 256), w3, disjoint=False)
                        for j in range(2):
                            f = cb * 2 + j
                            pa, pb, sil = PA[it % 2], PB[it % 2], SIL[it % 2]
                            it += 1
                            for kc in range(KC):
                                P.mm(pa[:, :n], w1[:, kc, j * 128:(j + 1) * 128], XT[:, kc, :n], start=(kc == 0), stop=(kc == KC - 1))
                            for kc in range(KC):
                                P.mm(pb[:, :n], w3[:, kc, j * 128:(j + 1) * 128], XT[:, kc, :n], start=(kc == 0), stop=(kc == KC - 1))
                            P.act(sil[:, :n], pa[:, :n], AF.Silu)
                            if moe:
                                P.tt(sil[:, :n], sil[:, :n], pb[:, :n], ALU.mult)
                                P.tt(HID[:, f, :n], sil[:, :n], GBC[:, e_, :n], ALU.mult)
                            else:
                                P.tt(HID[:, f, :n], sil[:, :n], pb[:, :n], ALU.mult)
                    for m in range(KC):
                        w2 = W2[w2i % 2]
                        po = PO[w2i % 2]
                        hr = HR[w2i % 2]
                        w2i += 1
                        P.dma("sp", w2[:, :, :], cview("w2", ffn_idx(l, e_) * 16 + m, FC, 128), w2, disjoint=False)
                        for f in range(FC):
                            P.mm(po[:, :n], w2[:, f, :], HID[:, f, :n], start=(f == 0), stop=(f == FC - 1))
                        if moe and e_ > 0:
                            P.tt(ACC[:, m, :n], ACC[:, m, :n], po[:, :n], ALU.add)
                        elif moe:
                            P.copy(ACC[:, m, :n], po[:, :n])
                        if (not moe) or e_ == nexp - 1:
                            P.dma("sp", hr[:, :n], HT32[m * 128:(m + 1) * 128, o:o + n], hr)
                            src_ = ACC[:, m, :n] if moe else po[:, :n]
                            P.stt(hr[:, :n], hr[:, :n], ALPHA, src_, ALU.mult, ALU.add)
                            P.dma("sp", NEWT[m * 128:(m + 1) * 128, o:o + n], hr[:, :n], hr)

    def stage_out(s):
        with P.stage() as st:
            HTt = [st.tile([128, KC, 128], F32) for _ in range(2)]
            OT = [st.tile([128, D], F32) for _ in range(2)]
            PS = [st.psum() for _ in range(2)]
            for ti, (o, sz) in enumerate(cfg.tiles[1:]):
                ht = HTt[ti % 2]
                ot = OT[ti % 2]
                P.dma("sp", ht[:, :, :], HT32v[:, :, o:o + sz], ht)
                for g in range(4):
                    ps = PS[g % 2]
                    for j in range(4):
                        kc = g * 4 + j
                        P.tr(ps[:, j * 128:(j + 1) * 128], ht[:, kc, :], ident(128))
                    P.copy(ot[:, g * 512:(g + 1) * 512], ps[:, :])
                P.dma("sp", out_t[s, o - NMETA:o - NMETA + sz, :], ot[:, :], ot)

    def stage_fox(l):
        tl = cfg.tiles
        with P.stage() as st:
            FG = st.tile([12, L], F32)
            CUM = st.tile([12, L], F32)
            ONE = st.tile([12, L], F32)
            NB = st.tile([12, 2], F32)
            ENDS = st.tile([12, NTl], F32)
            Q = st.tile([128, 6, L], BF16)
            Kt = st.tile([128, 6, L], BF16)
            VP = st.tile([128, NTl, 12, 65], BF16)
            CREF = st.tile([128, 12, NTl], F32)
            CUMK = st.tile([128, NTl, 12], F32)
            BIAS = st.tile([128, NTl, 12, NTl], F32)
            VT = [st.tile([128, 6, 128], F32) for _ in range(2)]
            PT = [st.tile([128, 128], BF16) for _ in range(3)]
            YTK = [st.tile([128, FW], F32) for _ in range(2)]
            RC = [st.tile([128, 1], F32) for _ in range(2)]
            YF = [st.tile([128, 6, 128], BF16) for _ in range(2)]
            PS_S = [st.psum() for _ in range(3)]
            PS_O = [st.psum() for _ in range(2)]
            PS_T = [st.psum() for _ in range(2)]
            zq = ZT.view("(c p) l -> p c l", p=128) if False else None
            P.dma("sp", FG[:, :], ZT[C1 + 2304:C1 + 2316, :], FG)
            P.dma("sp", NB[:, 0:1], foxbf[l], NB)
            P.ts(NB[:, 1:2], NB[:, 0:1], -1.0, ALU.mult)
            P.memset(ONE[:, :], 1.0)
            P.act(FG[:, :], FG[:, :], AF.Exp, bias=NB[:, 1:2], scale=-1.0)
            P.act(FG[:, :], FG[:, :], AF.Ln, bias=1.0)
            P.ts(FG[:, :], FG[:, :], -1.0, ALU.mult)
            P.scan(CUM[:, :], ONE[:, :], FG[:, :], 0.0, ALU.mult, ALU.add)
            qv = VA(ZT.t[C1:C1 + 768, :].rearrange("(c p) l -> p c l", p=128), ZT.res)
            kv = VA(ZT.t[C1 + 768:C1 + 1536, :].rearrange("(c p) l -> p c l", p=128), ZT.res)
            vv = VA(ZT.t[C1 + 1536:C1 + 2304, :].rearrange("(c p) l -> p c l", p=128), ZT.res)
            mk_stg(st, 2)
            wload(st, Q[:, :, :], qv, 128, 6, L)
            wload(st, Kt[:, :, :], kv, 128, 6, L)
            P.memset(VP[:, :, :, :], 1.0, eng="pool")
            for j, (o, sz) in enumerate(tl):
                P.copy(ENDS[:, j:j + 1], CUM[:, o + sz - 1:o + sz])
                ps = PS_T[j % 2]
                P.tr(ps[:sz, 0:12], CUM[:, o:o + sz], ident(12))
                P.copy(CUMK[:sz, j, :], ps[:sz, 0:12])
                vt = VT[j % 2]
                P.dma("sp", vt[:, :, :sz], vv[:, :, o:o + sz], vt)
                for c in range(6):
                    ps2 = PS_S[c % 3]
                    P.tr(ps2[:sz, 0:128], vt[:, c, :sz], ident(128))
                    P.copy(VP[:sz, j, 2 * c:2 * c + 2, 0:64],
                           VA(ps2.t[:sz, 0:128].rearrange("p (a b) -> p a b", a=2), ps2.res))
            psc = PS_O[0]
            for h in range(12):
                P.mm(psc[:, h * NTl:(h + 1) * NTl], CON[:12, CO_SEL + h * 128:CO_SEL + (h + 1) * 128], ENDS[:, :])
            P.copy(CREF[:, :, :], VA(psc.t[:, 0:12 * NTl].rearrange("p (a b) -> p a b", a=12), psc.res))
            for j, (o, sz) in enumerate(tl):
                for h in range(12):
                    P.ts(BIAS[:sz, j, h, :], CREF[:sz, h, :], CUMK[:sz, j, h:h + 1], ALU.subtract)
            it = 0
            for i, (oi, si) in enumerate(tl):
                ytk = YTK[i % 2]
                for h in range(12):
                    c, pr = h // 2, (h % 2) * 64
                    po = PS_O[h % 2]
                    rc = RC[h % 2]
                    for j in range(i + 1):
                        oj, sj = tl[j]
                        ps = PS_S[it % 3]
                        pt = PT[it % 3]
                        it += 1
                        P.mm(ps[:sj, :si], Kt[pr:pr + 64, c, oj:oj + sj], Q[pr:pr + 64, c, oi:oi + si])
                        P.act(pt[:sj, :si], ps[:sj, :si], AF.Exp, bias=BIAS[:sj, j, h, i:i + 1], scale=0.125)
                        if j == i:
                            P.tt(pt[:sj, :si], pt[:sj, :si], CONB[:sj, CO_MLE:CO_MLE + si], ALU.mult, eng="pool")
                        P.mm(po[:si, 0:65], pt[:sj, :si], VP[:sj, j, h, :], start=(j == 0), stop=(j == i))
                    P.recip(rc[:si, :], po[:si, 64:65])
                    P.ts(ytk[:si, h * 64:(h + 1) * 64], po[:si, 0:64], rc[:si, 0:1], ALU.mult)
                yf = YF[i % 2]
                for c in range(6):
                    ps = PS_T[c % 2]
                    P.tr(ps[:, :si], ytk[:si, c * 128:(c + 1) * 128], ident(si))
                    P.copy(yf[:, c, :si], ps[:, :si], eng="act")
                P.dma("sp", YTv[:, 6:12, oi:oi + si], yf[:, :, :si], yf)

    def stage_mlstm(l):
        tl = cfg.tiles
        SCL = 128 ** -0.5
        with P.stage() as st:
            XP = [st.tile([128, 3 + L], F32) for _ in range(2)]
            ACC = st.tile([128, L], F32)
            QT = st.tile([128, 4, L], BF16)
            KT = st.tile([128, 4, L], BF16)
            KTOK = st.tile([128, NTl, 4, 128], BF16)
            VP = st.tile([128, NTl, 4, 129], BF16)
            CW = st.tile([128, 8, 4], F32)
            IG = st.tile([4, L], F32)
            FGm = st.tile([4, L], F32)
            ONE = st.tile([4, L], F32)
            Bc = st.tile([4, L], F32)
            WK = st.tile([4, L], F32)
            EB = st.tile([4, L], F32)
            MB = st.tile([4, 4], F32)
            ENDE = st.tile([4, NTl], F32)
            GT = st.tile([128, NTl, 8], F32)
            EBE = st.tile([128, 4, NTl], F32)
            CN = st.tile([128, 4, 129], F32)
            CNB = st.tile([128, 4, 129], BF16)
            VT = [st.tile([128, 4, 128], F32) for _ in range(2)]
            OG = [st.tile([128, 4, 128], F32) for _ in range(2)]
            GM = [st.tile([128, 128], BF16) for _ in range(2)]
            HTK = [st.tile([128, MW], F32) for _ in range(2)]
            SC = [st.tile([128, 4], F32) for _ in range(2)]
            YM = [st.tile([128, 4, 128], BF16) for _ in range(2)]
            PS_G = [st.psum() for _ in range(2)]
            PS_O = [st.psum() for _ in range(2)]
            PS_C = st.psum()
            PS_T = [st.psum() for _ in range(2)]
            PS_TB = st.psum([128, 1024], BF16)
            P.dma("sp", CW[:, :, :], convw[:, l, :, :], CW)
            P.dma("sp", MB[:, 0:2], mlb[l], MB)
            P.ts(MB[:, 2:3], MB[:, 1:2], -1.0, ALU.mult)
            P.dma("sp", IG[:, :], ZT[C2 + 2048:C2 + 2052, :], IG)
            P.dma("sp", FGm[:, :], ZT[C2 + 2052:C2 + 2056, :], FGm)
            P.memset(ONE[:, :], 1.0)
            P.act(FGm[:, :], FGm[:, :], AF.Exp, bias=MB[:, 2:3], scale=-1.0)
            P.act(FGm[:, :], FGm[:, :], AF.Ln, bias=1.0)
            P.ts(FGm[:, :], FGm[:, :], -1.0, ALU.mult)
            for j, (o, sz) in enumerate(tl):
                P.scan(Bc[:, o:o + sz], ONE[:, o:o + sz], FGm[:, o:o + sz], 0.0, ALU.mult, ALU.add)
                P.copy(ENDE[:, j:j + 1], Bc[:, o + sz - 1:o + sz])
            P.act(ENDE[:, :], ENDE[:, :], AF.Exp)
            P.tt(WK[:, :], IG[:, :], Bc[:, :], ALU.subtract)
            P.act(WK[:, :], WK[:, :], AF.Exp, bias=MB[:, 0:1])
            P.act(EB[:, :], Bc[:, :], AF.Exp)
            for h in range(4):
                P.mm(PS_C[:, h * NTl:(h + 1) * NTl], CON[:4, CO_SEL + h * 128:CO_SEL + (h + 1) * 128], ENDE[:, :])
            P.copy(EBE[:, :, :], VA(PS_C.t[:, 0:4 * NTl].rearrange("p (a b) -> p a b", a=4), PS_C.res))
            for c in range(8):
                xp = XP[c % 2]
                P.memset(xp[:, 0:3], 0.0)
                P.dma("sp", xp[:, 3:3 + L], ZT[C2 + c * 128:C2 + (c + 1) * 128, :], xp)
                P.ts(ACC[:, :], xp[:, 0:L], CW[:, c, 0:1], ALU.mult)
                for jj in range(1, 4):
                    P.stt(ACC[:, :], xp[:, jj:jj + L], CW[:, c, jj:jj + 1], ACC[:, :], ALU.mult, ALU.add)
                if c < 4:
                    P.act(QT[:, c, :], ACC[:, :], AF.Silu)
                else:
                    P.act(ACC[:, :], ACC[:, :], AF.Silu)
                    P.ts(KT[:, c - 4, :], ACC[:, :], SCL, ALU.mult)
            vv = VA(ZT.t[C2 + 1024:C2 + 1536, :].rearrange("(c p) l -> p c l", p=128), ZT.res)
            ov = VA(ZT.t[C2 + 1536:C2 + 2048, :].rearrange("(c p) l -> p c l", p=128), ZT.res)
            for j, (o, sz) in enumerate(tl):
                pt = PS_T[j % 2]
                P.tr(pt[:sz, 0:4], WK[:, o:o + sz], ident(4))
                P.tr(pt[:sz, 4:8], EB[:, o:o + sz], ident(4))
                P.copy(GT[:sz, j, :], pt[:sz, 0:8])
                vt = VT[j % 2]
                P.dma("sp", vt[:, :, :sz], vv[:, :, o:o + sz], vt)
                for h in range(4):
                    P.tr(pt[:sz, 128 * (h % 2) + 128:128 * (h % 2) + 256], vt[:, h, :sz], ident(128))
                    P.ts(VP[:sz, j, h, 0:128], pt[:sz, 128 * (h % 2) + 128:128 * (h % 2) + 256], GT[:sz, j, h:h + 1], ALU.mult)
                    P.copy(VP[:sz, j, h, 128:129], GT[:sz, j, h:h + 1])
                    P.tr(PS_TB[:sz, h * 128:(h + 1) * 128], KT[:, h, o:o + sz], CONB[:, CO_ID:CO_ID + 128])
                P.copy(KTOK[:sz, j, :, :], VA(PS_TB.t[:sz, 0:512].rearrange("p (a b) -> p a b", a=4), PS_TB.res))
            for j, (o, sz) in enumerate(tl):
                htk = HTK[j % 2]
                sc = SC[j % 2]
                og = OG[j % 2]
                P.dma("sp", og[:, :, :sz], ov[:, :, o:o + sz], og)
                P.act(og[:, :, :sz], og[:, :, :sz], AF.Sigmoid)
                for h in range(4):
                    pg = PS_G[h % 2]
                    po = PS_O[h % 2]
                    gm = GM[h % 2]
                    P.mm(pg[:sz, :sz], KT[:, h, o:o + sz], QT[:, h, o:o + sz])
                    P.tt(gm[:sz, :sz], pg[:sz, :sz], CON[:sz, CO_MLE:CO_MLE + sz], ALU.mult)
                    P.mm(po[:sz, 0:129], gm[:sz, :sz], VP[:sz, j, h, :], start=True, stop=(j == 0))
                    if j > 0:
                        P.mm(po[:sz, 0:129], QT[:, h, o:o + sz], CNB[:, h, :], start=False, stop=True)
                    P.tt(sc[:sz, 0:1], po[:sz, 128:129], GT[:sz, j, 4 + h:5 + h], ALU.mult)
                    P.ts(sc[:sz, 1:2], sc[:sz, 0:1], -1.0, ALU.mult)
                    P.tt(sc[:sz, 1:2], sc[:sz, 1:2], sc[:sz, 0:1], ALU.max)
                    P.ts(sc[:sz, 1:2], sc[:sz, 1:2], 1.0, ALU.max)
                    P.recip(sc[:sz, 2:3], sc[:sz, 1:2])
                    P.tt(sc[:sz, 3:4], sc[:sz, 2:3], GT[:sz, j, 4 + h:5 + h], ALU.mult)
                    P.ts(htk[:sz, h * 128:(h + 1) * 128], po[:sz, 0:128], sc[:sz, 3:4], ALU.mult)
                    P.mm(PS_C[:, 0:129], KTOK[:sz, j, h, :], VP[:sz, j, h, :])
                    if j == 0:
                        P.ts(CN[:, h, :], PS_C[:, 0:129], EBE[:, h, j:j + 1], ALU.mult)
                    else:
                        P.tt(CN[:, h, :], CN[:, h, :], PS_C[:, 0:129], ALU.add)
                        P.ts(CN[:, h, :], CN[:, h, :], EBE[:, h, j:j + 1], ALU.mult)
                    P.copy(CNB[:, h, :], CN[:, h, :])
                ym = YM[j % 2]
                for h in range(4):
                    pt = PS_T[h % 2]
                    P.tr(pt[:, :sz], htk[:sz, h * 128:(h + 1) * 128], ident(sz))
                    P.tt(ym[:, h, :sz], pt[:, :sz], og[:, h, :sz], ALU.mult)
                P.dma("sp", YTv[:, 12:16, o:o + sz], ym[:, :, :sz], ym)

    def stage_rwkv(l, part=0, nlev_max=99, sub=99):
        tl = cfg.tiles
        BLK = CON[:, CO_BLK:CO_BLK + 128]
        RWSv = RWS.view("q (c p) l -> p q c l", p=128)
        with P.stage() as st:
            W2 = st.tile([96, RW], BF16)
            A2 = st.tile([96, RW], BF16)
            G2 = st.tile([128, 2, RW], BF16)
            mk_stg(st, 2)
            wload(st, VA(W2.t[:, :].rearrange("p (a b) -> p a b", a=1), W2.res),
                  VA(rw2.t[l].rearrange("p (a b) -> p a b", a=1), rw2.res), 96, 1, RW)
            wload(st, VA(A2.t[:, :].rearrange("p (a b) -> p a b", a=1), A2.res),
                  VA(ra2.t[l].rearrange("p (a b) -> p a b", a=1), ra2.res), 96, 1, RW)
            wload(st, G2[:, :, :], rg2.view("l (kc p) n -> l p kc n", p=128)[l], 128, 2, RW)
            ZP = [st.tile([128, 6, 513], F32) for _ in range(3)]
            ZL = [st.tile([128, 513], F32) for _ in range(4)]
            TMP = st.tile([128, 512], F32)
            XR = st.tile([128, 6, 512], F32)
            XK = st.tile([128, 6, 512], F32)
            XV = st.tile([128, 6, 512], F32)
            LW = st.tile([128, 6, 512], F32)
            AA = st.tile([128, 6, 512], F32)
            GG = st.tile([128, 6, 512], F32)
            KK = st.tile([128, 6, 512], F32)
            BB = st.tile([128, 6, 512], F32)
            BON = st.tile([128, 6, 512], F32)
            TW = st.tile([96, 512], BF16)
            TA = st.tile([96, 512], BF16)
            SG = st.tile([128, 2, 512], BF16)
            PS = [st.psum() for _ in range(4)]
            pi = 0
            for (o, n) in cfg.sbs:
                def load_shift(zp_slice, rows0, nrows, mu_col, out):
                    pass
                for qi, (r0, X) in enumerate([(0, XR), (768, XK), (1536, XV)]):
                    zp = ZP[qi]
                    src_ = VA(ZT.t[r0:r0 + 768, :].rearrange("(c p) l -> p c l", p=128), ZT.res)
                    if o == 0:
                        P.memset(zp[:, :, 0:1], 0.0)
                        P.dma("sp", zp[:, :, 1:1 + n], src_[:, :, 0:n], zp)
                    else:
                        P.dma("sp", zp[:, :, 0:1 + n], src_[:, :, o - 1:o + n], zp, disjoint=False)
                    for c in range(6):
                        P.tt(TMP[:, :n], zp[:, c, 0:n], zp[:, c, 1:1 + n], ALU.subtract)
                        P.stt(X[:, c, :n], TMP[:, :n], VR[:, l, qi, c:c + 1], zp[:, c, 1:1 + n], ALU.mult, ALU.add)
                for qi, (r0, nr, vi, vc) in enumerate([(2304, 96, 10, 0), (2400, 96, 11, 0), (2496, 128, 12, 0), (2624, 128, 12, 1)]):
                    zl = ZL[qi]
                    if o == 0:
                        P.memset(zl[:nr, 0:1], 0.0)
                        P.dma("sp", zl[:nr, 1:1 + n], ZT[r0:r0 + nr, 0:n], zl)
                    else:
                        P.dma("sp", zl[:nr, 0:1 + n], ZT[r0:r0 + nr, o - 1:o + n], zl, disjoint=False)
                    P.tt(TMP[:nr, :n], zl[:nr, 0:n], zl[:nr, 1:1 + n], ALU.subtract)
                    P.stt(TMP[:nr, :n], TMP[:nr, :n], VR[:nr, l, vi, vc:vc + 1], zl[:nr, 1:1 + n], ALU.mult, ALU.add)
                    if qi == 0:
                        P.act(TW[:, :n], TMP[:96, :n], AF.Tanh)
                    elif qi == 1:
                        P.copy(TA[:, :n], TMP[:96, :n])
                    else:
                        P.act(SG[:, qi - 2, :n], TMP[:, :n], AF.Sigmoid)
                for c in range(6):
                    cs = slice(c * 128, (c + 1) * 128)
                    ps = PS[pi % 4]; pi += 1
                    P.mm(ps[:, :n], W2[:, cs], TW[:, :n])
                    P.act(LW[:, c, :n], ps[:, :n], AF.Sigmoid, bias=VR[:, l, 3, c:c + 1])
                    P.ts(LW[:, c, :n], LW[:, c, :n], -0.6065306597126334, ALU.mult)
                    ps = PS[pi % 4]; pi += 1
                    P.mm(ps[:, :n], A2[:, cs], TA[:, :n])
                    P.act(AA[:, c, :n], ps[:, :n], AF.Sigmoid, bias=VR[:, l, 4, c:c + 1])
                    ps = PS[pi % 4]; pi += 1
                    P.mm(ps[:, :n], G2[:, 0, cs], SG[:, 0, :n], start=True, stop=False)
                    P.mm(ps[:, :n], G2[:, 1, cs], SG[:, 1, :n], start=False, stop=True)
                    P.copy(GG[:, c, :n], ps[:, :n], eng="act")
                    P.ts(KK[:, c, :n], XK[:, c, :n], VR[:, l, 5, c:c + 1], ALU.mult)
                    P.tt(TMP[:, :n], KK[:, c, :n], KK[:, c, :n], ALU.mult)
                    ps = PS[pi % 4]; pi += 1
                    P.mm(ps[:, :n], BLK, TMP[:, :n])
                    P.act(TMP[:, :n], ps[:, :n], AF.Sqrt)
                    P.ts(TMP[:, :n], TMP[:, :n], 1e-12, ALU.max)
                    P.recip(TMP[:, :n], TMP[:, :n])
                    P.tt(KK[:, c, :n], KK[:, c, :n], TMP[:, :n], ALU.mult)
                    P.ts(TMP[:, :n], AA[:, c, :n], -1.0, ALU.add, VR[:, l, 6, c:c + 1], ALU.mult)
                    P.ts(TMP[:, :n], TMP[:, :n], 1.0, ALU.add)
                    P.tt(XK[:, c, :n], XK[:, c, :n], TMP[:, :n], ALU.mult)
                    P.tt(BB[:, c, :n], AA[:, c, :n], KK[:, c, :n], ALU.mult)
                    P.tt(TMP[:, :n], XR[:, c, :n], XK[:, c, :n], ALU.mult)
                    P.ts(TMP[:, :n], TMP[:, :n], VR[:, l, 7, c:c + 1], ALU.mult)
                    ps = PS[pi % 4]; pi += 1
                    P.mm(ps[:, :n], BLK, TMP[:, :n])
                    P.tt(BON[:, c, :n], ps[:, :n], XV[:, c, :n], ALU.mult)
                for qi, X in enumerate([XR, XK, XV, LW, KK, BB, BON, GG]):
                    P.dma("sp", RWSv[:, qi, :, o:o + n], X[:, :, :n], X)
        if part == 1:
            return
        with P.stage() as st:
            X8 = [st.tile([128, 8, 128], F32) for _ in range(2)]
            ONE = st.tile([128, 128], F32)
            CWt = st.tile([128, 128], F32)
            CWX = st.tile([128, 128], F32)
            EW = st.tile([128, 3, 128], F32)
            QS = st.tile([128, 4, 128], F32)
            TOK = st.tile([128, 3, 128], F32)
            SST = st.tile([128, 6, 64], F32)
            AM = [st.tile([128, 5, 128], F32) for _ in range(2)]
            PW = [st.tile([128, 2, 128], F32) for _ in range(2)]
            UU = st.tile([128, 128], F32)
            OTOK = st.tile([128, 128], F32)
            OT = st.tile([128, 128], F32)
            T1 = st.tile([128, 128], F32)
            T2 = st.tile([128, 128], F32)
            YR = [st.tile([128, 128], BF16) for _ in range(2)]
            PA = [st.psum() for _ in range(3)]
            PB = [st.psum() for _ in range(2)]
            PC = [st.psum() for _ in range(2)]
            P.memset(ONE[:, :], 1.0)
            P.memset(SST[:, :, :], 0.0)
            MLE = CON[:, CO_MLE:CO_MLE + 128]
            MLT = CON[:, CO_MLT:CO_MLT + 128]
            MGE = CON[:, CO_MGE:CO_MGE + 128]
            it = 0
            for i, (o, sz) in enumerate(tl):
                nlev = min(int(np.ceil(np.log2(sz))), nlev_max)
                if nlev_max == 98 and i == 0:
                    continue
                for c in range(6):
                    x8 = X8[it % 2]
                    am = AM[it % 2]
                    yr = YR[it % 2]
                    it += 1
                    P.dma("sp", x8[:, :, :sz], RWSv[:, :, c, o:o + sz], x8)
                    R_, K_, V_, LW_, KK_, B_, BON_, G_ = [x8[:, q, :sz] for q in range(8)]
                    P.scan(CWt[:, :sz], ONE[:, :sz], LW_, 0.0, ALU.mult, ALU.add)
                    P.tt(CWX[:, :sz], CWt[:, :sz], LW_, ALU.subtract)
                    P.act(EW[:, 0, :sz], CWt[:, :sz], AF.Exp)
                    P.act(EW[:, 1, :sz], CWt[:, :sz], AF.Exp, scale=-1.0)
                    P.act(EW[:, 2, :sz], CWX[:, :sz], AF.Exp)
                    P.stt(QS[:, 0, :sz], KK_, -1.0, EW[:, 2, :sz], ALU.mult, ALU.mult)
                    P.tt(QS[:, 1, :sz], R_, EW[:, 0, :sz], ALU.mult)
                    P.tt(QS[:, 2, :sz], B_, EW[:, 1, :sz], ALU.mult)
                    P.tt(QS[:, 3, :sz], K_, EW[:, 1, :sz], ALU.mult)
                    pt = PA[0]
                    P.tr(pt[:sz, 0:128], V_, ident(128))
                    P.tr(pt[:sz, 128:256], QS[:, 2, :sz], ident(128))
                    P.tr(pt[:sz, 256:384], QS[:, 3, :sz], ident(128))
                    P.copy(TOK[:sz, :, :], VA(pt.t[:sz, 0:384].rearrange("p (a b) -> p a b", a=3), pt.res))
                    if sub == 0:
                        continue
                    for hh in range(2):
                        pr = slice(hh * 64, hh * 64 + 64)
                        AT = QS[pr, 0, :sz]
                        RT = QS[pr, 1, :sz]
                        BT = QS[pr, 2, :sz]
                        KT_ = QS[pr, 3, :sz]
                        p1, p2, p3 = PA[1], PA[2], PB[0]
                        P.mm(p1[:sz, 0:sz], BT, AT)
                        P.mm(p1[:sz, 128:128 + sz], BT, RT)
                        P.mm(p2[:sz, 0:sz], KT_, AT)
                        P.mm(p2[:sz, 128:128 + sz], KT_, RT)
                        P.mm(p3[:sz, 0:sz], AT, BT)
                        P.tt(am[:sz, 0, :sz], p1[:sz, 0:sz], MLT[:sz, :sz], ALU.mult)
                        P.tt(am[:sz, 1, :sz], p1[:sz, 128:128 + sz], MLE[:sz, :sz], ALU.mult)
                        P.tt(am[:sz, 2, :sz], p2[:sz, 0:sz], MLT[:sz, :sz], ALU.mult)
                        P.tt(am[:sz, 3, :sz], p2[:sz, 128:128 + sz], MLE[:sz, :sz], ALU.mult)
                        P.tt(am[:sz, 4, :sz], p3[:sz, 0:sz], MGE[:sz, :sz], ALU.mult)
                        if sub == 1:
                            continue
                        p4 = PB[1]
                        P.mm(p4[:sz, 0:64], AT, SST[pr, c, :], start=True, stop=False)
                        P.mm(p4[:sz, 0:64], am[:sz, 2, :sz], TOK[:sz, 0, pr], start=False, stop=True)
                        U = UU[:sz, pr]
                        P.copy(U, p4[:sz, 0:64])
                        Pm, PTm = am[:sz, 4, :sz], am[:sz, 0, :sz]
                        for lev in range(nlev):
                            pu = PC[lev % 2]
                            P.mm(pu[:sz, 0:64], PTm, U)
                            if lev < nlev - 1:
                                pw = PW[lev % 2]
                                pq, pq2 = PB[0], PB[1]
                                P.mm(pq[:sz, 0:sz], PTm, Pm)
                                P.mm(pq2[:sz, 0:sz], Pm, PTm)
                            P.tt(U, U, pu[:sz, 0:64], ALU.add)
                            if lev < nlev - 1:
                                P.copy(pw[:sz, 0, :sz], pq[:sz, 0:sz])
                                P.copy(pw[:sz, 1, :sz], pq2[:sz, 0:sz])
                                Pm, PTm = pw[:sz, 0, :sz], pw[:sz, 1, :sz]
                        if sub == 2:
                            continue
                        po = PC[0]
                        P.mm(po[:sz, 64:128], RT, SST[pr, c, :], start=True, stop=False)
                        P.mm(po[:sz, 64:128], am[:sz, 1, :sz], U, start=False, stop=False)
                        P.mm(po[:sz, 64:128], am[:sz, 3, :sz], TOK[:sz, 0, pr], start=False, stop=True)
                        P.copy(OTOK[:sz, pr], po[:sz, 64:128])
                    if sub in (1, 2, 3):
                        continue
                    psu = PA[1]
                    P.mm(psu[:, 0:128], TOK[:sz, 1, :], UU[:sz, :], start=True, stop=False)
                    P.mm(psu[:, 0:128], TOK[:sz, 2, :], TOK[:sz, 0, :], start=False, stop=True)
                    for hh in range(2):
                        pr = slice(hh * 64, hh * 64 + 64)
                        P.tt(SST[pr, c, :], SST[pr, c, :], psu[pr, pr], ALU.add)
                        P.ts(SST[pr, c, :], SST[pr, c, :], EW[pr, 0, sz - 1:sz], ALU.mult)
                    if sub == 4:
                        continue
                    ptt = PA[2]
                    P.tr(ptt[:, 0:sz], OTOK[:sz, :], ident(sz))
                    P.copy(OT[:, :sz], ptt[:, 0:sz])
                    P.tt(T1[:, :sz], OT[:, :sz], OT[:, :sz], ALU.mult)
                    pmn = PB[0]
                    P.mm(pmn[:, 0:sz], BLK, OT[:, :sz])
                    P.mm(pmn[:, 128:128 + sz], BLK, T1[:, :sz])
                    P.ts(T1[:, :sz], pmn[:, 0:sz], 1.0 / 64, ALU.mult)
                    P.tt(T2[:, :sz], T1[:, :sz], T1[:, :sz], ALU.mult)
                    P.stt(T2[:, :sz], pmn[:, 128:128 + sz], 1.0 / 64, T2[:, :sz], ALU.mult, ALU.subtract)
                    P.ts(T2[:, :sz], T2[:, :sz], 64e-5, ALU.add)
                    P.act(T2[:, :sz], T2[:, :sz], AF.Sqrt)
                    P.recip(T2[:, :sz], T2[:, :sz])
                    P.tt(OT[:, :sz], OT[:, :sz], T1[:, :sz], ALU.subtract)
                    P.tt(OT[:, :sz], OT[:, :sz], T2[:, :sz], ALU.mult)
                    P.ts(OT[:, :sz], OT[:, :sz], VR[:, l, 8, c:c + 1], ALU.mult, VR[:, l, 9, c:c + 1], ALU.add)
                    P.tt(OT[:, :sz], OT[:, :sz], BON_, ALU.add)
                    P.tt(yr[:, :sz], OT[:, :sz], G_, ALU.mult)
                    P.dma("sp", YT[c * 128:(c + 1) * 128, o:o + sz], yr[:, :sz], yr)
    def run_all():
        P.no_reset = (cfg.NSEQ == 1)
        prologue()
        P.reset_all()
        def body(s):
            stage_ln0(s)
            for l in range(dep):
                stage_win(l)
                stage_fox(l)
                stage_mlstm(l)
                stage_rwkv(l)
                stage_m1(l)
                stage_m2(l)
                ln_stage(NEWTv, 2 + 4 * l + 0, 2 + 4 * l + 1)
                stage_ffn(l)
                ln_stage(NEWTv, 2 + 4 * l + 2, 2 + 4 * l + 3)
            stage_out(s)
            P.reset_all()
        if cfg.NSEQ == 1:
            body(0)
        else:
            with nc.Fori(0, cfg.NSEQ) as s:
                body(s)
        P.es.close()
    return finish(locals())


def finish(lc):
    P, cfg, dbg = lc["P"], lc["cfg"], lc["dbg"]
    run = lc.get("run_stages")
    return lc


def pack_small(p, dep):
    def fm(v):
        return np.ascontiguousarray(np.asarray(v, np.float32).reshape(KC, 128).T)
    vecD = np.zeros((128, 2 + 4 * dep, KC), np.float32)
    vecD[:, 0] = fm(p["ln_emb_g"])
    vecD[:, 1] = fm(p["ln_emb_b"])
    for l in range(dep):
        vecD[:, 2 + 4 * l + 0] = fm(p["ln1_g"][l])
        vecD[:, 2 + 4 * l + 1] = fm(p["ln1_b"][l])
        vecD[:, 2 + 4 * l + 2] = fm(p["ln2_g"][l])
        vecD[:, 2 + 4 * l + 3] = fm(p["ln2_b"][l])
    vecR = np.zeros((128, dep, 13, 6), np.float32)

    def f6(v):
        return np.asarray(v, np.float32).reshape(6, 128).T
    for l in range(dep):
        mu = np.asarray(p["rwkv_mu"][l], np.float32)
        vecR[:, l, 0] = f6(mu[0:768])
        vecR[:, l, 1] = f6(mu[768:1536])
        vecR[:, l, 2] = f6(mu[1536:2304])
        vecR[:, l, 3] = f6(p["rwkv_w0"][l])
        vecR[:, l, 4] = f6(p["rwkv_a0"][l])
        vecR[:, l, 5] = f6(p["rwkv_k_k"][l])
        vecR[:, l, 6] = f6(p["rwkv_k_a"][l])
        vecR[:, l, 7] = f6(np.asarray(p["rwkv_r_k"][l]).reshape(768))
        vecR[:, l, 8] = f6(p["rwkv_gn_g"][l])
        vecR[:, l, 9] = f6(p["rwkv_gn_b"][l])
        vecR[:96, l, 10, 0] = mu[2304:2400]
        vecR[:96, l, 11, 0] = mu[2400:2496]
        vecR[:, l, 12, 0] = mu[2496:2624]
        vecR[:, l, 12, 1] = mu[2624:2752]
    foxbf = np.ascontiguousarray(np.asarray(p["fox_b_f"], np.float32)[:dep].reshape(dep, 12, 1))
    cw = np.asarray(p["mlstm_conv_w"], np.float32)[:dep]
    convw = np.ascontiguousarray(cw.reshape(dep, 4, 8, 128).transpose(3, 0, 2, 1))
    mlb = np.zeros((dep, 4, 2), np.float32)
    mlb[:, :, 0] = np.asarray(p["mlstm_b_i"], np.float32)[:dep]
    mlb[:, :, 1] = np.asarray(p["mlstm_b_f"], np.float32)[:dep]
    return {"consts": make_consts(), "vecD": vecD, "vecR": vecR, "foxbf": foxbf, "convw": convw, "mlb": mlb}


N_CORES = 4


def kernel(**inputs):
    x = np.asarray(inputs["x"], np.float32)
    B = x.shape[0]
    nseq = B // N_CORES
    cfg = Cfg(NT=16, NSEQ=nseq, depth=4)
    Res.ALL.clear()
    lc = build(cfg)
    lc["run_all"]()
    nc = lc["nc"]
    small = pack_small(inputs, 4)
    shared = dict(small)
    shared["meta"] = np.asarray(inputs["meta_tokens"], np.float32)
    for k in ["w_in", "rwkv_w2", "rwkv_a2", "rwkv_g2", "proj_rwkv", "proj_fox", "proj_mlstm", "w_out", "ffn_w1", "ffn_w3",
              "ffn_w2", "router_w", "moe_w1", "moe_w3", "moe_w2"]:
        shared[k] = np.asarray(inputs[k], np.float32)
    in_maps = []
    for c in range(N_CORES):
        m = dict(shared)
        m["x"] = np.ascontiguousarray(x[c * nseq:(c + 1) * nseq])
        in_maps.append(m)
    res = run_bass_kernel_spmd(nc, in_maps, core_ids=list(range(N_CORES)))
    return np.concatenate([r["out"] for r in res.results], axis=0).astype(np.float32)
```

```python
import numpy as np
from contextlib import ExitStack, contextmanager
import concourse.bass as bass
import concourse.mybir as mybir
from concourse.bass_utils import run_bass_kernel_spmd

F32 = mybir.dt.float32
BF16 = mybir.dt.bfloat16
AF = mybir.ActivationFunctionType
ALU = mybir.AluOpType
AX = mybir.AxisListType

D = 2048
KC = 16
NMETA = 16
RW = 768
RH = 12
FW = 768
MW = 512
MH = 4
C1 = 2752
C2 = C1 + 2316
C3 = C2 + 2056
NIN = C3 + 6144
DFF = 5632
FC = 44
NE = 8
ALPHA = 8 ** 0.25
NDS = 48


class Res:
    __slots__ = ("name", "w", "r", "sem", "base")

    ALL = []

    def __init__(self, name):
        self.name = name
        self.w = {}
        self.r = {}
        self.base = {}
        self.sem = None
        Res.ALL.append(self)


class VA:
    __slots__ = ("ap", "res")

    def __init__(self, ap, res):
        self.ap = ap
        self.res = res

    def __getitem__(self, idx):
        return VA(self.ap[idx], self.res)


class T:
    def __init__(self, t, name):
        self.t = t
        self.res = Res(name)

    def __getitem__(self, idx):
        return VA(self.t[idx], self.res)


class Prog:
    def __init__(self, nc):
        self.nc = nc
        self.es = ExitStack()
        self.eng = {"pe": nc.tensor, "act": nc.scalar, "dve": nc.vector, "pool": nc.gpsimd, "sp": nc.sync}
        self.esem = {k: self.es.enter_context(nc.semaphore(f"e_{k}")) for k in self.eng}
        self.ecnt = {k: 0 for k in self.eng}
        self.dsems = [self.es.enter_context(nc.semaphore(f"d{i}")) for i in range(NDS)]
        self.dcnt = [0] * NDS
        self.dnext = 0
        self.obs = {k: {} for k in self.eng}
        self.ninstr = 0

    def semof(self, key):
        return self.esem[key[1]] if key[0] == "e" else self.dsems[key[1]]

    def _waits(self, eng, reads, writes, disjoint):
        waits = {}
        for r in reads:
            for k, v in r.w.items():
                if waits.get(k, 0) < v:
                    waits[k] = v
        for w in writes:
            for k, v in w.r.items():
                if waits.get(k, 0) < v:
                    waits[k] = v
            for k, v in (w.base if disjoint else w.w).items():
                if waits.get(k, 0) < v:
                    waits[k] = v
        ob = self.obs[eng]
        e = self.eng[eng]
        for k, v in waits.items():
            if eng == "pe" and k == ("e", "pe"):
                continue
            if ob.get(k, 0) >= v:
                continue
            e.wait_ge(self.semof(k), v)
            ob[k] = v
            self.ninstr += 1

    def _record(self, key, val, reads, writes, disjoint):
        for r in reads:
            if r.r.get(key, 0) < val:
                r.r[key] = val
        for w in writes:
            if not disjoint:
                w.r = {}
                w.w = {key: val}
                w.base = {key: val}
            else:
                if w.w.get(key, 0) < val:
                    w.w[key] = val

    def op(self, eng, fn, reads, writes, disjoint=False):
        reads = [x.res for x in reads if x is not None]
        writes = [x.res for x in writes]
        self._waits(eng, reads, writes, disjoint)
        ins = fn(self.eng[eng])
        self.ecnt[eng] += 1
        ins.then_inc(self.esem[eng], 1)
        self.ninstr += 1
        self._record(("e", eng), self.ecnt[eng], reads, writes, disjoint)

    def dma(self, q, out, in_, tile, disjoint=True):
        res = tile.res
        if res.sem is None:
            res.sem = self.dnext % NDS
            self.dnext += 1
        idx = res.sem
        reads = [in_.res]
        writes = [out.res]
        self._waits(q, reads, writes, disjoint)
        ins = self.eng[q].dma_start(out=out.ap, in_=in_.ap)
        self.dcnt[idx] += 1
        ins.then_inc(self.dsems[idx], 16)
        self.ninstr += 1
        self._record(("d", idx), 16 * self.dcnt[idx], reads, writes, disjoint)

    def barrier(self):
        ev = {("e", k): v for k, v in self.ecnt.items() if v > 0}
        for i in range(NDS):
            if self.dcnt[i] > 0:
                ev[("d", i)] = 16 * self.dcnt[i]
        for eng, e in self.eng.items():
            ob = self.obs[eng]
            for k, v in ev.items():
                if ob.get(k, 0) >= v:
                    continue
                e.wait_ge(self.semof(k), v)
                ob[k] = v
                self.ninstr += 1

    def reset_all(self):
        nc = self.nc
        self.barrier()
        if getattr(self, "no_reset", False):
            return
        if not hasattr(self, "bsem"):
            self.bsem = [self.es.enter_context(nc.semaphore(f"bar{i}")) for i in range(4)]
        A, C, B, Dd = self.bsem
        order = ["pe", "act", "dve", "pool", "sp"]
        for k in order:
            e = self.eng[k]
            e.sem_inc(A, 1)
            e.wait_ge(A, 5)
            e.sem_inc(C, 1)
        pe = self.eng["pe"]
        pe.wait_ge(C, 5)
        for s_ in list(self.esem.values()) + list(self.dsems):
            pe.sem_clear(s_)
        pe.sem_clear(A)
        pe.sem_clear(C)
        pe.sem_inc(B, 1)
        for k in order[1:]:
            e = self.eng[k]
            e.wait_ge(B, 1)
            e.sem_inc(Dd, 1)
        pe.wait_ge(Dd, 4)
        pe.sem_clear(B)
        pe.sem_clear(Dd)
        self.ecnt = {k: 0 for k in self.eng}
        self.dcnt = [0] * NDS
        self.obs = {k: {} for k in self.eng}
        for r in Res.ALL:
            r.w = {}
            r.r = {}
            r.base = {}

    @contextmanager
    def stage(self):
        st = Stage(self)
        try:
            yield st
        finally:
            self.barrier()
            st.es.close()

    def dram(self, name, shape, dt):
        return T(self.nc.dram_tensor(name, list(shape), dt, kind="Internal"), name)

    def mm(self, out, lhsT, rhs, start=True, stop=True):
        self.op("pe", lambda e: e.matmul(out.ap, lhsT.ap, rhs.ap, start=start, stop=stop),
                [lhsT, rhs], [out], disjoint=True)

    def tr(self, out, in_, ident):
        self.op("pe", lambda e: e.transpose(out.ap, in_.ap, ident.ap), [in_, ident], [out], disjoint=True)

    def act(self, out, in_, func, bias=None, scale=1.0, eng="act"):
        def f(e):
            kw = {}
            if bias is not None:
                kw["bias"] = bias.ap if isinstance(bias, VA) else bias
            return e.activation(out=out.ap, in_=in_.ap, func=func, scale=scale, **kw)
        self.op("act", f, [in_, bias if isinstance(bias, VA) else None], [out], disjoint=True)

    def tt(self, out, a, b, op, eng="dve"):
        self.op(eng, lambda e: e.tensor_tensor(out=out.ap, in0=a.ap, in1=b.ap, op=op), [a, b], [out], disjoint=True)

    def ts(self, out, a, s1, op0, s2=None, op1=None, eng="dve"):
        def f(e):
            a1 = s1.ap if isinstance(s1, VA) else s1
            if s2 is None:
                return e.tensor_scalar(out=out.ap, in0=a.ap, scalar1=a1, scalar2=None, op0=op0)
            a2 = s2.ap if isinstance(s2, VA) else s2
            return e.tensor_scalar(out=out.ap, in0=a.ap, scalar1=a1, scalar2=a2, op0=op0, op1=op1)
        self.op(eng, f, [a, s1 if isinstance(s1, VA) else None, s2 if isinstance(s2, VA) else None], [out],
                disjoint=True)

    def stt(self, out, a, s, b, op0, op1):
        def f(e):
            sc = s.ap if isinstance(s, VA) else s
            return e.scalar_tensor_tensor(out=out.ap, in0=a.ap, scalar=sc, in1=b.ap, op0=op0, op1=op1)
        self.op("dve", f, [a, b, s if isinstance(s, VA) else None], [out], disjoint=True)

    def copy(self, out, in_, eng="dve"):
        if eng == "act":
            self.act(out, in_, AF.Copy)
        else:
            self.op(eng, lambda e: e.tensor_copy(out=out.ap, in_=in_.ap), [in_], [out], disjoint=True)

    def recip(self, out, in_):
        self.op("dve", lambda e: e.reciprocal(out=out.ap, in_=in_.ap), [in_], [out], disjoint=True)

    def memset(self, out, val, eng="dve"):
        self.op(eng, lambda e: e.memset(out.ap, val), [], [out], disjoint=False)

    def scan(self, out, d0, d1, init, op0, op1):
        self.op("dve", lambda e: e.tensor_tensor_scan(out=out.ap, data0=d0.ap, data1=d1.ap, initial=init,
                                                      op0=op0, op1=op1), [d0, d1], [out], disjoint=True)


class Stage:
    def __init__(self, p):
        self.p = p
        self.es = ExitStack()
        self.n = 0

    def tile(self, shape, dt, name=None):
        self.n += 1
        name = name or f"t{self.n}"
        self.p.uid = getattr(self.p, "uid", 0) + 1
        nm = f"{name}_{self.p.uid}"
        return T(self.es.enter_context(self.p.nc.sbuf_tensor(nm, list(shape), dt)), nm)

    def psum(self, shape=(128, 512), dt=F32, name=None):
        self.n += 1
        self.p.uid = getattr(self.p, "uid", 0) + 1
        nm = f"ps_{self.p.uid}"
        return T(self.es.enter_context(self.p.nc.psum_tensor(nm, list(shape), dt)), nm)


CO_ID = 0
CO_MEAN = 128
CO_BLK = 256
CO_MLE = 384
CO_MLT = 512
CO_MGE = 640
CO_ONE = 768
CO_SEL = 896
NCONST = CO_SEL + 12 * 128


def make_consts():
    c = np.zeros((128, NCONST), np.float32)
    i = np.arange(128)
    c[:, CO_ID:CO_ID + 128] = np.eye(128)
    c[:, CO_MEAN:CO_MEAN + 128] = 1.0 / D
    c[:64, CO_BLK:CO_BLK + 64] = 1.0
    c[64:, CO_BLK + 64:CO_BLK + 128] = 1.0
    c[:, CO_MLE:CO_MLE + 128] = (i[:, None] <= i[None, :])
    c[:, CO_MLT:CO_MLT + 128] = (i[:, None] < i[None, :])
    c[:, CO_MGE:CO_MGE + 128] = (i[:, None] > i[None, :])
    c[:, CO_ONE:CO_ONE + 128] = 1.0
    for h in range(12):
        c[h, CO_SEL + h * 128:CO_SEL + (h + 1) * 128] = 1.0
    return c


class TT(T):
    def view(self, pat, **kw):
        v = TT.__new__(TT)
        v.t = self.t.rearrange(pat, **kw)
        v.res = self.res
        return v


def va_re(va, pat, **kw):
    return VA(va.ap.rearrange(pat, **kw), va.res)


class Cfg:
    def __init__(self, NT=16, NSEQ=1, depth=4, debug=False):
        self.NT = NT
        self.NSEQ = NSEQ
        self.depth = depth
        self.L = NMETA + 128 * NT
        self.S = 128 * NT
        self.tiles = [(0, NMETA)] + [(NMETA + 128 * i, 128) for i in range(NT)]
        self.sbs = [(o, min(512, self.L - o)) for o in range(0, self.L, 512)]
        self.n_dense = (depth + 1) // 2
        self.n_moe = depth // 2
        self.debug = debug


def col_chunks():
    segs = [0, 768, 1536, 2304, 2400, 2496, C1, C1 + 768, C1 + 1536, C1 + 2304, C2, C2 + 512, C2 + 1024,
            C2 + 1536, C2 + 2048, C3, C3 + 2048, C3 + 4096, NIN]
    chunks = []
    for a, b in zip(segs[:-1], segs[1:]):
        c = a
        while c < b:
            m = min(128, b - c)
            chunks.append((c, m))
            c += m
    blocks = []
    cur = []
    for ch in chunks:
        if cur and (ch[0] + ch[1] - cur[0][0]) > 512:
            blocks.append(cur)
            cur = []
        cur.append(ch)
    blocks.append(cur)
    return blocks


def build(cfg):
    nc = bass.Bass("TRN2", target_bir_lowering=False)
    P = Prog(nc)
    L, NT, dep = cfg.L, cfg.NT, cfg.depth
    NTl = NT + 1

    def ext(name, shape, dt=F32):
        t = TT.__new__(TT)
        t.t = nc.dram_tensor(name, list(shape), dt, kind="ExternalInput").ap()
        t.res = Res(name)
        return t

    def scr(name, shape, dt=F32):
        t = TT.__new__(TT)
        t.t = nc.dram_tensor(name, list(shape), dt, kind="Internal").ap()
        t.res = Res(name)
        return t

    x_in = ext("x", [cfg.NSEQ, cfg.S, D])
    meta = ext("meta", [NMETA, D])
    consts = ext("consts", [128, NCONST])
    vecD = ext("vecD", [128, 2 + 4 * dep, KC])
    vecR = ext("vecR", [128, dep, 13, 6])
    foxbf = ext("foxbf", [dep, 12, 1])
    convw = ext("convw", [128, dep, 8, 4])
    mlb = ext("mlb", [dep, 4, 2])
    w_in = ext("w_in", [dep, D, NIN])
    rw2 = ext("rwkv_w2", [dep, 96, RW])
    ra2 = ext("rwkv_a2", [dep, 96, RW])
    rg2 = ext("rwkv_g2", [dep, 256, RW])
    p_r = ext("proj_rwkv", [dep, RW, D])
    p_f = ext("proj_fox", [dep, FW, D])
    p_m = ext("proj_mlstm", [dep, MW, D])
    w_o = ext("w_out", [dep, D, D])
    f_w1 = ext("ffn_w1", [cfg.n_dense, D, DFF])
    f_w3 = ext("ffn_w3", [cfg.n_dense, D, DFF])
    f_w2 = ext("ffn_w2", [cfg.n_dense, DFF, D])
    if cfg.n_moe:
        r_w = ext("router_w", [cfg.n_moe, D, NE])
        m_w1 = ext("moe_w1", [cfg.n_moe, NE, D, DFF])
        m_w3 = ext("moe_w3", [cfg.n_moe, NE, D, DFF])
        m_w2 = ext("moe_w2", [cfg.n_moe, NE, DFF, D])
    out_t = TT.__new__(TT)
    out_t.t = nc.dram_tensor("out", [cfg.NSEQ, cfg.S, D], F32, kind="ExternalOutput").ap()
    out_t.res = Res("out")
    dbg = {}
    if cfg.debug:
        for nm, shp in [("d_ht", [D, L]), ("d_zt", [NIN, L]), ("d_yt", [D, L]), ("d_new", [D, L])]:
            t = TT.__new__(TT)
            t.t = nc.dram_tensor(nm, shp, F32, kind="ExternalOutput").ap()
            t.res = Res(nm)
            dbg[nm] = t

    HT32 = scr("HT32", [D, L])
    HTb = scr("HTb", [D, L], BF16)
    ZT = scr("ZT", [NIN, L])
    YT = scr("YT", [D, L], BF16)
    MIXT = scr("MIXT", [D, L], BF16)
    NEWT = scr("NEWT", [D, L])
    RWS = scr("RWS", [8, RW, L])
    HT32v = HT32.view("(kc p) l -> p kc l", p=128)
    HTbv = HTb.view("(kc p) l -> p kc l", p=128)
    YTv = YT.view("(kc p) l -> p kc l", p=128)
    MIXTv = MIXT.view("(kc p) l -> p kc l", p=128)
    NEWTv = NEWT.view("(kc p) l -> p kc l", p=128)

    ges = P.es

    def gtile(name, shape, dt):
        return TT_from(ges.enter_context(nc.sbuf_tensor(name, list(shape), dt)), name)

    def TT_from(t, name):
        o = TT.__new__(TT)
        o.t = t
        o.res = Res(name)
        return o

    CON = gtile("CON", [128, NCONST], F32)
    CONB = gtile("CONB", [128, 896], BF16)
    VD = gtile("VD", [128, 2 + 4 * dep, KC], F32)
    VR = gtile("VR", [128, dep, 13, 6], F32)
    P.dma("sp", CON[:, :], consts[:, :], CON)
    P.dma("sp", VD[:, :, :], vecD[:, :, :], VD)
    P.dma("sp", VR[:, :, :, :], vecR[:, :, :, :], VR)
    P.copy(CONB[:, :], CON[:, 0:896])
    ID = CON[:, CO_ID:CO_ID + 128]

    def ident(n):
        return CON[:n, CO_ID:CO_ID + n]

    STG_N = 2816

    def mk_stg(st, n=3):
        st.stg = [st.tile([128, STG_N], F32) for _ in range(n)]
        st.stg_i = 0

    def wload(st, dst, src, np_, a, b, eng="pool", engs=None):
        step = max(1, STG_N // b)
        a0 = 0
        while a0 < a:
            a1 = min(a, a0 + step)
            stg = st.stg[st.stg_i % len(st.stg)]
            st.stg_i += 1
            view = VA(stg.t[:np_, 0:(a1 - a0) * b].rearrange("p (a b) -> p a b", a=a1 - a0), stg.res)
            P.dma("sp", view, src[:, a0:a1, :], stg, disjoint=False)
            P.copy(dst[:, a0:a1, :], view, eng=(engs[st.stg_i % len(engs)] if engs else eng))
            a0 = a1

    def pipelined(n, load, compute, depth=1):
        for i in range(min(depth, n)):
            load(i)
        for i in range(n):
            if i + depth < n:
                load(i + depth)
            compute(i)

    blocks = col_chunks()
    cache = {}
    NB_WIN = len(blocks)
    n_ffn = cfg.n_dense + cfg.n_moe * NE

    def cfam(name, nslots, elems):
        per = max(1, (96 << 20) // (128 * elems * 2))
        ts = [scr(f"C_{name}_{g}", [min(per, nslots - g * per), 128, elems], BF16)
              for g in range((nslots + per - 1) // per)]
        cache[name] = (per, ts)

    def cview(fam, slot, a, b):
        per, ts = cache[fam]
        t = ts[slot // per]
        return VA(t.t[slot % per, :, 0:a * b].rearrange("p (a b) -> p a b", a=a), t.res)

    cfam("win", dep * NB_WIN, KC * 512)
    cfam("pr", dep, 6 * D)
    cfam("pf", dep, 6 * D)
    cfam("pm", dep, 4 * D)
    cfam("wo", dep * 4, KC * 512)
    cfam("w1", n_ffn * 22, KC * 256)
    cfam("w3", n_ffn * 22, KC * 256)
    cfam("w2", n_ffn * 16, FC * 128)

    def ffn_idx(l, e_):
        return (l // 2) if l % 2 == 0 else cfg.n_dense + (l // 2) * NE + e_

    def prologue():
        with P.stage() as st:
            mk_stg(st, 4)
            WT = [st.tile([128, 6 * D], BF16) for _ in range(2)]
            k = [0]
            engs = ["pool", "act", "dve"]

            def fill(fam, slot, srcv, a, b):
                wt = WT[k[0] % 2]
                k[0] += 1
                dst = VA(wt.t[:, 0:a * b].rearrange("p (a b) -> p a b", a=a), wt.res)
                wload(st, dst, srcv, 128, a, b, engs=engs)
                P.dma("sp", cview(fam, slot, a, b), dst, wt)
            wv = w_in.view("l (kc p) n -> l p kc n", p=128)
            prv = p_r.view("l (kc p) n -> l p kc n", p=128)
            pfv = p_f.view("l (kc p) n -> l p kc n", p=128)
            pmv = p_m.view("l (kc p) n -> l p kc n", p=128)
            wov = w_o.view("l (kc p) n -> l p kc n", p=128)
            for l in range(dep):
                for bi_, blk in enumerate(blocks):
                    c0 = blk[0][0]
                    cw = blk[-1][0] + blk[-1][1] - c0
                    fill("win", l * NB_WIN + bi_, wv[l, :, :, c0:c0 + cw], KC, cw)
                fill("pr", l, prv[l], 6, D)
                fill("pf", l, pfv[l], 6, D)
                fill("pm", l, pmv[l], 4, D)
                for cb in range(4):
                    fill("wo", l * 4 + cb, wov[l, :, :, cb * 512:(cb + 1) * 512], KC, 512)
                moe = (l % 2 == 1)
                li = l // 2
                for e_ in range(NE if moe else 1):
                    if moe:
                        w1v = m_w1.view("l e (kc p) n -> l e p kc n", p=128)[li, e_]
                        w3v = m_w3.view("l e (kc p) n -> l e p kc n", p=128)[li, e_]
                        w2v = m_w2.view("l e (kc p) n -> l e p kc n", p=128)[li, e_]
                    else:
                        w1v = f_w1.view("l (kc p) n -> l p kc n", p=128)[li]
                        w3v = f_w3.view("l (kc p) n -> l p kc n", p=128)[li]
                        w2v = f_w2.view("l (kc p) n -> l p kc n", p=128)[li]
                    fi = ffn_idx(l, e_)
                    for cb in range(22):
                        fill("w1", fi * 22 + cb, w1v[:, :, cb * 256:(cb + 1) * 256], KC, 256)
                        fill("w3", fi * 22 + cb, w3v[:, :, cb * 256:(cb + 1) * 256], KC, 256)
                    for m in range(16):
                        fill("w2", fi * 16 + m, w2v[:, :, m * 128:(m + 1) * 128], FC, 128)

    def ln_stage(src_v, gi, bi, also_dbg=None):
        with P.stage() as st:
            NEW = [st.tile([128, KC, 512], F32) for _ in range(2)]
            SQ = st.tile([128, KC, 512], F32)
            OB = [st.tile([128, KC, 512], BF16) for _ in range(2)]
            mean = st.tile([128, 512], F32)
            rstd = st.tile([128, 512], F32)
            psm = st.psum()
            psq = st.psum()
            for bi_, (o, n) in enumerate(cfg.sbs):
                nw = NEW[bi_ % 2]
                ob = OB[bi_ % 2]
                P.dma("sp", nw[:, :, :n], src_v[:, :, o:o + n], nw)
                ln_core(st, nw, SQ, ob, mean, rstd, psm, psq, n, gi, bi)
                P.dma("sp", HT32v[:, :, o:o + n], nw[:, :, :n], nw)
                P.dma("sp", HTbv[:, :, o:o + n], ob[:, :, :n], ob)

    def ln_core(st, nw, SQ, ob, mean, rstd, psm, psq, n, gi, bi):
        MEANM = CON[:, CO_MEAN:CO_MEAN + 128]
        P.act(SQ[:, :, :n], nw[:, :, :n], AF.Square)
        for kc in range(KC):
            P.mm(psm[:, :n], MEANM, nw[:, kc, :n], start=(kc == 0), stop=(kc == KC - 1))
        for kc in range(KC):
            P.mm(psq[:, :n], MEANM, SQ[:, kc, :n], start=(kc == 0), stop=(kc == KC - 1))
        P.copy(mean[:, :n], psm[:, :n])
        P.tt(rstd[:, :n], mean[:, :n], mean[:, :n], ALU.mult)
        P.tt(rstd[:, :n], psq[:, :n], rstd[:, :n], ALU.subtract)
        P.ts(rstd[:, :n], rstd[:, :n], 1e-5, ALU.add)
        P.act(rstd[:, :n], rstd[:, :n], AF.Sqrt)
        P.recip(rstd[:, :n], rstd[:, :n])
        for kc in range(KC):
            P.tt(SQ[:, kc, :n], nw[:, kc, :n], mean[:, :n], ALU.subtract)
            P.tt(SQ[:, kc, :n], SQ[:, kc, :n], rstd[:, :n], ALU.mult)
            P.ts(nw[:, kc, :n], SQ[:, kc, :n], VD[:, gi, kc:kc + 1], ALU.mult, VD[:, bi, kc:kc + 1], ALU.add)
            P.copy(ob[:, kc, :n], nw[:, kc, :n], eng="pool")

    def stage_ln0(s):
        with P.stage() as st:
            XT = [st.tile([128, D], F32) for _ in range(2)]
            NEW = st.tile([128, KC, 128], F32)
            SQ = st.tile([128, KC, 128], F32)
            OB = st.tile([128, KC, 128], BF16)
            mean = st.tile([128, 128], F32)
            rstd = st.tile([128, 128], F32)
            pst = [st.psum() for _ in range(2)]
            psm = st.psum()
            psq = st.psum()
            for ti, (o, sz) in enumerate(cfg.tiles):
                xt = XT[ti % 2]
                if ti == 0:
                    P.dma("sp", xt[:sz, :], meta[:, :], xt)
                else:
                    P.dma("sp", xt[:sz, :], x_in[s, o - NMETA:o - NMETA + sz, :], xt)
                for g in range(4):
                    ps = pst[g % 2]
                    for j in range(4):
                        kc = g * 4 + j
                        P.tr(ps[:, j * 128:j * 128 + sz], xt[:sz, kc * 128:(kc + 1) * 128], ident(sz))
                    P.copy(NEW[:, g * 4:(g + 1) * 4, :sz],
                           va_re(ps[:, :], "p (a b) -> p a b", a=4)[:, :, :sz] if False else
                           VA(ps.t[:, :].rearrange("p (a b) -> p a b", a=4)[:, :, :sz], ps.res))
                ln_core(st, NEW, SQ, OB, mean, rstd, psm, psq, sz, 0, 1)
                P.dma("sp", HT32v[:, :, o:o + sz], NEW[:, :, :sz], NEW)
                P.dma("sp", HTbv[:, :, o:o + sz], OB[:, :, :sz], OB)

    blocks = col_chunks()

    def stage_win(l):
        wv = w_in.view("l (kc p) n -> l p kc n", p=128)
        with P.stage() as st:
            mk_stg(st, 3)
            XT = [st.tile([128, KC, 512], BF16) for _ in range(2)]
            W = [st.tile([128, KC, 512], BF16) for _ in range(3)]
            ZS = [st.tile([128, 512], F32) for _ in range(4)]
            PS = [st.psum() for _ in range(4)]
            jobs = [(bi_, o, n, blk, bj) for bi_, (o, n) in enumerate(cfg.sbs) for bj, blk in enumerate(blocks)]
            cnt = [0]

            def load(i):
                bi_, o, n, blk, bj = jobs[i]
                if bj == 0:
                    xt = XT[bi_ % 2]
                    P.dma("sp", xt[:, :, :n], HTbv[:, :, o:o + n], xt)
                c0 = blk[0][0]
                cw = blk[-1][0] + blk[-1][1] - c0
                w = W[i % 3]
                P.dma("sp", w[:, :, :cw], cview("win", l * NB_WIN + bj, KC, cw), w, disjoint=False)

            def compute(i):
                bi_, o, n, blk, bj = jobs[i]
                xt = XT[bi_ % 2]
                w = W[i % 3]
                c0 = blk[0][0]
                for (c, m) in blk:
                    ps = PS[cnt[0] % 4]
                    zs = ZS[cnt[0] % 4]
                    cnt[0] += 1
                    for kc in range(KC):
                        P.mm(ps[:m, :n], w[:, kc, c - c0:c - c0 + m], xt[:, kc, :n], start=(kc == 0),
                             stop=(kc == KC - 1))
                    P.act(zs[:m, :n], ps[:m, :n], AF.Sigmoid if c >= C3 else AF.Copy)
                    P.dma("sp", ZT[c:c + m, o:o + n], zs[:m, :n], zs)
            pipelined(len(jobs), load, compute, depth=2)

    def stage_m1(l):
        prv = p_r.view("l (kc p) n -> l p kc n", p=128)
        pfv = p_f.view("l (kc p) n -> l p kc n", p=128)
        pmv = p_m.view("l (kc p) n -> l p kc n", p=128)
        with P.stage() as st:
            PR = st.tile([128, 6, D], BF16)
            PF = st.tile([128, 6, D], BF16)
            PM = st.tile([128, 4, D], BF16)
            P.dma("sp", PR[:, :, :], cview("pr", l, 6, D), PR)
            P.dma("sp", PF[:, :, :], cview("pf", l, 6, D), PF)
            P.dma("sp", PM[:, :, :], cview("pm", l, 4, D), PM)
            Y = [st.tile([128, KC, 512], BF16) for _ in range(2)]
            G = [[st.tile([128, 512], F32) for _ in range(3)] for _ in range(2)]
            A1 = [st.tile([128, 512], F32) for _ in range(2)]
            A2 = [st.tile([128, 512], F32) for _ in range(2)]
            MO = [st.tile([128, 512], BF16) for _ in range(2)]
            PS = [[st.psum() for _ in range(3)] for _ in range(2)]
            it = 0
            for bi_, (o, n) in enumerate(cfg.sbs):
                y = Y[bi_ % 2]
                P.dma("sp", y[:, :, :n], YTv[:, :, o:o + n], y)
                for m in range(KC):
                    g3 = G[it % 2]
                    ps3 = PS[it % 2]
                    a1, a2, mo = A1[it % 2], A2[it % 2], MO[it % 2]
                    it += 1
                    for gi in range(3):
                        r0 = C3 + gi * D + m * 128
                        P.dma("sp", g3[gi][:, :n], ZT[r0:r0 + 128, o:o + n], g3[gi])
                    for kc in range(6):
                        P.mm(ps3[0][:, :n], PR[:, kc, m * 128:(m + 1) * 128], y[:, kc, :n], start=(kc == 0), stop=(kc == 5))
                    for kc in range(6):
                        P.mm(ps3[1][:, :n], PF[:, kc, m * 128:(m + 1) * 128], y[:, 6 + kc, :n], start=(kc == 0), stop=(kc == 5))
                    for kc in range(4):
                        P.mm(ps3[2][:, :n], PM[:, kc, m * 128:(m + 1) * 128], y[:, 12 + kc, :n], start=(kc == 0), stop=(kc == 3))
                    P.tt(a1[:, :n], ps3[0][:, :n], g3[0][:, :n], ALU.mult)
                    P.tt(a2[:, :n], ps3[1][:, :n], g3[1][:, :n], ALU.mult)
                    P.tt(a1[:, :n], a1[:, :n], a2[:, :n], ALU.add)
                    P.tt(a2[:, :n], ps3[2][:, :n], g3[2][:, :n], ALU.mult)
                    P.tt(mo[:, :n], a1[:, :n], a2[:, :n], ALU.add)
                    P.dma("sp", MIXT[m * 128:(m + 1) * 128, o:o + n], mo[:, :n], mo)

    def stage_proj_res(xv, nkc, wview, WB=512):
        with P.stage() as st:
            mk_stg(st, 3)
            XT = [st.tile([128, nkc, 512], BF16) for _ in range(2)]
            W = [st.tile([128, nkc, WB], BF16) for _ in range(2)]
            HR = [st.tile([128, 512], F32) for _ in range(3)]
            PS = [st.psum() for _ in range(3)]
            jobs = [(bi_, o, n, cb) for bi_, (o, n) in enumerate(cfg.sbs) for cb in range(D // WB)]
            it = [0]

            def load(i):
                bi_, o, n, cb = jobs[i]
                if cb == 0:
                    xt = XT[bi_ % 2]
                    P.dma("sp", xt[:, :, :n], xv[:, :, o:o + n], xt)
                w = W[i % 2]
                P.dma("sp", w[:, :, :], wview(cb), w, disjoint=False)

            def compute(i):
                bi_, o, n, cb = jobs[i]
                xt = XT[bi_ % 2]
                w = W[i % 2]
                for mm_ in range(WB // 128):
                    m = cb * (WB // 128) + mm_
                    ps = PS[it[0] % 3]
                    hr = HR[it[0] % 3]
                    it[0] += 1
                    P.dma("sp", hr[:, :n], HT32[m * 128:(m + 1) * 128, o:o + n], hr)
                    for kc in range(nkc):
                        P.mm(ps[:, :n], w[:, kc, mm_ * 128:(mm_ + 1) * 128], xt[:, kc, :n], start=(kc == 0),
                             stop=(kc == nkc - 1))
                    P.stt(hr[:, :n], hr[:, :n], ALPHA, ps[:, :n], ALU.mult, ALU.add)
                    P.dma("sp", NEWT[m * 128:(m + 1) * 128, o:o + n], hr[:, :n], hr)
            pipelined(len(jobs), load, compute, depth=1)

    def stage_m2(l):
        stage_proj_res(MIXTv, KC, lambda cb: cview("wo", l * 4 + cb, KC, 512))

    def stage_ffn(l):
        moe = (l % 2 == 1)
        li = l // 2
        nexp = NE if moe else 1
        with P.stage() as st:
            XT = st.tile([128, KC, 512], BF16)
            HID = st.tile([128, FC, 512], BF16)
            W1 = [st.tile([128, KC, 256], BF16) for _ in range(2)]
            W3 = [st.tile([128, KC, 256], BF16) for _ in range(2)]
            W2 = [st.tile([128, FC, 128], BF16) for _ in range(2)]
            SIL = [st.tile([128, 512], F32) for _ in range(2)]
            HR = [st.tile([128, 512], F32) for _ in range(2)]
            PA = [st.psum() for _ in range(2)]
            PB = [st.psum() for _ in range(2)]
            PO = [st.psum() for _ in range(2)]
            mk_stg(st, 2)
            if moe:
                ACC = st.tile([128, KC, 512], F32)
                GBC = st.tile([128, NE, 512], BF16)
                X32 = st.tile([128, KC, 128], F32)
                RWT = st.tile([128, KC, NE], F32)
                LG = st.tile([128, 8], F32)
                MX = st.tile([128, 8], F32)
                EX = st.tile([128, 8], F32)
                MK = st.tile([128, 8], F32)
                SC = st.tile([128, 4], F32)
                DG = st.tile([128, 128], F32)
                PR_ = st.psum()
                P.dma("sp", RWT[:, :, :], r_w.view("l (kc p) e -> l p kc e", p=128)[li], RWT)
            it = 0
            wi = 0
            w2i = 0
            for bi_, (o, n) in enumerate(cfg.sbs):
                P.dma("sp", XT[:, :, :n], HTbv[:, :, o:o + n], XT)
                if moe:
                    for t0 in range(0, n, 128):
                        tn = min(128, n - t0)
                        P.dma("sp", X32[:, :, :tn], HT32v[:, :, o + t0:o + t0 + tn], X32)
                        for kc in range(KC):
                            P.mm(PR_[:tn, 0:8], X32[:, kc, :tn], RWT[:, kc, :], start=(kc == 0), stop=(kc == KC - 1))
                        P.copy(LG[:tn, :], PR_[:tn, 0:8])
                        P.op("dve", lambda e: e.max(out=MX.t[:tn, :], in_=LG.t[:tn, :]), [LG], [MX], disjoint=True)
                        P.ts(MK[:tn, :], LG[:tn, :], MX[:tn, 1:2], ALU.is_ge)
                        P.ts(SC[:tn, 0:1], MX[:tn, 0:1], -1.0, ALU.mult)
                        P.act(EX[:tn, :], LG[:tn, :], AF.Exp, bias=SC[:tn, 0:1])
                        P.tt(EX[:tn, :], EX[:tn, :], MK[:tn, :], ALU.mult)
                        P.op("dve", lambda e: e.reduce_sum(out=SC.t[:tn, 1:2], in_=EX.t[:tn, :], axis=AX.X), [EX], [SC],
                             disjoint=True)
                        P.recip(SC[:tn, 2:3], SC[:tn, 1:2])
                        P.ts(EX[:tn, :], EX[:tn, :], SC[:tn, 2:3], ALU.mult)
                        for e_ in range(NE):
                            P.ts(DG[:tn, :tn], CON[:tn, CO_ID:CO_ID + tn], EX[:tn, e_:e_ + 1], ALU.mult)
                            P.mm(PR_[:, 128:128 + tn], CON[:tn, CO_ONE:CO_ONE + 128], DG[:tn, :tn])
                            P.copy(GBC[:, e_, t0:t0 + tn], PR_[:, 128:128 + tn])
                for e_ in range(nexp):
                    if moe:
                        w1v = m_w1.view("l e (kc p) n -> l e p kc n", p=128)[li, e_]
                        w3v = m_w3.view("l e (kc p) n -> l e p kc n", p=128)[li, e_]
                        w2v = m_w2.view("l e (kc p) n -> l e p kc n", p=128)[li, e_]
                    else:
                        w1v = f_w1.view("l (kc p) n -> l p kc n", p=128)[li]
                        w3v = f_w3.view("l (kc p) n -> l p kc n", p=128)[li]
                        w2v = f_w2.view("l (kc p) n -> l p kc n", p=128)[li]
                    for cb in range(DFF // 256):
                        w1 = W1[wi % 2]
                        w3 = W3[wi % 2]
                        wi += 1
                        P.dma("sp", w1[:, :, :], cview("w1", ffn_idx(l, e_) * 22 + cb, KC, 256), w1, disjoint=False)
                        P.dma("sp", w3[:, :, :], cview("w3", ffn_idx(l, e_) * 22 + cb, KC, 256), w3, disjoint=False)
                        for j in range(2):
                            f = cb * 2 + j
                            pa, pb, sil = PA[it % 2], PB[it % 2], SIL[it % 2]
                            it += 1
                            for kc in range(KC):
                                P.mm(pa[:, :n], w1[:, kc, j * 128:(j + 1) * 128], XT[:, kc, :n], start=(kc == 0), stop=(kc == KC - 1))
                            for kc in range(KC):
                                P.mm(pb[:, :n], w3[:, kc, j * 128:(j + 1) * 128], XT[:, kc, :n], start=(kc == 0), stop=(kc == KC - 1))
                            P.act(sil[:, :n], pa[:, :n], AF.Silu)
                            if moe:
                                P.tt(sil[:, :n], sil[:, :n], pb[:, :n], ALU.mult)
                                P.tt(HID[:, f, :n], sil[:, :n], GBC[:, e_, :n], ALU.mult)
                            else:
                                P.tt(HID[:, f, :n], sil[:, :n], pb[:, :n], ALU.mult)
                    for m in range(KC):
                        w2 = W2[w2i % 2]
                        po = PO[w2i % 2]
                        hr = HR[w2i % 2]
                        w2i += 1
                        P.dma("sp", w2[:, :, :], cview("w2", ffn_idx(l, e_) * 16 + m, FC, 128), w2, disjoint=False)
                        for f in range(FC):
                            P.mm(po[:, :n], w2[:, f, :], HID[:, f, :n], start=(f == 0), stop=(f == FC - 1))
                        if moe and e_ > 0:
                            P.tt(ACC[:, m, :n], ACC[:, m, :n], po[:, :n], ALU.add)
                        elif moe:
                            P.copy(ACC[:, m, :n], po[:, :n])
                        if (not moe) or e_ == nexp - 1:
                            P.dma("sp", hr[:, :n], HT32[m * 128:(m + 1) * 128, o:o + n], hr)
                            src_ = ACC[:, m, :n] if moe else po[:, :n]
                            P.stt(hr[:, :n], hr[:, :n], ALPHA, src_, ALU.mult, ALU.add)
                            P.dma("sp", NEWT[m * 128:(m + 1) * 128, o:o + n], hr[:, :n], hr)

    def stage_out(s):
        with P.stage() as st:
            HTt = [st.tile([128, KC, 128], F32) for _ in range(2)]
            OT = [st.tile([128, D], F32) for _ in range(2)]
            PS = [st.psum() for _ in range(2)]
            for ti, (o, sz) in enumerate(cfg.tiles[1:]):
                ht = HTt[ti % 2]
                ot = OT[ti % 2]
                P.dma("sp", ht[:, :, :], HT32v[:, :, o:o + sz], ht)
                for g in range(4):
                    ps = PS[g % 2]
                    for j in range(4):
                        kc = g * 4 + j
                        P.tr(ps[:, j * 128:(j + 1) * 128], ht[:, kc, :], ident(128))
                    P.copy(ot[:, g * 512:(g + 1) * 512], ps[:, :])
                P.dma("sp", out_t[s, o - NMETA:o - NMETA + sz, :], ot[:, :], ot)

    def stage_fox(l):
        tl = cfg.tiles
        with P.stage() as st:
            FG = st.tile([12, L], F32)
            CUM = st.tile([12, L], F32)
            ONE = st.tile([12, L], F32)
            NB = st.tile([12, 2], F32)
            ENDS = st.tile([12, NTl], F32)
            Q = st.tile([128, 6, L], BF16)
            Kt = st.tile([128, 6, L], BF16)
            VP = st.tile([128, NTl, 12, 65], BF16)
            CREF = st.tile([128, 12, NTl], F32)
            CUMK = st.tile([128, NTl, 12], F32)
            BIAS = st.tile([128, NTl, 12, NTl], F32)
            VT = [st.tile([128, 6, 128], F32) for _ in range(2)]
            PT = [st.tile([128, 128], BF16) for _ in range(3)]
            YTK = [st.tile([128, FW], F32) for _ in range(2)]
            RC = [st.tile([128, 1], F32) for _ in range(2)]
            YF = [st.tile([128, 6, 128], BF16) for _ in range(2)]
            PS_S = [st.psum() for _ in range(3)]
            PS_O = [st.psum() for _ in range(2)]
            PS_T = [st.psum() for _ in range(2)]
            zq = ZT.view("(c p) l -> p c l", p=128) if False else None
            P.dma("sp", FG[:, :], ZT[C1 + 2304:C1 + 2316, :], FG)
            P.dma("sp", NB[:, 0:1], foxbf[l], NB)
            P.ts(NB[:, 1:2], NB[:, 0:1], -1.0, ALU.mult)
            P.memset(ONE[:, :], 1.0)
            P.act(FG[:, :], FG[:, :], AF.Exp, bias=NB[:, 1:2], scale=-1.0)
            P.act(FG[:, :], FG[:, :], AF.Ln, bias=1.0)
            P.ts(FG[:, :], FG[:, :], -1.0, ALU.mult)
            P.scan(CUM[:, :], ONE[:, :], FG[:, :], 0.0, ALU.mult, ALU.add)
            qv = VA(ZT.t[C1:C1 + 768, :].rearrange("(c p) l -> p c l", p=128), ZT.res)
            kv = VA(ZT.t[C1 + 768:C1 + 1536, :].rearrange("(c p) l -> p c l", p=128), ZT.res)
            vv = VA(ZT.t[C1 + 1536:C1 + 2304, :].rearrange("(c p) l -> p c l", p=128), ZT.res)
            mk_stg(st, 2)
            wload(st, Q[:, :, :], qv, 128, 6, L)
            wload(st, Kt[:, :, :], kv, 128, 6, L)
            P.memset(VP[:, :, :, :], 1.0, eng="pool")
            for j, (o, sz) in enumerate(tl):
                P.copy(ENDS[:, j:j + 1], CUM[:, o + sz - 1:o + sz])
                ps = PS_T[j % 2]
                P.tr(ps[:sz, 0:12], CUM[:, o:o + sz], ident(12))
                P.copy(CUMK[:sz, j, :], ps[:sz, 0:12])
                vt = VT[j % 2]
                P.dma("sp", vt[:, :, :sz], vv[:, :, o:o + sz], vt)
                for c in range(6):
                    ps2 = PS_S[c % 3]
                    P.tr(ps2[:sz, 0:128], vt[:, c, :sz], ident(128))
                    P.copy(VP[:sz, j, 2 * c:2 * c + 2, 0:64],
                           VA(ps2.t[:sz, 0:128].rearrange("p (a b) -> p a b", a=2), ps2.res))
            psc = PS_O[0]
            for h in range(12):
                P.mm(psc[:, h * NTl:(h + 1) * NTl], CON[:12, CO_SEL + h * 128:CO_SEL + (h + 1) * 128], ENDS[:, :])
            P.copy(CREF[:, :, :], VA(psc.t[:, 0:12 * NTl].rearrange("p (a b) -> p a b", a=12), psc.res))
            for j, (o, sz) in enumerate(tl):
                for h in range(12):
                    P.ts(BIAS[:sz, j, h, :], CREF[:sz, h, :], CUMK[:sz, j, h:h + 1], ALU.subtract)
            it = 0
            for i, (oi, si) in enumerate(tl):
                ytk = YTK[i % 2]
                for h in range(12):
                    c, pr = h // 2, (h % 2) * 64
                    po = PS_O[h % 2]
                    rc = RC[h % 2]
                    for j in range(i + 1):
                        oj, sj = tl[j]
                        ps = PS_S[it % 3]
                        pt = PT[it % 3]
                        it += 1
                        P.mm(ps[:sj, :si], Kt[pr:pr + 64, c, oj:oj + sj], Q[pr:pr + 64, c, oi:oi + si])
                        P.act(pt[:sj, :si], ps[:sj, :si], AF.Exp, bias=BIAS[:sj, j, h, i:i + 1], scale=0.125)
                        if j == i:
                            P.tt(pt[:sj, :si], pt[:sj, :si], CONB[:sj, CO_MLE:CO_MLE + si], ALU.mult, eng="pool")
                        P.mm(po[:si, 0:65], pt[:sj, :si], VP[:sj, j, h, :], start=(j == 0), stop=(j == i))
                    P.recip(rc[:si, :], po[:si, 64:65])
                    P.ts(ytk[:si, h * 64:(h + 1) * 64], po[:si, 0:64], rc[:si, 0:1], ALU.mult)
                yf = YF[i % 2]
                for c in range(6):
                    ps = PS_T[c % 2]
                    P.tr(ps[:, :si], ytk[:si, c * 128:(c + 1) * 128], ident(si))
                    P.copy(yf[:, c, :si], ps[:, :si], eng="act")
                P.dma("sp", YTv[:, 6:12, oi:oi + si], yf[:, :, :si], yf)

    def stage_mlstm(l):
        tl = cfg.tiles
        SCL = 128 ** -0.5
        with P.stage() as st:
            XP = [st.tile([128, 3 + L], F32) for _ in range(2)]
            ACC = st.tile([128, L], F32)
            QT = st.tile([128, 4, L], BF16)
            KT = st.tile([128, 4, L], BF16)
            KTOK = st.tile([128, NTl, 4, 128], BF16)
            VP = st.tile([128, NTl, 4, 129], BF16)
            CW = st.tile([128, 8, 4], F32)
            IG = st.tile([4, L], F32)
            FGm = st.tile([4, L], F32)
            ONE = st.tile([4, L], F32)
            Bc = st.tile([4, L], F32)
            WK = st.tile([4, L], F32)
            EB = st.tile([4, L], F32)
            MB = st.tile([4, 4], F32)
            ENDE = st.tile([4, NTl], F32)
            GT = st.tile([128, NTl, 8], F32)
            EBE = st.tile([128, 4, NTl], F32)
            CN = st.tile([128, 4, 129], F32)
            CNB = st.tile([128, 4, 129], BF16)
            VT = [st.tile([128, 4, 128], F32) for _ in range(2)]
            OG = [st.tile([128, 4, 128], F32) for _ in range(2)]
            GM = [st.tile([128, 128], BF16) for _ in range(2)]
            HTK = [st.tile([128, MW], F32) for _ in range(2)]
            SC = [st.tile([128, 4], F32) for _ in range(2)]
            YM = [st.tile([128, 4, 128], BF16) for _ in range(2)]
            PS_G = [st.psum() for _ in range(2)]
            PS_O = [st.psum() for _ in range(2)]
            PS_C = st.psum()
            PS_T = [st.psum() for _ in range(2)]
            PS_TB = st.psum([128, 1024], BF16)
            P.dma("sp", CW[:, :, :], convw[:, l, :, :], CW)
            P.dma("sp", MB[:, 0:2], mlb[l], MB)
            P.ts(MB[:, 2:3], MB[:, 1:2], -1.0, ALU.mult)
            P.dma("sp", IG[:, :], ZT[C2 + 2048:C2 + 2052, :], IG)
            P.dma("sp", FGm[:, :], ZT[C2 + 2052:C2 + 2056, :], FGm)
            P.memset(ONE[:, :], 1.0)
            P.act(FGm[:, :], FGm[:, :], AF.Exp, bias=MB[:, 2:3], scale=-1.0)
            P.act(FGm[:, :], FGm[:, :], AF.Ln, bias=1.0)
            P.ts(FGm[:, :], FGm[:, :], -1.0, ALU.mult)
            for j, (o, sz) in enumerate(tl):
                P.scan(Bc[:, o:o + sz], ONE[:, o:o + sz], FGm[:, o:o + sz], 0.0, ALU.mult, ALU.add)
                P.copy(ENDE[:, j:j + 1], Bc[:, o + sz - 1:o + sz])
            P.act(ENDE[:, :], ENDE[:, :], AF.Exp)
            P.tt(WK[:, :], IG[:, :], Bc[:, :], ALU.subtract)
            P.act(WK[:, :], WK[:, :], AF.Exp, bias=MB[:, 0:1])
            P.act(EB[:, :], Bc[:, :], AF.Exp)
            for h in range(4):
                P.mm(PS_C[:, h * NTl:(h + 1) * NTl], CON[:4, CO_SEL + h * 128:CO_SEL + (h + 1) * 128], ENDE[:, :])
            P.copy(EBE[:, :, :], VA(PS_C.t[:, 0:4 * NTl].rearrange("p (a b) -> p a b", a=4), PS_C.res))
            for c in range(8):
                xp = XP[c % 2]
                P.memset(xp[:, 0:3], 0.0)
                P.dma("sp", xp[:, 3:3 + L], ZT[C2 + c * 128:C2 + (c + 1) * 128, :], xp)
                P.ts(ACC[:, :], xp[:, 0:L], CW[:, c, 0:1], ALU.mult)
                for jj in range(1, 4):
                    P.stt(ACC[:, :], xp[:, jj:jj + L], CW[:, c, jj:jj + 1], ACC[:, :], ALU.mult, ALU.add)
                if c < 4:
                    P.act(QT[:, c, :], ACC[:, :], AF.Silu)
                else:
                    P.act(ACC[:, :], ACC[:, :], AF.Silu)
                    P.ts(KT[:, c - 4, :], ACC[:, :], SCL, ALU.mult)
            vv = VA(ZT.t[C2 + 1024:C2 + 1536, :].rearrange("(c p) l -> p c l", p=128), ZT.res)
            ov = VA(ZT.t[C2 + 1536:C2 + 2048, :].rearrange("(c p) l -> p c l", p=128), ZT.res)
            for j, (o, sz) in enumerate(tl):
                pt = PS_T[j % 2]
                P.tr(pt[:sz, 0:4], WK[:, o:o + sz], ident(4))
                P.tr(pt[:sz, 4:8], EB[:, o:o + sz], ident(4))
                P.copy(GT[:sz, j, :], pt[:sz, 0:8])
                vt = VT[j % 2]
                P.dma("sp", vt[:, :, :sz], vv[:, :, o:o + sz], vt)
                for h in range(4):
                    P.tr(pt[:sz, 128 * (h % 2) + 128:128 * (h % 2) + 256], vt[:, h, :sz], ident(128))
                    P.ts(VP[:sz, j, h, 0:128], pt[:sz, 128 * (h % 2) + 128:128 * (h % 2) + 256], GT[:sz, j, h:h + 1], ALU.mult)
                    P.copy(VP[:sz, j, h, 128:129], GT[:sz, j, h:h + 1])
                    P.tr(PS_TB[:sz, h * 128:(h + 1) * 128], KT[:, h, o:o + sz], CONB[:, CO_ID:CO_ID + 128])
                P.copy(KTOK[:sz, j, :, :], VA(PS_TB.t[:sz, 0:512].rearrange("p (a b) -> p a b", a=4), PS_TB.res))
            for j, (o, sz) in enumerate(tl):
                htk = HTK[j % 2]
                sc = SC[j % 2]
                og = OG[j % 2]
                P.dma("sp", og[:, :, :sz], ov[:, :, o:o + sz], og)
                P.act(og[:, :, :sz], og[:, :, :sz], AF.Sigmoid)
                for h in range(4):
                    pg = PS_G[h % 2]
                    po = PS_O[h % 2]
                    gm = GM[h % 2]
                    P.mm(pg[:sz, :sz], KT[:, h, o:o + sz], QT[:, h, o:o + sz])
                    P.tt(gm[:sz, :sz], pg[:sz, :sz], CON[:sz, CO_MLE:CO_MLE + sz], ALU.mult)
                    P.mm(po[:sz, 0:129], gm[:sz, :sz], VP[:sz, j, h, :], start=True, stop=(j == 0))
                    if j > 0:
                        P.mm(po[:sz, 0:129], QT[:, h, o:o + sz], CNB[:, h, :], start=False, stop=True)
                    P.tt(sc[:sz, 0:1], po[:sz, 128:129], GT[:sz, j, 4 + h:5 + h], ALU.mult)
                    P.ts(sc[:sz, 1:2], sc[:sz, 0:1], -1.0, ALU.mult)
                    P.tt(sc[:sz, 1:2], sc[:sz, 1:2], sc[:sz, 0:1], ALU.max)
                    P.ts(sc[:sz, 1:2], sc[:sz, 1:2], 1.0, ALU.max)
                    P.recip(sc[:sz, 2:3], sc[:sz, 1:2])
                    P.tt(sc[:sz, 3:4], sc[:sz, 2:3], GT[:sz, j, 4 + h:5 + h], ALU.mult)
                    P.ts(htk[:sz, h * 128:(h + 1) * 128], po[:sz, 0:128], sc[:sz, 3:4], ALU.mult)
                    P.mm(PS_C[:, 0:129], KTOK[:sz, j, h, :], VP[:sz, j, h, :])
                    if j == 0:
                        P.ts(CN[:, h, :], PS_C[:, 0:129], EBE[:, h, j:j + 1], ALU.mult)
                    else:
                        P.tt(CN[:, h, :], CN[:, h, :], PS_C[:, 0:129], ALU.add)
                        P.ts(CN[:, h, :], CN[:, h, :], EBE[:, h, j:j + 1], ALU.mult)
                    P.copy(CNB[:, h, :], CN[:, h, :])
                ym = YM[j % 2]
                for h in range(4):
                    pt = PS_T[h % 2]
                    P.tr(pt[:, :sz], htk[:sz, h * 128:(h + 1) * 128], ident(sz))
                    P.tt(ym[:, h, :sz], pt[:, :sz], og[:, h, :sz], ALU.mult)
                P.dma("sp", YTv[:, 12:16, o:o + sz], ym[:, :, :sz], ym)

    def stage_rwkv(l, part=0, nlev_max=99, sub=99):
        tl = cfg.tiles
        BLK = CON[:, CO_BLK:CO_BLK + 128]
        RWSv = RWS.view("q (c p) l -> p q c l", p=128)
        with P.stage() as st:
            W2 = st.tile([96, RW], BF16)
            A2 = st.tile([96, RW], BF16)
            G2 = st.tile([128, 2, RW], BF16)
            mk_stg(st, 2)
            wload(st, VA(W2.t[:, :].rearrange("p (a b) -> p a b", a=1), W2.res),
                  VA(rw2.t[l].rearrange("p (a b) -> p a b", a=1), rw2.res), 96, 1, RW)
            wload(st, VA(A2.t[:, :].rearrange("p (a b) -> p a b", a=1), A2.res),
                  VA(ra2.t[l].rearrange("p (a b) -> p a b", a=1), ra2.res), 96, 1, RW)
            wload(st, G2[:, :, :], rg2.view("l (kc p) n -> l p kc n", p=128)[l], 128, 2, RW)
            ZP = [st.tile([128, 6, 513], F32) for _ in range(3)]
            ZL = [st.tile([128, 513], F32) for _ in range(4)]
            TMP = st.tile([128, 512], F32)
            XR = st.tile([128, 6, 512], F32)
            XK = st.tile([128, 6, 512], F32)
            XV = st.tile([128, 6, 512], F32)
            LW = st.tile([128, 6, 512], F32)
            AA = st.tile([128, 6, 512], F32)
            GG = st.tile([128, 6, 512], F32)
            KK = st.tile([128, 6, 512], F32)
            BB = st.tile([128, 6, 512], F32)
            BON = st.tile([128, 6, 512], F32)
            TW = st.tile([96, 512], BF16)
            TA = st.tile([96, 512], BF16)
            SG = st.tile([128, 2, 512], BF16)
            PS = [st.psum() for _ in range(4)]
            pi = 0
            for (o, n) in cfg.sbs:
                def load_shift(zp_slice, rows0, nrows, mu_col, out):
                    pass
                for qi, (r0, X) in enumerate([(0, XR), (768, XK), (1536, XV)]):
                    zp = ZP[qi]
                    src_ = VA(ZT.t[r0:r0 + 768, :].rearrange("(c p) l -> p c l", p=128), ZT.res)
                    if o == 0:
                        P.memset(zp[:, :, 0:1], 0.0)
                        P.dma("sp", zp[:, :, 1:1 + n], src_[:, :, 0:n], zp)
                    else:
                        P.dma("sp", zp[:, :, 0:1 + n], src_[:, :, o - 1:o + n], zp, disjoint=False)
                    for c in range(6):
                        P.tt(TMP[:, :n], zp[:, c, 0:n], zp[:, c, 1:1 + n], ALU.subtract)
                        P.stt(X[:, c, :n], TMP[:, :n], VR[:, l, qi, c:c + 1], zp[:, c, 1:1 + n], ALU.mult, ALU.add)
                for qi, (r0, nr, vi, vc) in enumerate([(2304, 96, 10, 0), (2400, 96, 11, 0), (2496, 128, 12, 0), (2624, 128, 12, 1)]):
                    zl = ZL[qi]
                    if o == 0:
                        P.memset(zl[:nr, 0:1], 0.0)
                        P.dma("sp", zl[:nr, 1:1 + n], ZT[r0:r0 + nr, 0:n], zl)
                    else:
                        P.dma("sp", zl[:nr, 0:1 + n], ZT[r0:r0 + nr, o - 1:o + n], zl, disjoint=False)
                    P.tt(TMP[:nr, :n], zl[:nr, 0:n], zl[:nr, 1:1 + n], ALU.subtract)
                    P.stt(TMP[:nr, :n], TMP[:nr, :n], VR[:nr, l, vi, vc:vc + 1], zl[:nr, 1:1 + n], ALU.mult, ALU.add)
                    if qi == 0:
                        P.act(TW[:, :n], TMP[:96, :n], AF.Tanh)
                    elif qi == 1:
                        P.copy(TA[:, :n], TMP[:96, :n])
                    else:
                        P.act(SG[:, qi - 2, :n], TMP[:, :n], AF.Sigmoid)
                for c in range(6):
                    cs = slice(c * 128, (c + 1) * 128)
                    ps = PS[pi % 4]; pi += 1
                    P.mm(ps[:, :n], W2[:, cs], TW[:, :n])
                    P.act(LW[:, c, :n], ps[:, :n], AF.Sigmoid, bias=VR[:, l, 3, c:c + 1])
                    P.ts(LW[:, c, :n], LW[:, c, :n], -0.6065306597126334, ALU.mult)
                    ps = PS[pi % 4]; pi += 1
                    P.mm(ps[:, :n], A2[:, cs], TA[:, :n])
                    P.act(AA[:, c, :n], ps[:, :n], AF.Sigmoid, bias=VR[:, l, 4, c:c + 1])
                    ps = PS[pi % 4]; pi += 1
                    P.mm(ps[:, :n], G2[:, 0, cs], SG[:, 0, :n], start=True, stop=False)
                    P.mm(ps[:, :n], G2[:, 1, cs], SG[:, 1, :n], start=False, stop=True)
                    P.copy(GG[:, c, :n], ps[:, :n], eng="act")
                    P.ts(KK[:, c, :n], XK[:, c, :n], VR[:, l, 5, c:c + 1], ALU.mult)
                    P.tt(TMP[:, :n], KK[:, c, :n], KK[:, c, :n], ALU.mult)
                    ps = PS[pi % 4]; pi += 1
                    P.mm(ps[:, :n], BLK, TMP[:, :n])
                    P.act(TMP[:, :n], ps[:, :n], AF.Sqrt)
                    P.ts(TMP[:, :n], TMP[:, :n], 1e-12, ALU.max)
                    P.recip(TMP[:, :n], TMP[:, :n])
                    P.tt(KK[:, c, :n], KK[:, c, :n], TMP[:, :n], ALU.mult)
                    P.ts(TMP[:, :n], AA[:, c, :n], -1.0, ALU.add, VR[:, l, 6, c:c + 1], ALU.mult)
                    P.ts(TMP[:, :n], TMP[:, :n], 1.0, ALU.add)
                    P.tt(XK[:, c, :n], XK[:, c, :n], TMP[:, :n], ALU.mult)
                    P.tt(BB[:, c, :n], AA[:, c, :n], KK[:, c, :n], ALU.mult)
                    P.tt(TMP[:, :n], XR[:, c, :n], XK[:, c, :n], ALU.mult)
                    P.ts(TMP[:, :n], TMP[:, :n], VR[:, l, 7, c:c + 1], ALU.mult)
                    ps = PS[pi % 4]; pi += 1
                    P.mm(ps[:, :n], BLK, TMP[:, :n])
                    P.tt(BON[:, c, :n], ps[:, :n], XV[:, c, :n], ALU.mult)
                for qi, X in enumerate([XR, XK, XV, LW, KK, BB, BON, GG]):
                    P.dma("sp", RWSv[:, qi, :, o:o + n], X[:, :, :n], X)
        if part == 1:
            return
        with P.stage() as st:
            X8 = [st.tile([128, 8, 128], F32) for _ in range(2)]
            ONE = st.tile([128, 128], F32)
            CWt = st.tile([128, 128], F32)
            CWX = st.tile([128, 128], F32)
            EW = st.tile([128, 3, 128], F32)
            QS = st.tile([128, 4, 128], F32)
            TOK = st.tile([128, 3, 128], F32)
            SST = st.tile([128, 6, 64], F32)
            AM = [st.tile([128, 5, 128], F32) for _ in range(2)]
            PW = [st.tile([128, 2, 128], F32) for _ in range(2)]
            UU = st.tile([128, 128], F32)
            OTOK = st.tile([128, 128], F32)
            OT = st.tile([128, 128], F32)
            T1 = st.tile([128, 128], F32)
            T2 = st.tile([128, 128], F32)
            YR = [st.tile([128, 128], BF16) for _ in range(2)]
            PA = [st.psum() for _ in range(3)]
            PB = [st.psum() for _ in range(2)]
            PC = [st.psum() for _ in range(2)]
            P.memset(ONE[:, :], 1.0)
            P.memset(SST[:, :, :], 0.0)
            MLE = CON[:, CO_MLE:CO_MLE + 128]
            MLT = CON[:, CO_MLT:CO_MLT + 128]
            MGE = CON[:, CO_MGE:CO_MGE + 128]
            it = 0
            for i, (o, sz) in enumerate(tl):
                nlev = min(int(np.ceil(np.log2(sz))), nlev_max)
                if nlev_max == 98 and i == 0:
                    continue
                for c in range(6):
                    x8 = X8[it % 2]
                    am = AM[it % 2]
                    yr = YR[it % 2]
                    it += 1
                    P.dma("sp", x8[:, :, :sz], RWSv[:, :, c, o:o + sz], x8)
                    R_, K_, V_, LW_, KK_, B_, BON_, G_ = [x8[:, q, :sz] for q in range(8)]
                    P.scan(CWt[:, :sz], ONE[:, :sz], LW_, 0.0, ALU.mult, ALU.add)
                    P.tt(CWX[:, :sz], CWt[:, :sz], LW_, ALU.subtract)
                    P.act(EW[:, 0, :sz], CWt[:, :sz], AF.Exp)
                    P.act(EW[:, 1, :sz], CWt[:, :sz], AF.Exp, scale=-1.0)
                    P.act(EW[:, 2, :sz], CWX[:, :sz], AF.Exp)
                    P.stt(QS[:, 0, :sz], KK_, -1.0, EW[:, 2, :sz], ALU.mult, ALU.mult)
                    P.tt(QS[:, 1, :sz], R_, EW[:, 0, :sz], ALU.mult)
                    P.tt(QS[:, 2, :sz], B_, EW[:, 1, :sz], ALU.mult)
                    P.tt(QS[:, 3, :sz], K_, EW[:, 1, :sz], ALU.mult)
                    pt = PA[0]
                    P.tr(pt[:sz, 0:128], V_, ident(128))
                    P.tr(pt[:sz, 128:256], QS[:, 2, :sz], ident(128))
                    P.tr(pt[:sz, 256:384], QS[:, 3, :sz], ident(128))
                    P.copy(TOK[:sz, :, :], VA(pt.t[:sz, 0:384].rearrange("p (a b) -> p a b", a=3), pt.res))
                    if sub == 0:
                        continue
                    for hh in range(2):
                        pr = slice(hh * 64, hh * 64 + 64)
                        AT = QS[pr, 0, :sz]
                        RT = QS[pr, 1, :sz]
                        BT = QS[pr, 2, :sz]
                        KT_ = QS[pr, 3, :sz]
                        p1, p2, p3 = PA[1], PA[2], PB[0]
                        P.mm(p1[:sz, 0:sz], BT, AT)
                        P.mm(p1[:sz, 128:128 + sz], BT, RT)
                        P.mm(p2[:sz, 0:sz], KT_, AT)
                        P.mm(p2[:sz, 128:128 + sz], KT_, RT)
                        P.mm(p3[:sz, 0:sz], AT, BT)
                        P.tt(am[:sz, 0, :sz], p1[:sz, 0:sz], MLT[:sz, :sz], ALU.mult)
                        P.tt(am[:sz, 1, :sz], p1[:sz, 128:128 + sz], MLE[:sz, :sz], ALU.mult)
                        P.tt(am[:sz, 2, :sz], p2[:sz, 0:sz], MLT[:sz, :sz], ALU.mult)
                        P.tt(am[:sz, 3, :sz], p2[:sz, 128:128 + sz], MLE[:sz, :sz], ALU.mult)
                        P.tt(am[:sz, 4, :sz], p3[:sz, 0:sz], MGE[:sz, :sz], ALU.mult)
                        if sub == 1:
                            continue
                        p4 = PB[1]
                        P.mm(p4[:sz, 0:64], AT, SST[pr, c, :], start=True, stop=False)
                        P.mm(p4[:sz, 0:64], am[:sz, 2, :sz], TOK[:sz, 0, pr], start=False, stop=True)
                        U = UU[:sz, pr]
                        P.copy(U, p4[:sz, 0:64])
                        Pm, PTm = am[:sz, 4, :sz], am[:sz, 0, :sz]
                        for lev in range(nlev):
                            pu = PC[lev % 2]
                            P.mm(pu[:sz, 0:64], PTm, U)
                            if lev < nlev - 1:
                                pw = PW[lev % 2]
                                pq, pq2 = PB[0], PB[1]
                                P.mm(pq[:sz, 0:sz], PTm, Pm)
                                P.mm(pq2[:sz, 0:sz], Pm, PTm)
                            P.tt(U, U, pu[:sz, 0:64], ALU.add)
                            if lev < nlev - 1:
                                P.copy(pw[:sz, 0, :sz], pq[:sz, 0:sz])
                                P.copy(pw[:sz, 1, :sz], pq2[:sz, 0:sz])
                                Pm, PTm = pw[:sz, 0, :sz], pw[:sz, 1, :sz]
                        if sub == 2:
                            continue
                        po = PC[0]
                        P.mm(po[:sz, 64:128], RT, SST[pr, c, :], start=True, stop=False)
                        P.mm(po[:sz, 64:128], am[:sz, 1, :sz], U, start=False, stop=False)
                        P.mm(po[:sz, 64:128], am[:sz, 3, :sz], TOK[:sz, 0, pr], start=False, stop=True)
                        P.copy(OTOK[:sz, pr], po[:sz, 64:128])
                    if sub in (1, 2, 3):
                        continue
                    psu = PA[1]
                    P.mm(psu[:, 0:128], TOK[:sz, 1, :], UU[:sz, :], start=True, stop=False)
                    P.mm(psu[:, 0:128], TOK[:sz, 2, :], TOK[:sz, 0, :], start=False, stop=True)
                    for hh in range(2):
                        pr = slice(hh * 64, hh * 64 + 64)
                        P.tt(SST[pr, c, :], SST[pr, c, :], psu[pr, pr], ALU.add)
                        P.ts(SST[pr, c, :], SST[pr, c, :], EW[pr, 0, sz - 1:sz], ALU.mult)
                    if sub == 4:
                        continue
                    ptt = PA[2]
                    P.tr(ptt[:, 0:sz], OTOK[:sz, :], ident(sz))
                    P.copy(OT[:, :sz], ptt[:, 0:sz])
                    P.tt(T1[:, :sz], OT[:, :sz], OT[:, :sz], ALU.mult)
                    pmn = PB[0]
                    P.mm(pmn[:, 0:sz], BLK, OT[:, :sz])
                    P.mm(pmn[:, 128:128 + sz], BLK, T1[:, :sz])
                    P.ts(T1[:, :sz], pmn[:, 0:sz], 1.0 / 64, ALU.mult)
                    P.tt(T2[:, :sz], T1[:, :sz], T1[:, :sz], ALU.mult)
                    P.stt(T2[:, :sz], pmn[:, 128:128 + sz], 1.0 / 64, T2[:, :sz], ALU.mult, ALU.subtract)
                    P.ts(T2[:, :sz], T2[:, :sz], 64e-5, ALU.add)
                    P.act(T2[:, :sz], T2[:, :sz], AF.Sqrt)
                    P.recip(T2[:, :sz], T2[:, :sz])
                    P.tt(OT[:, :sz], OT[:, :sz], T1[:, :sz], ALU.subtract)
                    P.tt(OT[:, :sz], OT[:, :sz], T2[:, :sz], ALU.mult)
                    P.ts(OT[:, :sz], OT[:, :sz], VR[:, l, 8, c:c + 1], ALU.mult, VR[:, l, 9, c:c + 1], ALU.add)
                    P.tt(OT[:, :sz], OT[:, :sz], BON_, ALU.add)
                    P.tt(yr[:, :sz], OT[:, :sz], G_, ALU.mult)
                    P.dma("sp", YT[c * 128:(c + 1) * 128, o:o + sz], yr[:, :sz], yr)
    def run_all():
        P.no_reset = (cfg.NSEQ == 1)
        prologue()
        P.reset_all()
        def body(s):
            stage_ln0(s)
            for l in range(dep):
                stage_win(l)
                stage_fox(l)
                stage_mlstm(l)
                stage_rwkv(l)
                stage_m1(l)
                stage_m2(l)
                ln_stage(NEWTv, 2 + 4 * l + 0, 2 + 4 * l + 1)
                stage_ffn(l)
                ln_stage(NEWTv, 2 + 4 * l + 2, 2 + 4 * l + 3)
            stage_out(s)
            P.reset_all()
        if cfg.NSEQ == 1:
            body(0)
        else:
            with nc.Fori(0, cfg.NSEQ) as s:
                body(s)
        P.es.close()
    return finish(locals())


def finish(lc):
    P, cfg, dbg = lc["P"], lc["cfg"], lc["dbg"]
    run = lc.get("run_stages")
    return lc


def pack_small(p, dep):
    def fm(v):
        return np.ascontiguousarray(np.asarray(v, np.float32).reshape(KC, 128).T)
    vecD = np.zeros((128, 2 + 4 * dep, KC), np.float32)
    vecD[:, 0] = fm(p["ln_emb_g"])
    vecD[:, 1] = fm(p["ln_emb_b"])
    for l in range(dep):
        vecD[:, 2 + 4 * l + 0] = fm(p["ln1_g"][l])
        vecD[:, 2 + 4 * l + 1] = fm(p["ln1_b"][l])
        vecD[:, 2 + 4 * l + 2] = fm(p["ln2_g"][l])
        vecD[:, 2 + 4 * l + 3] = fm(p["ln2_b"][l])
    vecR = np.zeros((128, dep, 13, 6), np.float32)

    def f6(v):
        return np.asarray(v, np.float32).reshape(6, 128).T
    for l in range(dep):
        mu = np.asarray(p["rwkv_mu"][l], np.float32)
        vecR[:, l, 0] = f6(mu[0:768])
        vecR[:, l, 1] = f6(mu[768:1536])
        vecR[:, l, 2] = f6(mu[1536:2304])
        vecR[:, l, 3] = f6(p["rwkv_w0"][l])
        vecR[:, l, 4] = f6(p["rwkv_a0"][l])
        vecR[:, l, 5] = f6(p["rwkv_k_k"][l])
        vecR[:, l, 6] = f6(p["rwkv_k_a"][l])
        vecR[:, l, 7] = f6(np.asarray(p["rwkv_r_k"][l]).reshape(768))
        vecR[:, l, 8] = f6(p["rwkv_gn_g"][l])
        vecR[:, l, 9] = f6(p["rwkv_gn_b"][l])
        vecR[:96, l, 10, 0] = mu[2304:2400]
        vecR[:96, l, 11, 0] = mu[2400:2496]
        vecR[:, l, 12, 0] = mu[2496:2624]
        vecR[:, l, 12, 1] = mu[2624:2752]
    foxbf = np.ascontiguousarray(np.asarray(p["fox_b_f"], np.float32)[:dep].reshape(dep, 12, 1))
    cw = np.asarray(p["mlstm_conv_w"], np.float32)[:dep]
    convw = np.ascontiguousarray(cw.reshape(dep, 4, 8, 128).transpose(3, 0, 2, 1))
    mlb = np.zeros((dep, 4, 2), np.float32)
    mlb[:, :, 0] = np.asarray(p["mlstm_b_i"], np.float32)[:dep]
    mlb[:, :, 1] = np.asarray(p["mlstm_b_f"], np.float32)[:dep]
    return {"consts": make_consts(), "vecD": vecD, "vecR": vecR, "foxbf": foxbf, "convw": convw, "mlb": mlb}


N_CORES = 8


def kernel(**inputs):
    x = np.asarray(inputs["x"], np.float32)
    B = x.shape[0]
    nseq = B // N_CORES
    cfg = Cfg(NT=16, NSEQ=nseq, depth=4)
    Res.ALL.clear()
    lc = build(cfg)
    lc["run_all"]()
    nc = lc["nc"]
    small = pack_small(inputs, 4)
    shared = dict(small)
    shared["meta"] = np.asarray(inputs["meta_tokens"], np.float32)
    for k in ["w_in", "rwkv_w2", "rwkv_a2", "rwkv_g2", "proj_rwkv", "proj_fox", "proj_mlstm", "w_out", "ffn_w1", "ffn_w3",
              "ffn_w2", "router_w", "moe_w1", "moe_w3", "moe_w2"]:
        shared[k] = np.asarray(inputs[k], np.float32)
    in_maps = []
    for c in range(N_CORES):
        m = dict(shared)
        m["x"] = np.ascontiguousarray(x[c * nseq:(c + 1) * nseq])
        in_maps.append(m)
    res = run_bass_kernel_spmd(nc, in_maps, core_ids=list(range(N_CORES)))
    return np.concatenate([r["out"] for r in res.results], axis=0).astype(np.float32)
```

```python
import numpy as np
from contextlib import ExitStack, contextmanager
import concourse.bass as bass
import concourse.mybir as mybir
from concourse.bass_utils import run_bass_kernel_spmd

F32 = mybir.dt.float32
BF16 = mybir.dt.bfloat16
AF = mybir.ActivationFunctionType
ALU = mybir.AluOpType
AX = mybir.AxisListType

D = 2048
KC = 16
NMETA = 16
RW = 768
RH = 12
FW = 768
MW = 512
MH = 4
C1 = 2752
C2 = C1 + 2316
C3 = C2 + 2056
NIN = C3 + 6144
DFF = 5632
FC = 44
NE = 8
ALPHA = 8 ** 0.25
NDS = 48
SAME_ENGINE_WAITS = True


class Res:
    __slots__ = ("name", "w", "r", "sem", "base")

    ALL = []

    def __init__(self, name):
        self.name = name
        self.w = {}
        self.r = {}
        self.base = {}
        self.sem = None
        Res.ALL.append(self)


class VA:
    __slots__ = ("ap", "res")

    def __init__(self, ap, res):
        self.ap = ap
        self.res = res

    def __getitem__(self, idx):
        return VA(self.ap[idx], self.res)


class T:
    def __init__(self, t, name):
        self.t = t
        self.res = Res(name)

    def __getitem__(self, idx):
        return VA(self.t[idx], self.res)


class Prog:
    def __init__(self, nc):
        self.nc = nc
        self.es = ExitStack()
        self.eng = {"pe": nc.tensor, "act": nc.scalar, "dve": nc.vector, "pool": nc.gpsimd, "sp": nc.sync}
        self.esem = {k: self.es.enter_context(nc.semaphore(f"e_{k}")) for k in self.eng}
        self.ecnt = {k: 0 for k in self.eng}
        self.dsems = [self.es.enter_context(nc.semaphore(f"d{i}")) for i in range(NDS)]
        self.dcnt = [0] * NDS
        self.dnext = 0
        self.obs = {k: {} for k in self.eng}
        self.ninstr = 0

    def semof(self, key):
        return self.esem[key[1]] if key[0] == "e" else self.dsems[key[1]]

    def _waits(self, eng, reads, writes, disjoint):
        waits = {}
        for r in reads:
            for k, v in r.w.items():
                if waits.get(k, 0) < v:
                    waits[k] = v
        for w in writes:
            for k, v in w.r.items():
                if waits.get(k, 0) < v:
                    waits[k] = v
            for k, v in (w.base if disjoint else w.w).items():
                if waits.get(k, 0) < v:
                    waits[k] = v
        ob = self.obs[eng]
        e = self.eng[eng]
        for k, v in waits.items():
            if k == ("e", eng) and (eng == "pe" or not SAME_ENGINE_WAITS):
                continue
            if ob.get(k, 0) >= v:
                continue
            e.wait_ge(self.semof(k), v)
            ob[k] = v
            self.ninstr += 1

    def _record(self, key, val, reads, writes, disjoint):
        for r in reads:
            if r.r.get(key, 0) < val:
                r.r[key] = val
        for w in writes:
            if not disjoint:
                w.r = {}
                w.w = {key: val}
                w.base = {key: val}
            else:
                if w.w.get(key, 0) < val:
                    w.w[key] = val

    def op(self, eng, fn, reads, writes, disjoint=False):
        reads = [x.res for x in reads if x is not None]
        writes = [x.res for x in writes]
        self._waits(eng, reads, writes, disjoint)
        ins = fn(self.eng[eng])
        self.ecnt[eng] += 1
        ins.then_inc(self.esem[eng], 1)
        self.ninstr += 1
        self._record(("e", eng), self.ecnt[eng], reads, writes, disjoint)

    def dma(self, q, out, in_, tile, disjoint=True):
        res = tile.res
        if res.sem is None:
            res.sem = self.dnext % NDS
            self.dnext += 1
        idx = res.sem
        reads = [in_.res]
        writes = [out.res]
        self._waits(q, reads, writes, disjoint)
        ins = self.eng[q].dma_start(out=out.ap, in_=in_.ap)
        self.dcnt[idx] += 1
        ins.then_inc(self.dsems[idx], 16)
        self.ninstr += 1
        self._record(("d", idx), 16 * self.dcnt[idx], reads, writes, disjoint)

    def barrier(self):
        ev = {("e", k): v for k, v in self.ecnt.items() if v > 0}
        for i in range(NDS):
            if self.dcnt[i] > 0:
                ev[("d", i)] = 16 * self.dcnt[i]
        for eng, e in self.eng.items():
            ob = self.obs[eng]
            for k, v in ev.items():
                if ob.get(k, 0) >= v:
                    continue
                e.wait_ge(self.semof(k), v)
                ob[k] = v
                self.ninstr += 1

    def reset_all(self):
        nc = self.nc
        self.barrier()
        if getattr(self, "no_reset", False):
            return
        if not hasattr(self, "bsem"):
            self.bsem = [self.es.enter_context(nc.semaphore(f"bar{i}")) for i in range(4)]
        A, C, B, Dd = self.bsem
        order = ["pe", "act", "dve", "pool", "sp"]
        for k in order:
            e = self.eng[k]
            e.sem_inc(A, 1)
            e.wait_ge(A, 5)
            e.sem_inc(C, 1)
        pe = self.eng["pe"]
        pe.wait_ge(C, 5)
        for s_ in list(self.esem.values()) + list(self.dsems):
            pe.sem_clear(s_)
        pe.sem_clear(A)
        pe.sem_clear(C)
        pe.sem_inc(B, 1)
        for k in order[1:]:
            e = self.eng[k]
            e.wait_ge(B, 1)
            e.sem_inc(Dd, 1)
        pe.wait_ge(Dd, 4)
        pe.sem_clear(B)
        pe.sem_clear(Dd)
        self.ecnt = {k: 0 for k in self.eng}
        self.dcnt = [0] * NDS
        self.obs = {k: {} for k in self.eng}
        for r in Res.ALL:
            r.w = {}
            r.r = {}
            r.base = {}

    @contextmanager
    def stage(self):
        st = Stage(self)
        try:
            yield st
        finally:
            self.barrier()
            st.es.close()

    def dram(self, name, shape, dt):
        return T(self.nc.dram_tensor(name, list(shape), dt, kind="Internal"), name)

    def mm(self, out, lhsT, rhs, start=True, stop=True):
        self.op("pe", lambda e: e.matmul(out.ap, lhsT.ap, rhs.ap, start=start, stop=stop),
                [lhsT, rhs], [out], disjoint=True)

    def tr(self, out, in_, ident):
        self.op("pe", lambda e: e.transpose(out.ap, in_.ap, ident.ap), [in_, ident], [out], disjoint=True)

    def act(self, out, in_, func, bias=None, scale=1.0, eng="act"):
        def f(e):
            kw = {}
            if bias is not None:
                kw["bias"] = bias.ap if isinstance(bias, VA) else bias
            return e.activation(out=out.ap, in_=in_.ap, func=func, scale=scale, **kw)
        self.op("act", f, [in_, bias if isinstance(bias, VA) else None], [out], disjoint=True)

    def tt(self, out, a, b, op, eng="dve"):
        self.op(eng, lambda e: e.tensor_tensor(out=out.ap, in0=a.ap, in1=b.ap, op=op), [a, b], [out], disjoint=True)

    def ts(self, out, a, s1, op0, s2=None, op1=None, eng="dve"):
        def f(e):
            a1 = s1.ap if isinstance(s1, VA) else s1
            if s2 is None:
                return e.tensor_scalar(out=out.ap, in0=a.ap, scalar1=a1, scalar2=None, op0=op0)
            a2 = s2.ap if isinstance(s2, VA) else s2
            return e.tensor_scalar(out=out.ap, in0=a.ap, scalar1=a1, scalar2=a2, op0=op0, op1=op1)
        self.op(eng, f, [a, s1 if isinstance(s1, VA) else None, s2 if isinstance(s2, VA) else None], [out],
                disjoint=True)

    def stt(self, out, a, s, b, op0, op1):
        def f(e):
            sc = s.ap if isinstance(s, VA) else s
            return e.scalar_tensor_tensor(out=out.ap, in0=a.ap, scalar=sc, in1=b.ap, op0=op0, op1=op1)
        self.op("dve", f, [a, b, s if isinstance(s, VA) else None], [out], disjoint=True)

    def copy(self, out, in_, eng="dve"):
        if eng == "act":
            self.act(out, in_, AF.Copy)
        else:
            self.op(eng, lambda e: e.tensor_copy(out=out.ap, in_=in_.ap), [in_], [out], disjoint=True)

    def recip(self, out, in_):
        self.op("dve", lambda e: e.reciprocal(out=out.ap, in_=in_.ap), [in_], [out], disjoint=True)

    def memset(self, out, val, eng="dve"):
        self.op(eng, lambda e: e.memset(out.ap, val), [], [out], disjoint=False)

    def scan(self, out, d0, d1, init, op0, op1):
        self.op("dve", lambda e: e.tensor_tensor_scan(out=out.ap, data0=d0.ap, data1=d1.ap, initial=init,
                                                      op0=op0, op1=op1), [d0, d1], [out], disjoint=True)


class Stage:
    def __init__(self, p):
        self.p = p
        self.es = ExitStack()
        self.n = 0

    def tile(self, shape, dt, name=None):
        self.n += 1
        name = name or f"t{self.n}"
        self.p.uid = getattr(self.p, "uid", 0) + 1
        nm = f"{name}_{self.p.uid}"
        return T(self.es.enter_context(self.p.nc.sbuf_tensor(nm, list(shape), dt)), nm)

    def psum(self, shape=(128, 512), dt=F32, name=None):
        self.n += 1
        self.p.uid = getattr(self.p, "uid", 0) + 1
        nm = f"ps_{self.p.uid}"
        return T(self.es.enter_context(self.p.nc.psum_tensor(nm, list(shape), dt)), nm)


CO_ID = 0
CO_MEAN = 128
CO_BLK = 256
CO_MLE = 384
CO_MLT = 512
CO_MGE = 640
CO_ONE = 768
CO_SEL = 896
NCONST = CO_SEL + 12 * 128


def make_consts():
    c = np.zeros((128, NCONST), np.float32)
    i = np.arange(128)
    c[:, CO_ID:CO_ID + 128] = np.eye(128)
    c[:, CO_MEAN:CO_MEAN + 128] = 1.0 / D
    c[:64, CO_BLK:CO_BLK + 64] = 1.0
    c[64:, CO_BLK + 64:CO_BLK + 128] = 1.0
    c[:, CO_MLE:CO_MLE + 128] = (i[:, None] <= i[None, :])
    c[:, CO_MLT:CO_MLT + 128] = (i[:, None] < i[None, :])
    c[:, CO_MGE:CO_MGE + 128] = (i[:, None] > i[None, :])
    c[:, CO_ONE:CO_ONE + 128] = 1.0
    for h in range(12):
        c[h, CO_SEL + h * 128:CO_SEL + (h + 1) * 128] = 1.0
    return c


class TT(T):
    def view(self, pat, **kw):
        v = TT.__new__(TT)
        v.t = self.t.rearrange(pat, **kw)
        v.res = self.res
        return v


def va_re(va, pat, **kw):
    return VA(va.ap.rearrange(pat, **kw), va.res)


class Cfg:
    def __init__(self, NT=16, NSEQ=1, depth=4, debug=False):
        self.NT = NT
        self.NSEQ = NSEQ
        self.depth = depth
        self.L = NMETA + 128 * NT
        self.S = 128 * NT
        self.tiles = [(0, NMETA)] + [(NMETA + 128 * i, 128) for i in range(NT)]
        self.sbs = [(o, min(512, self.L - o)) for o in range(0, self.L, 512)]
        self.n_dense = (depth + 1) // 2
        self.n_moe = depth // 2
        self.debug = debug


def col_chunks():
    segs = [0, 768, 1536, 2304, 2400, 2496, C1, C1 + 768, C1 + 1536, C1 + 2304, C2, C2 + 512, C2 + 1024,
            C2 + 1536, C2 + 2048, C3, C3 + 2048, C3 + 4096, NIN]
    chunks = []
    for a, b in zip(segs[:-1], segs[1:]):
        c = a
        while c < b:
            m = min(128, b - c)
            chunks.append((c, m))
            c += m
    blocks = []
    cur = []
    for ch in chunks:
        if cur and (ch[0] + ch[1] - cur[0][0]) > 512:
            blocks.append(cur)
            cur = []
        cur.append(ch)
    blocks.append(cur)
    return blocks


def build(cfg):
    nc = bass.Bass("TRN2", target_bir_lowering=False)
    P = Prog(nc)
    L, NT, dep = cfg.L, cfg.NT, cfg.depth
    NTl = NT + 1

    def ext(name, shape, dt=F32):
        t = TT.__new__(TT)
        t.t = nc.dram_tensor(name, list(shape), dt, kind="ExternalInput").ap()
        t.res = Res(name)
        return t

    def scr(name, shape, dt=F32):
        t = TT.__new__(TT)
        t.t = nc.dram_tensor(name, list(shape), dt, kind="Internal").ap()
        t.res = Res(name)
        return t

    x_in = ext("x", [cfg.NSEQ, cfg.S, D])
    meta = ext("meta", [NMETA, D])
    consts = ext("consts", [128, NCONST])
    vecD = ext("vecD", [128, 2 + 4 * dep, KC])
    vecR = ext("vecR", [128, dep, 13, 6])
    foxbf = ext("foxbf", [dep, 12, 1])
    convw = ext("convw", [128, dep, 8, 4])
    mlb = ext("mlb", [dep, 4, 2])
    w_in = ext("w_in", [dep, D, NIN])
    rw2 = ext("rwkv_w2", [dep, 96, RW])
    ra2 = ext("rwkv_a2", [dep, 96, RW])
    rg2 = ext("rwkv_g2", [dep, 256, RW])
    p_r = ext("proj_rwkv", [dep, RW, D])
    p_f = ext("proj_fox", [dep, FW, D])
    p_m = ext("proj_mlstm", [dep, MW, D])
    w_o = ext("w_out", [dep, D, D])
    f_w1 = ext("ffn_w1", [cfg.n_dense, D, DFF])
    f_w3 = ext("ffn_w3", [cfg.n_dense, D, DFF])
    f_w2 = ext("ffn_w2", [cfg.n_dense, DFF, D])
    if cfg.n_moe:
        r_w = ext("router_w", [cfg.n_moe, D, NE])
        m_w1 = ext("moe_w1", [cfg.n_moe, NE, D, DFF])
        m_w3 = ext("moe_w3", [cfg.n_moe, NE, D, DFF])
        m_w2 = ext("moe_w2", [cfg.n_moe, NE, DFF, D])
    out_t = TT.__new__(TT)
    out_t.t = nc.dram_tensor("out", [cfg.NSEQ, cfg.S, D], F32, kind="ExternalOutput").ap()
    out_t.res = Res("out")
    dbg = {}
    if cfg.debug:
        for nm, shp in [("d_ht", [D, L]), ("d_zt", [NIN, L]), ("d_yt", [D, L]), ("d_new", [D, L])]:
            t = TT.__new__(TT)
            t.t = nc.dram_tensor(nm, shp, F32, kind="ExternalOutput").ap()
            t.res = Res(nm)
            dbg[nm] = t

    HT32 = scr("HT32", [D, L])
    HTb = scr("HTb", [D, L], BF16)
    ZT = scr("ZT", [NIN, L])
    YT = scr("YT", [D, L], BF16)
    MIXT = scr("MIXT", [D, L], BF16)
    NEWT = scr("NEWT", [D, L])
    RWS = scr("RWS", [8, RW, L])
    HT32v = HT32.view("(kc p) l -> p kc l", p=128)
    HTbv = HTb.view("(kc p) l -> p kc l", p=128)
    YTv = YT.view("(kc p) l -> p kc l", p=128)
    MIXTv = MIXT.view("(kc p) l -> p kc l", p=128)
    NEWTv = NEWT.view("(kc p) l -> p kc l", p=128)

    ges = P.es

    def gtile(name, shape, dt):
        return TT_from(ges.enter_context(nc.sbuf_tensor(name, list(shape), dt)), name)

    def TT_from(t, name):
        o = TT.__new__(TT)
        o.t = t
        o.res = Res(name)
        return o

    CON = gtile("CON", [128, NCONST], F32)
    CONB = gtile("CONB", [128, 896], BF16)
    VD = gtile("VD", [128, 2 + 4 * dep, KC], F32)
    VR = gtile("VR", [128, dep, 13, 6], F32)
    P.dma("sp", CON[:, :], consts[:, :], CON)
    P.dma("sp", VD[:, :, :], vecD[:, :, :], VD)
    P.dma("sp", VR[:, :, :, :], vecR[:, :, :, :], VR)
    P.copy(CONB[:, :], CON[:, 0:896])
    ID = CON[:, CO_ID:CO_ID + 128]

    def ident(n):
        return CON[:n, CO_ID:CO_ID + n]

    STG_N = 2816

    def mk_stg(st, n=3):
        st.stg = [st.tile([128, STG_N], F32) for _ in range(n)]
        st.stg_i = 0

    def wload(st, dst, src, np_, a, b, eng="pool", engs=None):
        step = max(1, STG_N // b)
        a0 = 0
        while a0 < a:
            a1 = min(a, a0 + step)
            stg = st.stg[st.stg_i % len(st.stg)]
            st.stg_i += 1
            view = VA(stg.t[:np_, 0:(a1 - a0) * b].rearrange("p (a b) -> p a b", a=a1 - a0), stg.res)
            P.dma("sp", view, src[:, a0:a1, :], stg, disjoint=False)
            P.copy(dst[:, a0:a1, :], view, eng=(engs[st.stg_i % len(engs)] if engs else eng))
            a0 = a1

    def pipelined(n, load, compute, depth=1):
        for i in range(min(depth, n)):
            load(i)
        for i in range(n):
            if i + depth < n:
                load(i + depth)
            compute(i)

    blocks = col_chunks()
    cache = {}
    NB_WIN = len(blocks)
    n_ffn = cfg.n_dense + cfg.n_moe * NE

    def cfam(name, nslots, elems):
        per = max(1, (96 << 20) // (128 * elems * 2))
        ts = [scr(f"C_{name}_{g}", [min(per, nslots - g * per), 128, elems], BF16)
              for g in range((nslots + per - 1) // per)]
        cache[name] = (per, ts)

    def cview(fam, slot, a, b):
        per, ts = cache[fam]
        t = ts[slot // per]
        return VA(t.t[slot % per, :, 0:a * b].rearrange("p (a b) -> p a b", a=a), t.res)

    cfam("win", dep * NB_WIN, KC * 512)
    cfam("pr", dep, 6 * D)
    cfam("pf", dep, 6 * D)
    cfam("pm", dep, 4 * D)
    cfam("wo", dep * 4, KC * 512)
    cfam("w1", n_ffn * 22, KC * 256)
    cfam("w3", n_ffn * 22, KC * 256)
    cfam("w2", n_ffn * 16, FC * 128)

    def ffn_idx(l, e_):
        return (l // 2) if l % 2 == 0 else cfg.n_dense + (l // 2) * NE + e_

    def prologue():
        with P.stage() as st:
            mk_stg(st, 4)
            WT = [st.tile([128, 6 * D], BF16) for _ in range(2)]
            k = [0]
            engs = ["pool", "act", "dve"]

            def fill(fam, slot, srcv, a, b):
                wt = WT[k[0] % 2]
                k[0] += 1
                dst = VA(wt.t[:, 0:a * b].rearrange("p (a b) -> p a b", a=a), wt.res)
                wload(st, dst, srcv, 128, a, b, engs=engs)
                P.dma("sp", cview(fam, slot, a, b), dst, wt)
            wv = w_in.view("l (kc p) n -> l p kc n", p=128)
            prv = p_r.view("l (kc p) n -> l p kc n", p=128)
            pfv = p_f.view("l (kc p) n -> l p kc n", p=128)
            pmv = p_m.view("l (kc p) n -> l p kc n", p=128)
            wov = w_o.view("l (kc p) n -> l p kc n", p=128)
            for l in range(dep):
                for bi_, blk in enumerate(blocks):
                    c0 = blk[0][0]
                    cw = blk[-1][0] + blk[-1][1] - c0
                    fill("win", l * NB_WIN + bi_, wv[l, :, :, c0:c0 + cw], KC, cw)
                fill("pr", l, prv[l], 6, D)
                fill("pf", l, pfv[l], 6, D)
                fill("pm", l, pmv[l], 4, D)
                for cb in range(4):
                    fill("wo", l * 4 + cb, wov[l, :, :, cb * 512:(cb + 1) * 512], KC, 512)
                moe = (l % 2 == 1)
                li = l // 2
                for e_ in range(NE if moe else 1):
                    if moe:
                        w1v = m_w1.view("l e (kc p) n -> l e p kc n", p=128)[li, e_]
                        w3v = m_w3.view("l e (kc p) n -> l e p kc n", p=128)[li, e_]
                        w2v = m_w2.view("l e (kc p) n -> l e p kc n", p=128)[li, e_]
                    else:
                        w1v = f_w1.view("l (kc p) n -> l p kc n", p=128)[li]
                        w3v = f_w3.view("l (kc p) n -> l p kc n", p=128)[li]
                        w2v = f_w2.view("l (kc p) n -> l p kc n", p=128)[li]
                    fi = ffn_idx(l, e_)
                    for cb in range(22):
                        fill("w1", fi * 22 + cb, w1v[:, :, cb * 256:(cb + 1) * 256], KC, 256)
                        fill("w3", fi * 22 + cb, w3v[:, :, cb * 256:(cb + 1) * 256], KC, 256)
                    for m in range(16):
                        fill("w2", fi * 16 + m, w2v[:, :, m * 128:(m + 1) * 128], FC, 128)

    def ln_stage(src_v, gi, bi, also_dbg=None):
        with P.stage() as st:
            NEW = [st.tile([128, KC, 512], F32) for _ in range(2)]
            SQ = st.tile([128, KC, 512], F32)
            OB = [st.tile([128, KC, 512], BF16) for _ in range(2)]
            mean = st.tile([128, 512], F32)
            rstd = st.tile([128, 512], F32)
            psm = st.psum()
            psq = st.psum()
            for bi_, (o, n) in enumerate(cfg.sbs):
                nw = NEW[bi_ % 2]
                ob = OB[bi_ % 2]
                P.dma("sp", nw[:, :, :n], src_v[:, :, o:o + n], nw)
                ln_core(st, nw, SQ, ob, mean, rstd, psm, psq, n, gi, bi)
                P.dma("sp", HT32v[:, :, o:o + n], nw[:, :, :n], nw)
                P.dma("sp", HTbv[:, :, o:o + n], ob[:, :, :n], ob)

    def ln_core(st, nw, SQ, ob, mean, rstd, psm, psq, n, gi, bi):
        MEANM = CON[:, CO_MEAN:CO_MEAN + 128]
        P.act(SQ[:, :, :n], nw[:, :, :n], AF.Square)
        for kc in range(KC):
            P.mm(psm[:, :n], MEANM, nw[:, kc, :n], start=(kc == 0), stop=(kc == KC - 1))
        for kc in range(KC):
            P.mm(psq[:, :n], MEANM, SQ[:, kc, :n], start=(kc == 0), stop=(kc == KC - 1))
        P.copy(mean[:, :n], psm[:, :n])
        P.tt(rstd[:, :n], mean[:, :n], mean[:, :n], ALU.mult)
        P.tt(rstd[:, :n], psq[:, :n], rstd[:, :n], ALU.subtract)
        P.ts(rstd[:, :n], rstd[:, :n], 1e-5, ALU.add)
        P.act(rstd[:, :n], rstd[:, :n], AF.Sqrt)
        P.recip(rstd[:, :n], rstd[:, :n])
        for kc in range(KC):
            P.tt(SQ[:, kc, :n], nw[:, kc, :n], mean[:, :n], ALU.subtract)
            P.tt(SQ[:, kc, :n], SQ[:, kc, :n], rstd[:, :n], ALU.mult)
            P.ts(nw[:, kc, :n], SQ[:, kc, :n], VD[:, gi, kc:kc + 1], ALU.mult, VD[:, bi, kc:kc + 1], ALU.add)
            P.copy(ob[:, kc, :n], nw[:, kc, :n], eng="pool")

    def stage_ln0(s):
        with P.stage() as st:
            XT = [st.tile([128, D], F32) for _ in range(2)]
            NEW = st.tile([128, KC, 128], F32)
            SQ = st.tile([128, KC, 128], F32)
            OB = st.tile([128, KC, 128], BF16)
            mean = st.tile([128, 128], F32)
            rstd = st.tile([128, 128], F32)
            pst = [st.psum() for _ in range(2)]
            psm = st.psum()
            psq = st.psum()
            for ti, (o, sz) in enumerate(cfg.tiles):
                xt = XT[ti % 2]
                if ti == 0:
                    P.dma("sp", xt[:sz, :], meta[:, :], xt)
                else:
                    P.dma("sp", xt[:sz, :], x_in[s, o - NMETA:o - NMETA + sz, :], xt)
                for g in range(4):
                    ps = pst[g % 2]
                    for j in range(4):
                        kc = g * 4 + j
                        P.tr(ps[:, j * 128:j * 128 + sz], xt[:sz, kc * 128:(kc + 1) * 128], ident(sz))
                    P.copy(NEW[:, g * 4:(g + 1) * 4, :sz],
                           va_re(ps[:, :], "p (a b) -> p a b", a=4)[:, :, :sz] if False else
                           VA(ps.t[:, :].rearrange("p (a b) -> p a b", a=4)[:, :, :sz], ps.res))
                ln_core(st, NEW, SQ, OB, mean, rstd, psm, psq, sz, 0, 1)
                P.dma("sp", HT32v[:, :, o:o + sz], NEW[:, :, :sz], NEW)
                P.dma("sp", HTbv[:, :, o:o + sz], OB[:, :, :sz], OB)

    blocks = col_chunks()

    def stage_win(l):
        wv = w_in.view("l (kc p) n -> l p kc n", p=128)
        with P.stage() as st:
            mk_stg(st, 3)
            XT = [st.tile([128, KC, 512], BF16) for _ in range(2)]
            W = [st.tile([128, KC, 512], BF16) for _ in range(3)]
            ZS = [st.tile([128, 512], F32) for _ in range(4)]
            PS = [st.psum() for _ in range(4)]
            jobs = [(bi_, o, n, blk, bj) for bi_, (o, n) in enumerate(cfg.sbs) for bj, blk in enumerate(blocks)]
            cnt = [0]

            def load(i):
                bi_, o, n, blk, bj = jobs[i]
                if bj == 0:
                    xt = XT[bi_ % 2]
                    P.dma("sp", xt[:, :, :n], HTbv[:, :, o:o + n], xt)
                c0 = blk[0][0]
                cw = blk[-1][0] + blk[-1][1] - c0
                w = W[i % 3]
                P.dma("sp", w[:, :, :cw], cview("win", l * NB_WIN + bj, KC, cw), w, disjoint=False)

            def compute(i):
                bi_, o, n, blk, bj = jobs[i]
                xt = XT[bi_ % 2]
                w = W[i % 3]
                c0 = blk[0][0]
                for (c, m) in blk:
                    ps = PS[cnt[0] % 4]
                    zs = ZS[cnt[0] % 4]
                    cnt[0] += 1
                    for kc in range(KC):
                        P.mm(ps[:m, :n], w[:, kc, c - c0:c - c0 + m], xt[:, kc, :n], start=(kc == 0),
                             stop=(kc == KC - 1))
                    P.act(zs[:m, :n], ps[:m, :n], AF.Sigmoid if c >= C3 else AF.Copy)
                    P.dma("sp", ZT[c:c + m, o:o + n], zs[:m, :n], zs)
            pipelined(len(jobs), load, compute, depth=2)

    def stage_m1(l):
        prv = p_r.view("l (kc p) n -> l p kc n", p=128)
        pfv = p_f.view("l (kc p) n -> l p kc n", p=128)
        pmv = p_m.view("l (kc p) n -> l p kc n", p=128)
        with P.stage() as st:
            PR = st.tile([128, 6, D], BF16)
            PF = st.tile([128, 6, D], BF16)
            PM = st.tile([128, 4, D], BF16)
            P.dma("sp", PR[:, :, :], cview("pr", l, 6, D), PR)
            P.dma("sp", PF[:, :, :], cview("pf", l, 6, D), PF)
            P.dma("sp", PM[:, :, :], cview("pm", l, 4, D), PM)
            Y = [st.tile([128, KC, 512], BF16) for _ in range(2)]
            G = [[st.tile([128, 512], F32) for _ in range(3)] for _ in range(2)]
            A1 = [st.tile([128, 512], F32) for _ in range(2)]
            A2 = [st.tile([128, 512], F32) for _ in range(2)]
            MO = [st.tile([128, 512], BF16) for _ in range(2)]
            PS = [[st.psum() for _ in range(3)] for _ in range(2)]
            it = 0
            for bi_, (o, n) in enumerate(cfg.sbs):
                y = Y[bi_ % 2]
                P.dma("sp", y[:, :, :n], YTv[:, :, o:o + n], y)
                for m in range(KC):
                    g3 = G[it % 2]
                    ps3 = PS[it % 2]
                    a1, a2, mo = A1[it % 2], A2[it % 2], MO[it % 2]
                    it += 1
                    for gi in range(3):
                        r0 = C3 + gi * D + m * 128
                        P.dma("sp", g3[gi][:, :n], ZT[r0:r0 + 128, o:o + n], g3[gi])
                    for kc in range(6):
                        P.mm(ps3[0][:, :n], PR[:, kc, m * 128:(m + 1) * 128], y[:, kc, :n], start=(kc == 0), stop=(kc == 5))
                    for kc in range(6):
                        P.mm(ps3[1][:, :n], PF[:, kc, m * 128:(m + 1) * 128], y[:, 6 + kc, :n], start=(kc == 0), stop=(kc == 5))
                    for kc in range(4):
                        P.mm(ps3[2][:, :n], PM[:, kc, m * 128:(m + 1) * 128], y[:, 12 + kc, :n], start=(kc == 0), stop=(kc == 3))
                    P.tt(a1[:, :n], ps3[0][:, :n], g3[0][:, :n], ALU.mult)
                    P.tt(a2[:, :n], ps3[1][:, :n], g3[1][:, :n], ALU.mult)
                    P.tt(a1[:, :n], a1[:, :n], a2[:, :n], ALU.add)
                    P.tt(a2[:, :n], ps3[2][:, :n], g3[2][:, :n], ALU.mult)
                    P.tt(mo[:, :n], a1[:, :n], a2[:, :n], ALU.add)
                    P.dma("sp", MIXT[m * 128:(m + 1) * 128, o:o + n], mo[:, :n], mo)

    def stage_proj_res(xv, nkc, wview, WB=512):
        with P.stage() as st:
            mk_stg(st, 3)
            XT = [st.tile([128, nkc, 512], BF16) for _ in range(2)]
            W = [st.tile([128, nkc, WB], BF16) for _ in range(2)]
            HR = [st.tile([128, 512], F32) for _ in range(3)]
            PS = [st.psum() for _ in range(3)]
            jobs = [(bi_, o, n, cb) for bi_, (o, n) in enumerate(cfg.sbs) for cb in range(D // WB)]
            it = [0]

            def load(i):
                bi_, o, n, cb = jobs[i]
                if cb == 0:
                    xt = XT[bi_ % 2]
                    P.dma("sp", xt[:, :, :n], xv[:, :, o:o + n], xt)
                w = W[i % 2]
                P.dma("sp", w[:, :, :], wview(cb), w, disjoint=False)

            def compute(i):
                bi_, o, n, cb = jobs[i]
                xt = XT[bi_ % 2]
                w = W[i % 2]
                for mm_ in range(WB // 128):
                    m = cb * (WB // 128) + mm_
                    ps = PS[it[0] % 3]
                    hr = HR[it[0] % 3]
                    it[0] += 1
                    P.dma("sp", hr[:, :n], HT32[m * 128:(m + 1) * 128, o:o + n], hr)
                    for kc in range(nkc):
                        P.mm(ps[:, :n], w[:, kc, mm_ * 128:(mm_ + 1) * 128], xt[:, kc, :n], start=(kc == 0),
                             stop=(kc == nkc - 1))
                    P.stt(hr[:, :n], hr[:, :n], ALPHA, ps[:, :n], ALU.mult, ALU.add)
                    P.dma("sp", NEWT[m * 128:(m + 1) * 128, o:o + n], hr[:, :n], hr)
            pipelined(len(jobs), load, compute, depth=1)

    def stage_m2(l):
        stage_proj_res(MIXTv, KC, lambda cb: cview("wo", l * 4 + cb, KC, 512))

    def stage_ffn(l):
        moe = (l % 2 == 1)
        li = l // 2
        nexp = NE if moe else 1
        with P.stage() as st:
            XT = st.tile([128, KC, 512], BF16)
            HID = st.tile([128, FC, 512], BF16)
            W1 = [st.tile([128, KC, 256], BF16) for _ in range(2)]
            W3 = [st.tile([128, KC, 256], BF16) for _ in range(2)]
            W2 = [st.tile([128, FC, 128], BF16) for _ in range(2)]
            SIL = [st.tile([128, 512], F32) for _ in range(2)]
            HR = [st.tile([128, 512], F32) for _ in range(2)]
            PA = [st.psum() for _ in range(2)]
            PB = [st.psum() for _ in range(2)]
            PO = [st.psum() for _ in range(2)]
            mk_stg(st, 2)
            if moe:
                ACC = st.tile([128, KC, 512], F32)
                GBC = st.tile([128, NE, 512], BF16)
                X32 = st.tile([128, KC, 128], F32)
                RWT = st.tile([128, KC, NE], F32)
                LG = st.tile([128, 8], F32)
                MX = st.tile([128, 8], F32)
                EX = st.tile([128, 8], F32)
                MK = st.tile([128, 8], F32)
                SC = st.tile([128, 4], F32)
                DG = st.tile([128, 128], F32)
                PR_ = st.psum()
                P.dma("sp", RWT[:, :, :], r_w.view("l (kc p) e -> l p kc e", p=128)[li], RWT)
            it = 0
            wi = 0
            w2i = 0
            for bi_, (o, n) in enumerate(cfg.sbs):
                P.dma("sp", XT[:, :, :n], HTbv[:, :, o:o + n], XT)
                if moe:
                    for t0 in range(0, n, 128):
                        tn = min(128, n - t0)
                        P.dma("sp", X32[:, :, :tn], HT32v[:, :, o + t0:o + t0 + tn], X32)
                        for kc in range(KC):
                            P.mm(PR_[:tn, 0:8], X32[:, kc, :tn], RWT[:, kc, :], start=(kc == 0), stop=(kc == KC - 1))
                        P.copy(LG[:tn, :], PR_[:tn, 0:8])
                        P.op("dve", lambda e: e.max(out=MX.t[:tn, :], in_=LG.t[:tn, :]), [LG], [MX], disjoint=True)
                        P.ts(MK[:tn, :], LG[:tn, :], MX[:tn, 1:2], ALU.is_ge)
                        P.ts(SC[:tn, 0:1], MX[:tn, 0:1], -1.0, ALU.mult)
                        P.act(EX[:tn, :], LG[:tn, :], AF.Exp, bias=SC[:tn, 0:1])
                        P.tt(EX[:tn, :], EX[:tn, :], MK[:tn, :], ALU.mult)
                        P.op("dve", lambda e: e.reduce_sum(out=SC.t[:tn, 1:2], in_=EX.t[:tn, :], axis=AX.X), [EX], [SC],
                             disjoint=True)
                        P.recip(SC[:tn, 2:3], SC[:tn, 1:2])
                        P.ts(EX[:tn, :], EX[:tn, :], SC[:tn, 2:3], ALU.mult)
                        for e_ in range(NE):
                            P.ts(DG[:tn, :tn], CON[:tn, CO_ID:CO_ID + tn], EX[:tn, e_:e_ + 1], ALU.mult)
                            P.mm(PR_[:, 128:128 + tn], CON[:tn, CO_ONE:CO_ONE + 128], DG[:tn, :tn])
                            P.copy(GBC[:, e_, t0:t0 + tn], PR_[:, 128:128 + tn])
                for e_ in range(nexp):
                    if moe:
                        w1v = m_w1.view("l e (kc p) n -> l e p kc n", p=128)[li, e_]
                        w3v = m_w3.view("l e (kc p) n -> l e p kc n", p=128)[li, e_]
                        w2v = m_w2.view("l e (kc p) n -> l e p kc n", p=128)[li, e_]
                    else:
                        w1v = f_w1.view("l (kc p) n -> l p kc n", p=128)[li]
                        w3v = f_w3.view("l (kc p) n -> l p kc n", p=128)[li]
                        w2v = f_w2.view("l (kc p) n -> l p kc n", p=128)[li]
                    for cb in range(DFF // 256):
                        w1 = W1[wi % 2]
                        w3 = W3[wi % 2]
                        wi += 1
                        P.dma("sp", w1[:, :, :], cview("w1", ffn_idx(l, e_) * 22 + cb, KC, 256), w1, disjoint=False)
                        P.dma("sp", w3[:, :, :], cview("w3", ffn_idx(l, e_) * 22 + cb, KC, 256), w3, disjoint=False)
                        for j in range(2):
                            f = cb * 2 + j
                            pa, pb, sil = PA[it % 2], PB[it % 2], SIL[it % 2]
                            it += 1
                            for kc in range(KC):
                                P.mm(pa[:, :n], w1[:, kc, j * 128:(j + 1) * 128], XT[:, kc, :n], start=(kc == 0), stop=(kc == KC - 1))
                            for kc in range(KC):
                                P.mm(pb[:, :n], w3[:, kc, j * 128:(j + 1) * 128], XT[:, kc, :n], start=(kc == 0), stop=(kc == KC - 1))
                            P.act(sil[:, :n], pa[:, :n], AF.Silu)
                            if moe:
                                P.tt(sil[:, :n], sil[:, :n], pb[:, :n], ALU.mult)
                                P.tt(HID[:, f, :n], sil[:, :n], GBC[:, e_, :n], ALU.mult)
                            else:
                                P.tt(HID[:, f, :n], sil[:, :n], pb[:, :n], ALU.mult)
                    for m in range(KC):
                        w2 = W2[w2i % 2]
                        po = PO[w2i % 2]
                        hr = HR[w2i % 2]
                        w2i += 1
                        P.dma("sp", w2[:, :, :], cview("w2", ffn_idx(l, e_) * 16 + m, FC, 128), w2, disjoint=False)
                        for f in range(FC):
                            P.mm(po[:, :n], w2[:, f, :], HID[:, f, :n], start=(f == 0), stop=(f == FC - 1))
                        if moe and e_ > 0:
                            P.tt(ACC[:, m, :n], ACC[:, m, :n], po[:, :n], ALU.add)
                        elif moe:
                            P.copy(ACC[:, m, :n], po[:, :n])
                        if (not moe) or e_ == nexp - 1:
                            P.dma("sp", hr[:, :n], HT32[m * 128:(m + 1) * 128, o:o + n], hr)
                            src_ = ACC[:, m, :n] if moe else po[:, :n]
                            P.stt(hr[:, :n], hr[:, :n], ALPHA, src_, ALU.mult, ALU.add)
                            P.dma("sp", NEWT[m * 128:(m + 1) * 128, o:o + n], hr[:, :n], hr)

    def stage_out(s):
        with P.stage() as st:
            HTt = [st.tile([128, KC, 128], F32) for _ in range(2)]
            OT = [st.tile([128, D], F32) for _ in range(2)]
            PS = [st.psum() for _ in range(2)]
            for ti, (o, sz) in enumerate(cfg.tiles[1:]):
                ht = HTt[ti % 2]
                ot = OT[ti % 2]
                P.dma("sp", ht[:, :, :], HT32v[:, :, o:o + sz], ht)
                for g in range(4):
                    ps = PS[g % 2]
                    for j in range(4):
                        kc = g * 4 + j
                        P.tr(ps[:, j * 128:(j + 1) * 128], ht[:, kc, :], ident(128))
                    P.copy(ot[:, g * 512:(g + 1) * 512], ps[:, :])
                P.dma("sp", out_t[s, o - NMETA:o - NMETA + sz, :], ot[:, :], ot)

    def stage_fox(l):
        tl = cfg.tiles
        with P.stage() as st:
            FG = st.tile([12, L], F32)
            CUM = st.tile([12, L], F32)
            ONE = st.tile([12, L], F32)
            NB = st.tile([12, 2], F32)
            ENDS = st.tile([12, NTl], F32)
            Q = st.tile([128, 6, L], BF16)
            Kt = st.tile([128, 6, L], BF16)
            VP = st.tile([128, NTl, 12, 65], BF16)
            CREF = st.tile([128, 12, NTl], F32)
            CUMK = st.tile([128, NTl, 12], F32)
            BIAS = st.tile([128, NTl, 12, NTl], F32)
            VT = [st.tile([128, 6, 128], F32) for _ in range(2)]
            PT = [st.tile([128, 128], BF16) for _ in range(3)]
            YTK = [st.tile([128, FW], F32) for _ in range(2)]
            RC = [st.tile([128, 1], F32) for _ in range(2)]
            YF = [st.tile([128, 6, 128], BF16) for _ in range(2)]
            PS_S = [st.psum() for _ in range(3)]
            PS_O = [st.psum() for _ in range(2)]
            PS_T = [st.psum() for _ in range(2)]
            zq = ZT.view("(c p) l -> p c l", p=128) if False else None
            P.dma("sp", FG[:, :], ZT[C1 + 2304:C1 + 2316, :], FG)
            P.dma("sp", NB[:, 0:1], foxbf[l], NB)
            P.ts(NB[:, 1:2], NB[:, 0:1], -1.0, ALU.mult)
            P.memset(ONE[:, :], 1.0)
            P.act(FG[:, :], FG[:, :], AF.Exp, bias=NB[:, 1:2], scale=-1.0)
            P.act(FG[:, :], FG[:, :], AF.Ln, bias=1.0)
            P.ts(FG[:, :], FG[:, :], -1.0, ALU.mult)
            P.scan(CUM[:, :], ONE[:, :], FG[:, :], 0.0, ALU.mult, ALU.add)
            qv = VA(ZT.t[C1:C1 + 768, :].rearrange("(c p) l -> p c l", p=128), ZT.res)
            kv = VA(ZT.t[C1 + 768:C1 + 1536, :].rearrange("(c p) l -> p c l", p=128), ZT.res)
            vv = VA(ZT.t[C1 + 1536:C1 + 2304, :].rearrange("(c p) l -> p c l", p=128), ZT.res)
            mk_stg(st, 2)
            wload(st, Q[:, :, :], qv, 128, 6, L)
            wload(st, Kt[:, :, :], kv, 128, 6, L)
            P.memset(VP[:, :, :, :], 1.0, eng="pool")
            for j, (o, sz) in enumerate(tl):
                P.copy(ENDS[:, j:j + 1], CUM[:, o + sz // 2 - 1:o + sz // 2])
                ps = PS_T[j % 2]
                P.tr(ps[:sz, 0:12], CUM[:, o:o + sz], ident(12))
                P.copy(CUMK[:sz, j, :], ps[:sz, 0:12])
                vt = VT[j % 2]
                P.dma("sp", vt[:, :, :sz], vv[:, :, o:o + sz], vt)
                for c in range(6):
                    ps2 = PS_S[c % 3]
                    P.tr(ps2[:sz, 0:128], vt[:, c, :sz], ident(128))
                    P.copy(VP[:sz, j, 2 * c:2 * c + 2, 0:64],
                           VA(ps2.t[:sz, 0:128].rearrange("p (a b) -> p a b", a=2), ps2.res))
            psc = PS_O[0]
            for h in range(12):
                P.mm(psc[:, h * NTl:(h + 1) * NTl], CON[:12, CO_SEL + h * 128:CO_SEL + (h + 1) * 128], ENDS[:, :])
            P.copy(CREF[:, :, :], VA(psc.t[:, 0:12 * NTl].rearrange("p (a b) -> p a b", a=12), psc.res))
            for j, (o, sz) in enumerate(tl):
                for h in range(12):
                    P.ts(BIAS[:sz, j, h, :], CREF[:sz, h, :], CUMK[:sz, j, h:h + 1], ALU.subtract)
            it = 0
            for i, (oi, si) in enumerate(tl):
                ytk = YTK[i % 2]
                for h in range(12):
                    c, pr = h // 2, (h % 2) * 64
                    po = PS_O[h % 2]
                    rc = RC[h % 2]
                    for j in range(i + 1):
                        oj, sj = tl[j]
                        ps = PS_S[it % 3]
                        pt = PT[it % 3]
                        it += 1
                        P.mm(ps[:sj, :si], Kt[pr:pr + 64, c, oj:oj + sj], Q[pr:pr + 64, c, oi:oi + si])
                        P.act(pt[:sj, :si], ps[:sj, :si], AF.Exp, bias=BIAS[:sj, j, h, i:i + 1], scale=0.125)
                        if j == i:
                            P.tt(pt[:sj, :si], pt[:sj, :si], CONB[:sj, CO_MLE:CO_MLE + si], ALU.mult, eng="pool")
                        P.mm(po[:si, 0:65], pt[:sj, :si], VP[:sj, j, h, :], start=(j == 0), stop=(j == i))
                    P.recip(rc[:si, :], po[:si, 64:65])
                    P.ts(ytk[:si, h * 64:(h + 1) * 64], po[:si, 0:64], rc[:si, 0:1], ALU.mult)
                yf = YF[i % 2]
                for c in range(6):
                    ps = PS_T[c % 2]
                    P.tr(ps[:, :si], ytk[:si, c * 128:(c + 1) * 128], ident(si))
                    P.copy(yf[:, c, :si], ps[:, :si], eng="act")
                P.dma("sp", YTv[:, 6:12, oi:oi + si], yf[:, :, :si], yf)

    def stage_mlstm(l):
        tl = cfg.tiles
        SCL = 128 ** -0.5
        with P.stage() as st:
            XP = [st.tile([128, 3 + L], F32) for _ in range(2)]
            ACC = st.tile([128, L], F32)
            QT = st.tile([128, 4, L], BF16)
            KT = st.tile([128, 4, L], BF16)
            KTOK = st.tile([128, NTl, 4, 128], BF16)
            VP = st.tile([128, NTl, 4, 129], BF16)
            CW = st.tile([128, 8, 4], F32)
            IG = st.tile([4, L], F32)
            FGm = st.tile([4, L], F32)
            ONE = st.tile([4, L], F32)
            Bc = st.tile([4, L], F32)
            WK = st.tile([4, L], F32)
            EB = st.tile([4, L], F32)
            MB = st.tile([4, 4], F32)
            ENDE = st.tile([4, NTl], F32)
            GT = st.tile([128, NTl, 8], F32)
            EBE = st.tile([128, 4, NTl], F32)
            CN = st.tile([128, 4, 129], F32)
            CNB = st.tile([128, 4, 129], BF16)
            VT = [st.tile([128, 4, 128], F32) for _ in range(2)]
            OG = [st.tile([128, 4, 128], F32) for _ in range(2)]
            GM = [st.tile([128, 128], BF16) for _ in range(2)]
            HTK = [st.tile([128, MW], F32) for _ in range(2)]
            SC = [st.tile([128, 4], F32) for _ in range(2)]
            YM = [st.tile([128, 4, 128], BF16) for _ in range(2)]
            PS_G = [st.psum() for _ in range(2)]
            PS_O = [st.psum() for _ in range(2)]
            PS_C = st.psum()
            PS_T = [st.psum() for _ in range(2)]
            PS_TB = st.psum([128, 1024], BF16)
            P.dma("sp", CW[:, :, :], convw[:, l, :, :], CW)
            P.dma("sp", MB[:, 0:2], mlb[l], MB)
            P.ts(MB[:, 2:3], MB[:, 1:2], -1.0, ALU.mult)
            P.dma("sp", IG[:, :], ZT[C2 + 2048:C2 + 2052, :], IG)
            P.dma("sp", FGm[:, :], ZT[C2 + 2052:C2 + 2056, :], FGm)
            P.memset(ONE[:, :], 1.0)
            P.act(FGm[:, :], FGm[:, :], AF.Exp, bias=MB[:, 2:3], scale=-1.0)
            P.act(FGm[:, :], FGm[:, :], AF.Ln, bias=1.0)
            P.ts(FGm[:, :], FGm[:, :], -1.0, ALU.mult)
            for j, (o, sz) in enumerate(tl):
                P.scan(Bc[:, o:o + sz], ONE[:, o:o + sz], FGm[:, o:o + sz], 0.0, ALU.mult, ALU.add)
                P.copy(ENDE[:, j:j + 1], Bc[:, o + sz - 1:o + sz])
            P.act(ENDE[:, :], ENDE[:, :], AF.Exp)
            P.tt(WK[:, :], IG[:, :], Bc[:, :], ALU.subtract)
            P.act(WK[:, :], WK[:, :], AF.Exp, bias=MB[:, 0:1])
            P.act(EB[:, :], Bc[:, :], AF.Exp)
            for h in range(4):
                P.mm(PS_C[:, h * NTl:(h + 1) * NTl], CON[:4, CO_SEL + h * 128:CO_SEL + (h + 1) * 128], ENDE[:, :])
            P.copy(EBE[:, :, :], VA(PS_C.t[:, 0:4 * NTl].rearrange("p (a b) -> p a b", a=4), PS_C.res))
            for c in range(8):
                xp = XP[c % 2]
                P.memset(xp[:, 0:3], 0.0)
                P.dma("sp", xp[:, 3:3 + L], ZT[C2 + c * 128:C2 + (c + 1) * 128, :], xp)
                P.ts(ACC[:, :], xp[:, 0:L], CW[:, c, 0:1], ALU.mult)
                for jj in range(1, 4):
                    P.stt(ACC[:, :], xp[:, jj:jj + L], CW[:, c, jj:jj + 1], ACC[:, :], ALU.mult, ALU.add)
                if c < 4:
                    P.act(QT[:, c, :], ACC[:, :], AF.Silu)
                else:
                    P.act(ACC[:, :], ACC[:, :], AF.Silu)
                    P.ts(KT[:, c - 4, :], ACC[:, :], SCL, ALU.mult)
            vv = VA(ZT.t[C2 + 1024:C2 + 1536, :].rearrange("(c p) l -> p c l", p=128), ZT.res)
            ov = VA(ZT.t[C2 + 1536:C2 + 2048, :].rearrange("(c p) l -> p c l", p=128), ZT.res)
            for j, (o, sz) in enumerate(tl):
                pt = PS_T[j % 2]
                P.tr(pt[:sz, 0:4], WK[:, o:o + sz], ident(4))
                P.tr(pt[:sz, 4:8], EB[:, o:o + sz], ident(4))
                P.copy(GT[:sz, j, :], pt[:sz, 0:8])
                vt = VT[j % 2]
                P.dma("sp", vt[:, :, :sz], vv[:, :, o:o + sz], vt)
                for h in range(4):
                    P.tr(pt[:sz, 128 * (h % 2) + 128:128 * (h % 2) + 256], vt[:, h, :sz], ident(128))
                    P.ts(VP[:sz, j, h, 0:128], pt[:sz, 128 * (h % 2) + 128:128 * (h % 2) + 256], GT[:sz, j, h:h + 1], ALU.mult)
                    P.copy(VP[:sz, j, h, 128:129], GT[:sz, j, h:h + 1])
                    P.tr(PS_TB[:sz, h * 128:(h + 1) * 128], KT[:, h, o:o + sz], CONB[:, CO_ID:CO_ID + 128])
                P.copy(KTOK[:sz, j, :, :], VA(PS_TB.t[:sz, 0:512].rearrange("p (a b) -> p a b", a=4), PS_TB.res))
            for j, (o, sz) in enumerate(tl):
                htk = HTK[j % 2]
                sc = SC[j % 2]
                og = OG[j % 2]
                P.dma("sp", og[:, :, :sz], ov[:, :, o:o + sz], og)
                P.act(og[:, :, :sz], og[:, :, :sz], AF.Sigmoid)
                for h in range(4):
                    pg = PS_G[h % 2]
                    po = PS_O[h % 2]
                    gm = GM[h % 2]
                    P.mm(pg[:sz, :sz], KT[:, h, o:o + sz], QT[:, h, o:o + sz])
                    P.tt(gm[:sz, :sz], pg[:sz, :sz], CON[:sz, CO_MLE:CO_MLE + sz], ALU.mult)
                    P.mm(po[:sz, 0:129], gm[:sz, :sz], VP[:sz, j, h, :], start=True, stop=(j == 0))
                    if j > 0:
                        P.mm(po[:sz, 0:129], QT[:, h, o:o + sz], CNB[:, h, :], start=False, stop=True)
                    P.tt(sc[:sz, 0:1], po[:sz, 128:129], GT[:sz, j, 4 + h:5 + h], ALU.mult)
                    P.ts(sc[:sz, 1:2], sc[:sz, 0:1], -1.0, ALU.mult)
                    P.tt(sc[:sz, 1:2], sc[:sz, 1:2], sc[:sz, 0:1], ALU.max)
                    P.ts(sc[:sz, 1:2], sc[:sz, 1:2], 1.0, ALU.max)
                    P.recip(sc[:sz, 2:3], sc[:sz, 1:2])
                    P.tt(sc[:sz, 3:4], sc[:sz, 2:3], GT[:sz, j, 4 + h:5 + h], ALU.mult)
                    P.ts(htk[:sz, h * 128:(h + 1) * 128], po[:sz, 0:128], sc[:sz, 3:4], ALU.mult)
                    P.mm(PS_C[:, 0:129], KTOK[:sz, j, h, :], VP[:sz, j, h, :])
                    if j == 0:
                        P.ts(CN[:, h, :], PS_C[:, 0:129], EBE[:, h, j:j + 1], ALU.mult)
                    else:
                        P.tt(CN[:, h, :], CN[:, h, :], PS_C[:, 0:129], ALU.add)
                        P.ts(CN[:, h, :], CN[:, h, :], EBE[:, h, j:j + 1], ALU.mult)
                    P.copy(CNB[:, h, :], CN[:, h, :])
                ym = YM[j % 2]
                for h in range(4):
                    pt = PS_T[h % 2]
                    P.tr(pt[:, :sz], htk[:sz, h * 128:(h + 1) * 128], ident(sz))
                    P.tt(ym[:, h, :sz], pt[:, :sz], og[:, h, :sz], ALU.mult)
                P.dma("sp", YTv[:, 12:16, o:o + sz], ym[:, :, :sz], ym)

    def stage_rwkv(l, part=0, nlev_max=99, sub=99):
        tl = cfg.tiles
        BLK = CON[:, CO_BLK:CO_BLK + 128]
        RWSv = RWS.view("q (c p) l -> p q c l", p=128)
        with P.stage() as st:
            W2 = st.tile([96, RW], BF16)
            A2 = st.tile([96, RW], BF16)
            G2 = st.tile([128, 2, RW], BF16)
            mk_stg(st, 2)
            wload(st, VA(W2.t[:, :].rearrange("p (a b) -> p a b", a=1), W2.res),
                  VA(rw2.t[l].rearrange("p (a b) -> p a b", a=1), rw2.res), 96, 1, RW)
            wload(st, VA(A2.t[:, :].rearrange("p (a b) -> p a b", a=1), A2.res),
                  VA(ra2.t[l].rearrange("p (a b) -> p a b", a=1), ra2.res), 96, 1, RW)
            wload(st, G2[:, :, :], rg2.view("l (kc p) n -> l p kc n", p=128)[l], 128, 2, RW)
            ZP = [st.tile([128, 6, 513], F32) for _ in range(3)]
            ZL = [st.tile([128, 513], F32) for _ in range(4)]
            TMP = st.tile([128, 512], F32)
            XR = st.tile([128, 6, 512], F32)
            XK = st.tile([128, 6, 512], F32)
            XV = st.tile([128, 6, 512], F32)
            LW = st.tile([128, 6, 512], F32)
            AA = st.tile([128, 6, 512], F32)
            GG = st.tile([128, 6, 512], F32)
            KK = st.tile([128, 6, 512], F32)
            BB = st.tile([128, 6, 512], F32)
            BON = st.tile([128, 6, 512], F32)
            TW = st.tile([96, 512], BF16)
            TA = st.tile([96, 512], BF16)
            SG = st.tile([128, 2, 512], BF16)
            PS = [st.psum() for _ in range(4)]
            pi = 0
            for (o, n) in cfg.sbs:
                def load_shift(zp_slice, rows0, nrows, mu_col, out):
                    pass
                for qi, (r0, X) in enumerate([(0, XR), (768, XK), (1536, XV)]):
                    zp = ZP[qi]
                    src_ = VA(ZT.t[r0:r0 + 768, :].rearrange("(c p) l -> p c l", p=128), ZT.res)
                    if o == 0:
                        P.memset(zp[:, :, 0:1], 0.0)
                        P.dma("sp", zp[:, :, 1:1 + n], src_[:, :, 0:n], zp)
                    else:
                        P.dma("sp", zp[:, :, 0:1 + n], src_[:, :, o - 1:o + n], zp, disjoint=False)
                    for c in range(6):
                        P.tt(TMP[:, :n], zp[:, c, 0:n], zp[:, c, 1:1 + n], ALU.subtract)
                        P.stt(X[:, c, :n], TMP[:, :n], VR[:, l, qi, c:c + 1], zp[:, c, 1:1 + n], ALU.mult, ALU.add)
                for qi, (r0, nr, vi, vc) in enumerate([(2304, 96, 10, 0), (2400, 96, 11, 0), (2496, 128, 12, 0), (2624, 128, 12, 1)]):
                    zl = ZL[qi]
                    if o == 0:
                        P.memset(zl[:nr, 0:1], 0.0)
                        P.dma("sp", zl[:nr, 1:1 + n], ZT[r0:r0 + nr, 0:n], zl)
                    else:
                        P.dma("sp", zl[:nr, 0:1 + n], ZT[r0:r0 + nr, o - 1:o + n], zl, disjoint=False)
                    P.tt(TMP[:nr, :n], zl[:nr, 0:n], zl[:nr, 1:1 + n], ALU.subtract)
                    P.stt(TMP[:nr, :n], TMP[:nr, :n], VR[:nr, l, vi, vc:vc + 1], zl[:nr, 1:1 + n], ALU.mult, ALU.add)
                    if qi == 0:
                        P.act(TW[:, :n], TMP[:96, :n], AF.Tanh)
                    elif qi == 1:
                        P.copy(TA[:, :n], TMP[:96, :n])
                    else:
                        P.act(SG[:, qi - 2, :n], TMP[:, :n], AF.Sigmoid)
                for c in range(6):
                    cs = slice(c * 128, (c + 1) * 128)
                    ps = PS[pi % 4]; pi += 1
                    P.mm(ps[:, :n], W2[:, cs], TW[:, :n])
                    P.act(LW[:, c, :n], ps[:, :n], AF.Sigmoid, bias=VR[:, l, 3, c:c + 1])
                    P.ts(LW[:, c, :n], LW[:, c, :n], -0.6065306597126334, ALU.mult)
                    ps = PS[pi % 4]; pi += 1
                    P.mm(ps[:, :n], A2[:, cs], TA[:, :n])
                    P.act(AA[:, c, :n], ps[:, :n], AF.Sigmoid, bias=VR[:, l, 4, c:c + 1])
                    ps = PS[pi % 4]; pi += 1
                    P.mm(ps[:, :n], G2[:, 0, cs], SG[:, 0, :n], start=True, stop=False)
                    P.mm(ps[:, :n], G2[:, 1, cs], SG[:, 1, :n], start=False, stop=True)
                    P.copy(GG[:, c, :n], ps[:, :n], eng="act")
                    P.ts(KK[:, c, :n], XK[:, c, :n], VR[:, l, 5, c:c + 1], ALU.mult)
                    P.tt(TMP[:, :n], KK[:, c, :n], KK[:, c, :n], ALU.mult)
                    ps = PS[pi % 4]; pi += 1
                    P.mm(ps[:, :n], BLK, TMP[:, :n])
                    P.act(TMP[:, :n], ps[:, :n], AF.Sqrt)
                    P.ts(TMP[:, :n], TMP[:, :n], 1e-12, ALU.max)
                    P.recip(TMP[:, :n], TMP[:, :n])
                    P.tt(KK[:, c, :n], KK[:, c, :n], TMP[:, :n], ALU.mult)
                    P.ts(TMP[:, :n], AA[:, c, :n], -1.0, ALU.add, VR[:, l, 6, c:c + 1], ALU.mult)
                    P.ts(TMP[:, :n], TMP[:, :n], 1.0, ALU.add)
                    P.tt(XK[:, c, :n], XK[:, c, :n], TMP[:, :n], ALU.mult)
                    P.tt(BB[:, c, :n], AA[:, c, :n], KK[:, c, :n], ALU.mult)
                    P.tt(TMP[:, :n], XR[:, c, :n], XK[:, c, :n], ALU.mult)
                    P.ts(TMP[:, :n], TMP[:, :n], VR[:, l, 7, c:c + 1], ALU.mult)
                    ps = PS[pi % 4]; pi += 1
                    P.mm(ps[:, :n], BLK, TMP[:, :n])
                    P.tt(BON[:, c, :n], ps[:, :n], XV[:, c, :n], ALU.mult)
                for qi, X in enumerate([XR, XK, XV, LW, KK, BB, BON, GG]):
                    P.dma("sp", RWSv[:, qi, :, o:o + n], X[:, :, :n], X)
        if part == 1:
            return
        with P.stage() as st:
            X8 = [st.tile([128, 8, 128], F32) for _ in range(2)]
            ONE = st.tile([128, 128], F32)
            CWt = st.tile([128, 128], F32)
            CWX = st.tile([128, 128], F32)
            EW = st.tile([128, 3, 128], F32)
            QS = st.tile([128, 5, 128], BF16)
            TOK = st.tile([128, 3, 128], BF16)
            SST = st.tile([128, 6, 64], F32)
            SSTB = st.tile([128, 6, 64], BF16)
            AM = [st.tile([128, 5, 128], BF16) for _ in range(2)]
            PW = [st.tile([128, 2, 128], BF16) for _ in range(2)]
            UU = st.tile([128, 128], F32)
            UB = st.tile([128, 128], BF16)
            PTB = st.psum([128, 1024], BF16)
            OTOK = st.tile([128, 128], F32)
            OT = st.tile([128, 128], F32)
            T1 = st.tile([128, 128], F32)
            T2 = st.tile([128, 128], F32)
            YR = [st.tile([128, 128], BF16) for _ in range(2)]
            PA = [st.psum() for _ in range(3)]
            PB = [st.psum() for _ in range(2)]
            PC = [st.psum() for _ in range(2)]
            P.memset(ONE[:, :], 1.0)
            P.memset(SST[:, :, :], 0.0)
            P.memset(SSTB[:, :, :], 0.0)
            MLE = CON[:, CO_MLE:CO_MLE + 128]
            MLT = CON[:, CO_MLT:CO_MLT + 128]
            MGE = CON[:, CO_MGE:CO_MGE + 128]
            it = 0
            for i, (o, sz) in enumerate(tl):
                nlev = min(int(np.ceil(np.log2(sz))), nlev_max)
                if nlev_max == 98 and i == 0:
                    continue
                for c in range(6):
                    x8 = X8[it % 2]
                    am = AM[it % 2]
                    yr = YR[it % 2]
                    it += 1
                    P.dma("sp", x8[:, :, :sz], RWSv[:, :, c, o:o + sz], x8)
                    R_, K_, V_, LW_, KK_, B_, BON_, G_ = [x8[:, q, :sz] for q in range(8)]
                    P.scan(CWt[:, :sz], ONE[:, :sz], LW_, 0.0, ALU.mult, ALU.add)
                    P.tt(CWX[:, :sz], CWt[:, :sz], LW_, ALU.subtract)
                    P.act(EW[:, 0, :sz], CWt[:, :sz], AF.Exp)
                    P.act(EW[:, 1, :sz], CWt[:, :sz], AF.Exp, scale=-1.0)
                    P.act(EW[:, 2, :sz], CWX[:, :sz], AF.Exp)
                    P.stt(QS[:, 0, :sz], KK_, -1.0, EW[:, 2, :sz], ALU.mult, ALU.mult)
                    P.tt(QS[:, 1, :sz], R_, EW[:, 0, :sz], ALU.mult)
                    P.tt(QS[:, 2, :sz], B_, EW[:, 1, :sz], ALU.mult)
                    P.tt(QS[:, 3, :sz], K_, EW[:, 1, :sz], ALU.mult)
                    P.copy(QS[:, 4, :sz], V_, eng="act")
                    IDB = CONB[:, CO_ID:CO_ID + 128]
                    P.tr(PTB[:sz, 0:128], QS[:, 4, :sz], IDB)
                    P.tr(PTB[:sz, 128:256], QS[:, 2, :sz], IDB)
                    P.tr(PTB[:sz, 256:384], QS[:, 3, :sz], IDB)
                    P.copy(TOK[:sz, :, :], VA(PTB.t[:sz, 0:384].rearrange("p (a b) -> p a b", a=3), PTB.res))
                    if sub == 0:
                        continue
                    for hh in range(2):
                        pr = slice(hh * 64, hh * 64 + 64)
                        AT = QS[pr, 0, :sz]
                        RT = QS[pr, 1, :sz]
                        BT = QS[pr, 2, :sz]
                        KT_ = QS[pr, 3, :sz]
                        p1, p2, p3 = PA[1], PA[2], PB[0]
                        P.mm(p1[:sz, 0:sz], BT, AT)
                        P.mm(p1[:sz, 128:128 + sz], BT, RT)
                        P.mm(p2[:sz, 0:sz], KT_, AT)
                        P.mm(p2[:sz, 128:128 + sz], KT_, RT)
                        P.mm(p3[:sz, 0:sz], AT, BT)
                        P.tt(am[:sz, 0, :sz], p1[:sz, 0:sz], MLT[:sz, :sz], ALU.mult)
                        P.tt(am[:sz, 1, :sz], p1[:sz, 128:128 + sz], MLE[:sz, :sz], ALU.mult)
                        P.tt(am[:sz, 2, :sz], p2[:sz, 0:sz], MLT[:sz, :sz], ALU.mult)
                        P.tt(am[:sz, 3, :sz], p2[:sz, 128:128 + sz], MLE[:sz, :sz], ALU.mult)
                        P.tt(am[:sz, 4, :sz], p3[:sz, 0:sz], MGE[:sz, :sz], ALU.mult)
                        if sub == 1:
                            continue
                        p4 = PB[1]
                        P.mm(p4[:sz, 0:64], AT, SSTB[pr, c, :], start=True, stop=False)
                        P.mm(p4[:sz, 0:64], am[:sz, 2, :sz], TOK[:sz, 0, pr], start=False, stop=True)
                        U = UU[:sz, pr]
                        Ub = UB[:sz, pr]
                        P.copy(U, p4[:sz, 0:64])
                        P.copy(Ub, p4[:sz, 0:64], eng="act")
                        Pm, PTm = am[:sz, 4, :sz], am[:sz, 0, :sz]
                        for lev in range(nlev):
                            pu = PC[lev % 2]
                            P.mm(pu[:sz, 0:64], PTm, Ub)
                            if lev < nlev - 1:
                                pw = PW[lev % 2]
                                pq, pq2 = PB[0], PB[1]
                                P.mm(pq[:sz, 0:sz], PTm, Pm)
                                P.mm(pq2[:sz, 0:sz], Pm, PTm)
                            P.tt(U, U, pu[:sz, 0:64], ALU.add)
                            P.copy(Ub, U, eng="act")
                            if lev < nlev - 1:
                                P.copy(pw[:sz, 0, :sz], pq[:sz, 0:sz])
                                P.copy(pw[:sz, 1, :sz], pq2[:sz, 0:sz], eng="act")
                                Pm, PTm = pw[:sz, 0, :sz], pw[:sz, 1, :sz]
                        if sub == 2:
                            continue
                        po = PC[0]
                        P.mm(po[:sz, 64:128], RT, SSTB[pr, c, :], start=True, stop=False)
                        P.mm(po[:sz, 64:128], am[:sz, 1, :sz], Ub, start=False, stop=False)
                        P.mm(po[:sz, 64:128], am[:sz, 3, :sz], TOK[:sz, 0, pr], start=False, stop=True)
                        P.copy(OTOK[:sz, pr], po[:sz, 64:128])
                    if sub in (1, 2, 3):
                        continue
                    psu = PA[1]
                    P.mm(psu[:, 0:128], TOK[:sz, 1, :], UB[:sz, :], start=True, stop=False)
                    P.mm(psu[:, 0:128], TOK[:sz, 2, :], TOK[:sz, 0, :], start=False, stop=True)
                    for hh in range(2):
                        pr = slice(hh * 64, hh * 64 + 64)
                        P.tt(SST[pr, c, :], SST[pr, c, :], psu[pr, pr], ALU.add)
                        P.ts(SST[pr, c, :], SST[pr, c, :], EW[pr, 0, sz - 1:sz], ALU.mult)
                        P.copy(SSTB[pr, c, :], SST[pr, c, :], eng="act")
                    if sub == 4:
                        continue
                    ptt = PA[2]
                    P.tr(ptt[:, 0:sz], OTOK[:sz, :], ident(sz))
                    P.copy(OT[:, :sz], ptt[:, 0:sz])
                    P.tt(T1[:, :sz], OT[:, :sz], OT[:, :sz], ALU.mult)
                    pmn = PB[0]
                    P.mm(pmn[:, 0:sz], BLK, OT[:, :sz])
                    P.mm(pmn[:, 128:128 + sz], BLK, T1[:, :sz])
                    P.ts(T1[:, :sz], pmn[:, 0:sz], 1.0 / 64, ALU.mult)
                    P.tt(T2[:, :sz], T1[:, :sz], T1[:, :sz], ALU.mult)
                    P.stt(T2[:, :sz], pmn[:, 128:128 + sz], 1.0 / 64, T2[:, :sz], ALU.mult, ALU.subtract)
                    P.ts(T2[:, :sz], T2[:, :sz], 64e-5, ALU.add)
                    P.act(T2[:, :sz], T2[:, :sz], AF.Sqrt)
                    P.recip(T2[:, :sz], T2[:, :sz])
                    P.tt(OT[:, :sz], OT[:, :sz], T1[:, :sz], ALU.subtract)
                    P.tt(OT[:, :sz], OT[:, :sz], T2[:, :sz], ALU.mult)
                    P.ts(OT[:, :sz], OT[:, :sz], VR[:, l, 8, c:c + 1], ALU.mult, VR[:, l, 9, c:c + 1], ALU.add)
                    P.tt(OT[:, :sz], OT[:, :sz], BON_, ALU.add)
                    P.tt(yr[:, :sz], OT[:, :sz], G_, ALU.mult)
                    P.dma("sp", YT[c * 128:(c + 1) * 128, o:o + sz], yr[:, :sz], yr)
    def run_all():
        P.no_reset = (cfg.NSEQ == 1)
        prologue()
        P.reset_all()
        def body(s):
            stage_ln0(s)
            for l in range(dep):
                stage_win(l)
                stage_fox(l)
                stage_mlstm(l)
                stage_rwkv(l)
                stage_m1(l)
                stage_m2(l)
                ln_stage(NEWTv, 2 + 4 * l + 0, 2 + 4 * l + 1)
                stage_ffn(l)
                ln_stage(NEWTv, 2 + 4 * l + 2, 2 + 4 * l + 3)
            stage_out(s)
            P.reset_all()
        if cfg.NSEQ == 1:
            body(0)
        else:
            with nc.Fori(0, cfg.NSEQ) as s:
                body(s)
        P.es.close()
    return finish(locals())


def finish(lc):
    P, cfg, dbg = lc["P"], lc["cfg"], lc["dbg"]
    run = lc.get("run_stages")
    return lc


def pack_small(p, dep):
    def fm(v):
        return np.ascontiguousarray(np.asarray(v, np.float32).reshape(KC, 128).T)
    vecD = np.zeros((128, 2 + 4 * dep, KC), np.float32)
    vecD[:, 0] = fm(p["ln_emb_g"])
    vecD[:, 1] = fm(p["ln_emb_b"])
    for l in range(dep):
        vecD[:, 2 + 4 * l + 0] = fm(p["ln1_g"][l])
        vecD[:, 2 + 4 * l + 1] = fm(p["ln1_b"][l])
        vecD[:, 2 + 4 * l + 2] = fm(p["ln2_g"][l])
        vecD[:, 2 + 4 * l + 3] = fm(p["ln2_b"][l])
    vecR = np.zeros((128, dep, 13, 6), np.float32)

    def f6(v):
        return np.asarray(v, np.float32).reshape(6, 128).T
    for l in range(dep):
        mu = np.asarray(p["rwkv_mu"][l], np.float32)
        vecR[:, l, 0] = f6(mu[0:768])
        vecR[:, l, 1] = f6(mu[768:1536])
        vecR[:, l, 2] = f6(mu[1536:2304])
        vecR[:, l, 3] = f6(p["rwkv_w0"][l])
        vecR[:, l, 4] = f6(p["rwkv_a0"][l])
        vecR[:, l, 5] = f6(p["rwkv_k_k"][l])
        vecR[:, l, 6] = f6(p["rwkv_k_a"][l])
        vecR[:, l, 7] = f6(np.asarray(p["rwkv_r_k"][l]).reshape(768))
        vecR[:, l, 8] = f6(p["rwkv_gn_g"][l])
        vecR[:, l, 9] = f6(p["rwkv_gn_b"][l])
        vecR[:96, l, 10, 0] = mu[2304:2400]
        vecR[:96, l, 11, 0] = mu[2400:2496]
        vecR[:, l, 12, 0] = mu[2496:2624]
        vecR[:, l, 12, 1] = mu[2624:2752]
    foxbf = np.ascontiguousarray(np.asarray(p["fox_b_f"], np.float32)[:dep].reshape(dep, 12, 1))
    cw = np.asarray(p["mlstm_conv_w"], np.float32)[:dep]
    convw = np.ascontiguousarray(cw.reshape(dep, 4, 8, 128).transpose(3, 0, 2, 1))
    mlb = np.zeros((dep, 4, 2), np.float32)
    mlb[:, :, 0] = np.asarray(p["mlstm_b_i"], np.float32)[:dep]
    mlb[:, :, 1] = np.asarray(p["mlstm_b_f"], np.float32)[:dep]
    return {"consts": make_consts(), "vecD": vecD, "vecR": vecR, "foxbf": foxbf, "convw": convw, "mlb": mlb}


N_CORES = 8


def kernel(**inputs):
    x = np.asarray(inputs["x"], np.float32)
    B = x.shape[0]
    nseq = B // N_CORES
    cfg = Cfg(NT=16, NSEQ=nseq, depth=4)
    Res.ALL.clear()
    lc = build(cfg)
    lc["run_all"]()
    nc = lc["nc"]
    small = pack_small(inputs, 4)
    shared = dict(small)
    shared["meta"] = np.asarray(inputs["meta_tokens"], np.float32)
    for k in ["w_in", "rwkv_w2", "rwkv_a2", "rwkv_g2", "proj_rwkv", "proj_fox", "proj_mlstm", "w_out", "ffn_w1", "ffn_w3",
              "ffn_w2", "router_w", "moe_w1", "moe_w3", "moe_w2"]:
        shared[k] = np.asarray(inputs[k], np.float32)
    in_maps = []
    for c in range(N_CORES):
        m = dict(shared)
        m["x"] = np.ascontiguousarray(x[c * nseq:(c + 1) * nseq])
        in_maps.append(m)
    res = run_bass_kernel_spmd(nc, in_maps, core_ids=list(range(N_CORES)))
    return np.concatenate([r["out"] for r in res.results], axis=0).astype(np.float32)
```

```python
import numpy as np
from contextlib import ExitStack, contextmanager
import concourse.bass as bass
import concourse.mybir as mybir
from concourse.bass_utils import run_bass_kernel_spmd

F32 = mybir.dt.float32
BF16 = mybir.dt.bfloat16
AF = mybir.ActivationFunctionType
ALU = mybir.AluOpType
AX = mybir.AxisListType

D = 2048
KC = 16
NMETA = 16
RW = 768
RH = 12
FW = 768
MW = 512
MH = 4
C1 = 2752
C2 = C1 + 2316
C3 = C2 + 2056
NIN = C3 + 6144
DFF = 5632
FC = 44
NE = 8
ALPHA = 8 ** 0.25
NDS = 48
SAME_ENGINE_WAITS = True


class Res:
    __slots__ = ("name", "w", "r", "sem", "base")

    ALL = []

    def __init__(self, name):
        self.name = name
        self.w = {}
        self.r = {}
        self.base = {}
        self.sem = None
        Res.ALL.append(self)


class VA:
    __slots__ = ("ap", "res")

    def __init__(self, ap, res):
        self.ap = ap
        self.res = res

    def __getitem__(self, idx):
        return VA(self.ap[idx], self.res)


class T:
    def __init__(self, t, name):
        self.t = t
        self.res = Res(name)

    def __getitem__(self, idx):
        return VA(self.t[idx], self.res)


class Prog:
    def __init__(self, nc):
        self.nc = nc
        self.es = ExitStack()
        self.eng = {"pe": nc.tensor, "act": nc.scalar, "dve": nc.vector, "pool": nc.gpsimd, "sp": nc.sync}
        self.esem = {k: self.es.enter_context(nc.semaphore(f"e_{k}")) for k in self.eng}
        self.ecnt = {k: 0 for k in self.eng}
        self.dsems = [self.es.enter_context(nc.semaphore(f"d{i}")) for i in range(NDS)]
        self.dcnt = [0] * NDS
        self.dnext = 0
        self.obs = {k: {} for k in self.eng}
        self.ninstr = 0

    def semof(self, key):
        return self.esem[key[1]] if key[0] == "e" else self.dsems[key[1]]

    def _waits(self, eng, reads, writes, disjoint):
        waits = {}
        for r in reads:
            for k, v in r.w.items():
                if waits.get(k, 0) < v:
                    waits[k] = v
        for w in writes:
            for k, v in w.r.items():
                if waits.get(k, 0) < v:
                    waits[k] = v
            for k, v in (w.base if disjoint else w.w).items():
                if waits.get(k, 0) < v:
                    waits[k] = v
        ob = self.obs[eng]
        e = self.eng[eng]
        for k, v in waits.items():
            if k == ("e", eng) and (eng == "pe" or not SAME_ENGINE_WAITS):
                continue
            if ob.get(k, 0) >= v:
                continue
            e.wait_ge(self.semof(k), v)
            ob[k] = v
            self.ninstr += 1

    def _record(self, key, val, reads, writes, disjoint):
        for r in reads:
            if r.r.get(key, 0) < val:
                r.r[key] = val
        for w in writes:
            if not disjoint:
                w.r = {}
                w.w = {key: val}
                w.base = {key: val}
            else:
                if w.w.get(key, 0) < val:
                    w.w[key] = val

    def op(self, eng, fn, reads, writes, disjoint=False):
        reads = [x.res for x in reads if x is not None]
        writes = [x.res for x in writes]
        self._waits(eng, reads, writes, disjoint)
        ins = fn(self.eng[eng])
        self.ecnt[eng] += 1
        ins.then_inc(self.esem[eng], 1)
        self.ninstr += 1
        self._record(("e", eng), self.ecnt[eng], reads, writes, disjoint)

    def dma(self, q, out, in_, tile, disjoint=True):
        res = tile.res
        if res.sem is None:
            res.sem = self.dnext % NDS
            self.dnext += 1
        idx = res.sem
        reads = [in_.res]
        writes = [out.res]
        self._waits(q, reads, writes, disjoint)
        ins = self.eng[q].dma_start(out=out.ap, in_=in_.ap)
        self.dcnt[idx] += 1
        ins.then_inc(self.dsems[idx], 16)
        self.ninstr += 1
        self._record(("d", idx), 16 * self.dcnt[idx], reads, writes, disjoint)

    def barrier(self):
        ev = {("e", k): v for k, v in self.ecnt.items() if v > 0}
        for i in range(NDS):
            if self.dcnt[i] > 0:
                ev[("d", i)] = 16 * self.dcnt[i]
        for eng, e in self.eng.items():
            ob = self.obs[eng]
            for k, v in ev.items():
                if ob.get(k, 0) >= v:
                    continue
                e.wait_ge(self.semof(k), v)
                ob[k] = v
                self.ninstr += 1

    def reset_all(self):
        nc = self.nc
        self.barrier()
        if getattr(self, "no_reset", False):
            return
        if not hasattr(self, "bsem"):
            self.bsem = [self.es.enter_context(nc.semaphore(f"bar{i}")) for i in range(4)]
        A, C, B, Dd = self.bsem
        order = ["pe", "act", "dve", "pool", "sp"]
        for k in order:
            e = self.eng[k]
            e.sem_inc(A, 1)
            e.wait_ge(A, 5)
            e.sem_inc(C, 1)
        pe = self.eng["pe"]
        pe.wait_ge(C, 5)
        for s_ in list(self.esem.values()) + list(self.dsems):
            pe.sem_clear(s_)
        pe.sem_clear(A)
        pe.sem_clear(C)
        pe.sem_inc(B, 1)
        for k in order[1:]:
            e = self.eng[k]
            e.wait_ge(B, 1)
            e.sem_inc(Dd, 1)
        pe.wait_ge(Dd, 4)
        pe.sem_clear(B)
        pe.sem_clear(Dd)
        self.ecnt = {k: 0 for k in self.eng}
        self.dcnt = [0] * NDS
        self.obs = {k: {} for k in self.eng}
        for r in Res.ALL:
            r.w = {}
            r.r = {}
            r.base = {}

    @contextmanager
    def stage(self):
        st = Stage(self)
        try:
            yield st
        finally:
            self.barrier()
            st.es.close()

    def dram(self, name, shape, dt):
        return T(self.nc.dram_tensor(name, list(shape), dt, kind="Internal"), name)

    def mm(self, out, lhsT, rhs, start=True, stop=True):
        self.op("pe", lambda e: e.matmul(out.ap, lhsT.ap, rhs.ap, start=start, stop=stop),
                [lhsT, rhs], [out], disjoint=True)

    def tr(self, out, in_, ident):
        self.op("pe", lambda e: e.transpose(out.ap, in_.ap, ident.ap), [in_, ident], [out], disjoint=True)

    def act(self, out, in_, func, bias=None, scale=1.0, eng="act"):
        def f(e):
            kw = {}
            if bias is not None:
                kw["bias"] = bias.ap if isinstance(bias, VA) else bias
            return e.activation(out=out.ap, in_=in_.ap, func=func, scale=scale, **kw)
        self.op("act", f, [in_, bias if isinstance(bias, VA) else None], [out], disjoint=True)

    def tt(self, out, a, b, op, eng="dve"):
        self.op(eng, lambda e: e.tensor_tensor(out=out.ap, in0=a.ap, in1=b.ap, op=op), [a, b], [out], disjoint=True)

    def ts(self, out, a, s1, op0, s2=None, op1=None, eng="dve"):
        def f(e):
            a1 = s1.ap if isinstance(s1, VA) else s1
            if s2 is None:
                return e.tensor_scalar(out=out.ap, in0=a.ap, scalar1=a1, scalar2=None, op0=op0)
            a2 = s2.ap if isinstance(s2, VA) else s2
            return e.tensor_scalar(out=out.ap, in0=a.ap, scalar1=a1, scalar2=a2, op0=op0, op1=op1)
        self.op(eng, f, [a, s1 if isinstance(s1, VA) else None, s2 if isinstance(s2, VA) else None], [out],
                disjoint=True)

    def stt(self, out, a, s, b, op0, op1):
        def f(e):
            sc = s.ap if isinstance(s, VA) else s
            return e.scalar_tensor_tensor(out=out.ap, in0=a.ap, scalar=sc, in1=b.ap, op0=op0, op1=op1)
        self.op("dve", f, [a, b, s if isinstance(s, VA) else None], [out], disjoint=True)

    def copy(self, out, in_, eng="dve"):
        if eng == "act":
            self.act(out, in_, AF.Copy)
        else:
            self.op(eng, lambda e: e.tensor_copy(out=out.ap, in_=in_.ap), [in_], [out], disjoint=True)

    def recip(self, out, in_):
        self.op("dve", lambda e: e.reciprocal(out=out.ap, in_=in_.ap), [in_], [out], disjoint=True)

    def memset(self, out, val, eng="dve"):
        self.op(eng, lambda e: e.memset(out.ap, val), [], [out], disjoint=False)

    def scan(self, out, d0, d1, init, op0, op1):
        self.op("dve", lambda e: e.tensor_tensor_scan(out=out.ap, data0=d0.ap, data1=d1.ap, initial=init,
                                                      op0=op0, op1=op1), [d0, d1], [out], disjoint=True)


class Stage:
    def __init__(self, p):
        self.p = p
        self.es = ExitStack()
        self.n = 0

    def tile(self, shape, dt, name=None):
        self.n += 1
        name = name or f"t{self.n}"
        self.p.uid = getattr(self.p, "uid", 0) + 1
        nm = f"{name}_{self.p.uid}"
        return T(self.es.enter_context(self.p.nc.sbuf_tensor(nm, list(shape), dt)), nm)

    def psum(self, shape=(128, 512), dt=F32, name=None):
        self.n += 1
        self.p.uid = getattr(self.p, "uid", 0) + 1
        nm = f"ps_{self.p.uid}"
        return T(self.es.enter_context(self.p.nc.psum_tensor(nm, list(shape), dt)), nm)


CO_ID = 0
CO_MEAN = 128
CO_BLK = 256
CO_MLE = 384
CO_MLT = 512
CO_MGE = 640
CO_ONE = 768
CO_SEL = 896
NCONST = CO_SEL + 12 * 128


def make_consts():
    c = np.zeros((128, NCONST), np.float32)
    i = np.arange(128)
    c[:, CO_ID:CO_ID + 128] = np.eye(128)
    c[:, CO_MEAN:CO_MEAN + 128] = 1.0 / D
    c[:64, CO_BLK:CO_BLK + 64] = 1.0
    c[64:, CO_BLK + 64:CO_BLK + 128] = 1.0
    c[:, CO_MLE:CO_MLE + 128] = (i[:, None] <= i[None, :])
    c[:, CO_MLT:CO_MLT + 128] = (i[:, None] < i[None, :])
    c[:, CO_MGE:CO_MGE + 128] = (i[:, None] > i[None, :])
    c[:, CO_ONE:CO_ONE + 128] = 1.0
    for h in range(12):
        c[h, CO_SEL + h * 128:CO_SEL + (h + 1) * 128] = 1.0
    return c


class TT(T):
    def view(self, pat, **kw):
        v = TT.__new__(TT)
        v.t = self.t.rearrange(pat, **kw)
        v.res = self.res
        return v


def va_re(va, pat, **kw):
    return VA(va.ap.rearrange(pat, **kw), va.res)


class Cfg:
    def __init__(self, NT=16, NSEQ=1, depth=4, debug=False):
        self.NT = NT
        self.NSEQ = NSEQ
        self.depth = depth
        self.L = NMETA + 128 * NT
        self.S = 128 * NT
        self.tiles = [(0, NMETA)] + [(NMETA + 128 * i, 128) for i in range(NT)]
        self.sbs = [(o, min(512, self.L - o)) for o in range(0, self.L, 512)]
        self.n_dense = (depth + 1) // 2
        self.n_moe = depth // 2
        self.debug = debug


def col_chunks():
    segs = [0, 768, 1536, 2304, 2400, 2496, C1, C1 + 768, C1 + 1536, C1 + 2304, C2, C2 + 512, C2 + 1024,
            C2 + 1536, C2 + 2048, C3, C3 + 2048, C3 + 4096, NIN]
    chunks = []
    for a, b in zip(segs[:-1], segs[1:]):
        c = a
        while c < b:
            m = min(128, b - c)
            chunks.append((c, m))
            c += m
    blocks = []
    cur = []
    for ch in chunks:
        if cur and (ch[0] + ch[1] - cur[0][0]) > 512:
            blocks.append(cur)
            cur = []
        cur.append(ch)
    blocks.append(cur)
    return blocks


def build(cfg):
    nc = bass.Bass("TRN2", target_bir_lowering=False)
    P = Prog(nc)
    L, NT, dep = cfg.L, cfg.NT, cfg.depth
    NTl = NT + 1

    def ext(name, shape, dt=F32):
        t = TT.__new__(TT)
        t.t = nc.dram_tensor(name, list(shape), dt, kind="ExternalInput").ap()
        t.res = Res(name)
        return t

    def scr(name, shape, dt=F32):
        t = TT.__new__(TT)
        t.t = nc.dram_tensor(name, list(shape), dt, kind="Internal").ap()
        t.res = Res(name)
        return t

    x_in = ext("x", [cfg.NSEQ, cfg.S, D])
    meta = ext("meta", [NMETA, D])
    consts = ext("consts", [128, NCONST])
    vecD = ext("vecD", [128, 2 + 4 * dep, KC])
    vecR = ext("vecR", [128, dep, 13, 6])
    foxbf = ext("foxbf", [dep, 12, 1])
    convw = ext("convw", [128, dep, 8, 4])
    mlb = ext("mlb", [dep, 4, 2])
    w_in = ext("w_in", [dep, D, NIN])
    rw2 = ext("rwkv_w2", [dep, 96, RW])
    ra2 = ext("rwkv_a2", [dep, 96, RW])
    rg2 = ext("rwkv_g2", [dep, 256, RW])
    p_r = ext("proj_rwkv", [dep, RW, D])
    p_f = ext("proj_fox", [dep, FW, D])
    p_m = ext("proj_mlstm", [dep, MW, D])
    w_o = ext("w_out", [dep, D, D])
    f_w1 = ext("ffn_w1", [cfg.n_dense, D, DFF])
    f_w3 = ext("ffn_w3", [cfg.n_dense, D, DFF])
    f_w2 = ext("ffn_w2", [cfg.n_dense, DFF, D])
    if cfg.n_moe:
        r_w = ext("router_w", [cfg.n_moe, D, NE])
        m_w1 = ext("moe_w1", [cfg.n_moe, NE, D, DFF])
        m_w3 = ext("moe_w3", [cfg.n_moe, NE, D, DFF])
        m_w2 = ext("moe_w2", [cfg.n_moe, NE, DFF, D])
    out_t = TT.__new__(TT)
    out_t.t = nc.dram_tensor("out", [cfg.NSEQ, cfg.S, D], F32, kind="ExternalOutput").ap()
    out_t.res = Res("out")
    dbg = {}
    if cfg.debug:
        for nm, shp in [("d_ht", [D, L]), ("d_zt", [NIN, L]), ("d_yt", [D, L]), ("d_new", [D, L])]:
            t = TT.__new__(TT)
            t.t = nc.dram_tensor(nm, shp, F32, kind="ExternalOutput").ap()
            t.res = Res(nm)
            dbg[nm] = t

    HT32 = scr("HT32", [D, L])
    HTb = scr("HTb", [D, L], BF16)
    ZT = scr("ZT", [NIN, L])
    YT = scr("YT", [D, L], BF16)
    MIXT = scr("MIXT", [D, L], BF16)
    NEWT = scr("NEWT", [D, L])
    RWS = scr("RWS", [8, RW, L])
    HT32v = HT32.view("(kc p) l -> p kc l", p=128)
    HTbv = HTb.view("(kc p) l -> p kc l", p=128)
    YTv = YT.view("(kc p) l -> p kc l", p=128)
    MIXTv = MIXT.view("(kc p) l -> p kc l", p=128)
    NEWTv = NEWT.view("(kc p) l -> p kc l", p=128)

    ges = P.es

    def gtile(name, shape, dt):
        return TT_from(ges.enter_context(nc.sbuf_tensor(name, list(shape), dt)), name)

    def TT_from(t, name):
        o = TT.__new__(TT)
        o.t = t
        o.res = Res(name)
        return o

    CON = gtile("CON", [128, NCONST], F32)
    CONB = gtile("CONB", [128, 896], BF16)
    VD = gtile("VD", [128, 2 + 4 * dep, KC], F32)
    VR = gtile("VR", [128, dep, 13, 6], F32)
    P.dma("sp", CON[:, :], consts[:, :], CON)
    P.dma("sp", VD[:, :, :], vecD[:, :, :], VD)
    P.dma("sp", VR[:, :, :, :], vecR[:, :, :, :], VR)
    P.copy(CONB[:, :], CON[:, 0:896])
    ID = CON[:, CO_ID:CO_ID + 128]

    def ident(n):
        return CON[:n, CO_ID:CO_ID + n]

    STG_N = 2816

    def mk_stg(st, n=3):
        st.stg = [st.tile([128, STG_N], F32) for _ in range(n)]
        st.stg_i = 0

    def wload(st, dst, src, np_, a, b, eng="pool", engs=None, queues=("sp",)):
        step = max(1, STG_N // b)
        a0 = 0
        while a0 < a:
            a1 = min(a, a0 + step)
            stg = st.stg[st.stg_i % len(st.stg)]
            st.stg_i += 1
            view = VA(stg.t[:np_, 0:(a1 - a0) * b].rearrange("p (a b) -> p a b", a=a1 - a0), stg.res)
            P.dma(queues[st.stg_i % len(queues)], view, src[:, a0:a1, :], stg, disjoint=False)
            P.copy(dst[:, a0:a1, :], view, eng=(engs[st.stg_i % len(engs)] if engs else eng))
            a0 = a1

    def pipelined(n, load, compute, depth=1):
        for i in range(min(depth, n)):
            load(i)
        for i in range(n):
            if i + depth < n:
                load(i + depth)
            compute(i)

    blocks = col_chunks()
    cache = {}
    NB_WIN = len(blocks)
    n_ffn = cfg.n_dense + cfg.n_moe * NE

    def cfam(name, nslots, elems):
        per = max(1, (96 << 20) // (128 * elems * 2))
        ts = [scr(f"C_{name}_{g}", [min(per, nslots - g * per), 128, elems], BF16)
              for g in range((nslots + per - 1) // per)]
        cache[name] = (per, ts)

    def cview(fam, slot, a, b):
        per, ts = cache[fam]
        t = ts[slot // per]
        return VA(t.t[slot % per, :, 0:a * b].rearrange("p (a b) -> p a b", a=a), t.res)

    cfam("win", dep * NB_WIN, KC * 512)
    cfam("pr", dep, 6 * D)
    cfam("pf", dep, 6 * D)
    cfam("pm", dep, 4 * D)
    cfam("wo", dep * 4, KC * 512)
    cfam("w1", n_ffn * 22, KC * 256)
    cfam("w3", n_ffn * 22, KC * 256)
    cfam("w2", n_ffn * 16, FC * 128)

    def ffn_idx(l, e_):
        return (l // 2) if l % 2 == 0 else cfg.n_dense + (l // 2) * NE + e_

    def prologue():
        with P.stage() as st:
            mk_stg(st, 8)
            WT = [st.tile([128, 6 * D], BF16) for _ in range(3)]
            k = [0]
            engs = ["pool", "dve", "pool", "dve", "act"]

            def fill(fam, slot, srcv, a, b):
                wt = WT[k[0] % 3]
                k[0] += 1
                dst = VA(wt.t[:, 0:a * b].rearrange("p (a b) -> p a b", a=a), wt.res)
                wload(st, dst, srcv, 128, a, b, engs=engs, queues=("sp", "act"))
                P.dma("sp" if k[0] % 2 else "act", cview(fam, slot, a, b), dst, wt)
            wv = w_in.view("l (kc p) n -> l p kc n", p=128)
            prv = p_r.view("l (kc p) n -> l p kc n", p=128)
            pfv = p_f.view("l (kc p) n -> l p kc n", p=128)
            pmv = p_m.view("l (kc p) n -> l p kc n", p=128)
            wov = w_o.view("l (kc p) n -> l p kc n", p=128)
            for l in range(dep):
                for bi_, blk in enumerate(blocks):
                    c0 = blk[0][0]
                    cw = blk[-1][0] + blk[-1][1] - c0
                    fill("win", l * NB_WIN + bi_, wv[l, :, :, c0:c0 + cw], KC, cw)
                fill("pr", l, prv[l], 6, D)
                fill("pf", l, pfv[l], 6, D)
                fill("pm", l, pmv[l], 4, D)
                for cb in range(4):
                    fill("wo", l * 4 + cb, wov[l, :, :, cb * 512:(cb + 1) * 512], KC, 512)
                moe = (l % 2 == 1)
                li = l // 2
                for e_ in range(NE if moe else 1):
                    if moe:
                        w1v = m_w1.view("l e (kc p) n -> l e p kc n", p=128)[li, e_]
                        w3v = m_w3.view("l e (kc p) n -> l e p kc n", p=128)[li, e_]
                        w2v = m_w2.view("l e (kc p) n -> l e p kc n", p=128)[li, e_]
                    else:
                        w1v = f_w1.view("l (kc p) n -> l p kc n", p=128)[li]
                        w3v = f_w3.view("l (kc p) n -> l p kc n", p=128)[li]
                        w2v = f_w2.view("l (kc p) n -> l p kc n", p=128)[li]
                    fi = ffn_idx(l, e_)
                    for cb in range(22):
                        fill("w1", fi * 22 + cb, w1v[:, :, cb * 256:(cb + 1) * 256], KC, 256)
                        fill("w3", fi * 22 + cb, w3v[:, :, cb * 256:(cb + 1) * 256], KC, 256)
                    for m in range(16):
                        fill("w2", fi * 16 + m, w2v[:, :, m * 128:(m + 1) * 128], FC, 128)

    def ln_stage(src_v, gi, bi, also_dbg=None):
        with P.stage() as st:
            NEW = [st.tile([128, KC, 512], F32) for _ in range(2)]
            SQ = st.tile([128, KC, 512], F32)
            OB = [st.tile([128, KC, 512], BF16) for _ in range(2)]
            mean = st.tile([128, 512], F32)
            rstd = st.tile([128, 512], F32)
            psm = st.psum()
            psq = st.psum()
            for bi_, (o, n) in enumerate(cfg.sbs):
                nw = NEW[bi_ % 2]
                ob = OB[bi_ % 2]
                P.dma("sp", nw[:, :, :n], src_v[:, :, o:o + n], nw)
                ln_core(st, nw, SQ, ob, mean, rstd, psm, psq, n, gi, bi)
                P.dma("sp", HT32v[:, :, o:o + n], nw[:, :, :n], nw)
                P.dma("sp", HTbv[:, :, o:o + n], ob[:, :, :n], ob)

    def ln_core(st, nw, SQ, ob, mean, rstd, psm, psq, n, gi, bi):
        MEANM = CON[:, CO_MEAN:CO_MEAN + 128]
        P.act(SQ[:, :, :n], nw[:, :, :n], AF.Square)
        for kc in range(KC):
            P.mm(psm[:, :n], MEANM, nw[:, kc, :n], start=(kc == 0), stop=(kc == KC - 1))
        for kc in range(KC):
            P.mm(psq[:, :n], MEANM, SQ[:, kc, :n], start=(kc == 0), stop=(kc == KC - 1))
        P.copy(mean[:, :n], psm[:, :n])
        P.tt(rstd[:, :n], mean[:, :n], mean[:, :n], ALU.mult)
        P.tt(rstd[:, :n], psq[:, :n], rstd[:, :n], ALU.subtract)
        P.ts(rstd[:, :n], rstd[:, :n], 1e-5, ALU.add)
        P.act(rstd[:, :n], rstd[:, :n], AF.Sqrt)
        P.recip(rstd[:, :n], rstd[:, :n])
        for kc in range(KC):
            P.tt(SQ[:, kc, :n], nw[:, kc, :n], mean[:, :n], ALU.subtract)
            P.tt(SQ[:, kc, :n], SQ[:, kc, :n], rstd[:, :n], ALU.mult)
            P.ts(nw[:, kc, :n], SQ[:, kc, :n], VD[:, gi, kc:kc + 1], ALU.mult, VD[:, bi, kc:kc + 1], ALU.add)
            P.copy(ob[:, kc, :n], nw[:, kc, :n], eng="pool")

    def stage_ln0(s):
        with P.stage() as st:
            XT = [st.tile([128, D], F32) for _ in range(2)]
            NEW = st.tile([128, KC, 128], F32)
            SQ = st.tile([128, KC, 128], F32)
            OB = st.tile([128, KC, 128], BF16)
            mean = st.tile([128, 128], F32)
            rstd = st.tile([128, 128], F32)
            pst = [st.psum() for _ in range(2)]
            psm = st.psum()
            psq = st.psum()
            for ti, (o, sz) in enumerate(cfg.tiles):
                xt = XT[ti % 2]
                if ti == 0:
                    P.dma("sp", xt[:sz, :], meta[:, :], xt)
                else:
                    P.dma("sp", xt[:sz, :], x_in[s, o - NMETA:o - NMETA + sz, :], xt)
                for g in range(4):
                    ps = pst[g % 2]
                    for j in range(4):
                        kc = g * 4 + j
                        P.tr(ps[:, j * 128:j * 128 + sz], xt[:sz, kc * 128:(kc + 1) * 128], ident(sz))
                    P.copy(NEW[:, g * 4:(g + 1) * 4, :sz],
                           va_re(ps[:, :], "p (a b) -> p a b", a=4)[:, :, :sz] if False else
                           VA(ps.t[:, :].rearrange("p (a b) -> p a b", a=4)[:, :, :sz], ps.res))
                ln_core(st, NEW, SQ, OB, mean, rstd, psm, psq, sz, 0, 1)
                P.dma("sp", HT32v[:, :, o:o + sz], NEW[:, :, :sz], NEW)
                P.dma("sp", HTbv[:, :, o:o + sz], OB[:, :, :sz], OB)

    blocks = col_chunks()

    def stage_win(l):
        wv = w_in.view("l (kc p) n -> l p kc n", p=128)
        with P.stage() as st:
            mk_stg(st, 3)
            XT = [st.tile([128, KC, 512], BF16) for _ in range(2)]
            W = [st.tile([128, KC, 512], BF16) for _ in range(3)]
            ZS = [st.tile([128, 512], F32) for _ in range(4)]
            PS = [st.psum() for _ in range(4)]
            jobs = [(bi_, o, n, blk, bj) for bi_, (o, n) in enumerate(cfg.sbs) for bj, blk in enumerate(blocks)]
            cnt = [0]

            def load(i):
                bi_, o, n, blk, bj = jobs[i]
                if bj == 0:
                    xt = XT[bi_ % 2]
                    P.dma("sp", xt[:, :, :n], HTbv[:, :, o:o + n], xt)
                c0 = blk[0][0]
                cw = blk[-1][0] + blk[-1][1] - c0
                w = W[i % 3]
                P.dma("sp", w[:, :, :cw], cview("win", l * NB_WIN + bj, KC, cw), w, disjoint=False)

            def compute(i):
                bi_, o, n, blk, bj = jobs[i]
                xt = XT[bi_ % 2]
                w = W[i % 3]
                c0 = blk[0][0]
                for (c, m) in blk:
                    ps = PS[cnt[0] % 4]
                    zs = ZS[cnt[0] % 4]
                    cnt[0] += 1
                    for kc in range(KC):
                        P.mm(ps[:m, :n], w[:, kc, c - c0:c - c0 + m], xt[:, kc, :n], start=(kc == 0),
                             stop=(kc == KC - 1))
                    P.act(zs[:m, :n], ps[:m, :n], AF.Sigmoid if c >= C3 else AF.Copy)
                    P.dma("sp", ZT[c:c + m, o:o + n], zs[:m, :n], zs)
            pipelined(len(jobs), load, compute, depth=2)

    def stage_m1(l):
        prv = p_r.view("l (kc p) n -> l p kc n", p=128)
        pfv = p_f.view("l (kc p) n -> l p kc n", p=128)
        pmv = p_m.view("l (kc p) n -> l p kc n", p=128)
        with P.stage() as st:
            PR = st.tile([128, 6, D], BF16)
            PF = st.tile([128, 6, D], BF16)
            PM = st.tile([128, 4, D], BF16)
            P.dma("sp", PR[:, :, :], cview("pr", l, 6, D), PR)
            P.dma("sp", PF[:, :, :], cview("pf", l, 6, D), PF)
            P.dma("sp", PM[:, :, :], cview("pm", l, 4, D), PM)
            Y = [st.tile([128, KC, 512], BF16) for _ in range(2)]
            G = [[st.tile([128, 512], F32) for _ in range(3)] for _ in range(2)]
            A1 = [st.tile([128, 512], F32) for _ in range(2)]
            A2 = [st.tile([128, 512], F32) for _ in range(2)]
            MO = [st.tile([128, 512], BF16) for _ in range(2)]
            PS = [[st.psum() for _ in range(3)] for _ in range(2)]
            it = 0
            for bi_, (o, n) in enumerate(cfg.sbs):
                y = Y[bi_ % 2]
                P.dma("sp", y[:, :, :n], YTv[:, :, o:o + n], y)
                for m in range(KC):
                    g3 = G[it % 2]
                    ps3 = PS[it % 2]
                    a1, a2, mo = A1[it % 2], A2[it % 2], MO[it % 2]
                    it += 1
                    for gi in range(3):
                        r0 = C3 + gi * D + m * 128
                        P.dma("sp", g3[gi][:, :n], ZT[r0:r0 + 128, o:o + n], g3[gi])
                    for kc in range(6):
                        P.mm(ps3[0][:, :n], PR[:, kc, m * 128:(m + 1) * 128], y[:, kc, :n], start=(kc == 0), stop=(kc == 5))
                    for kc in range(6):
                        P.mm(ps3[1][:, :n], PF[:, kc, m * 128:(m + 1) * 128], y[:, 6 + kc, :n], start=(kc == 0), stop=(kc == 5))
                    for kc in range(4):
                        P.mm(ps3[2][:, :n], PM[:, kc, m * 128:(m + 1) * 128], y[:, 12 + kc, :n], start=(kc == 0), stop=(kc == 3))
                    P.tt(a1[:, :n], ps3[0][:, :n], g3[0][:, :n], ALU.mult)
                    P.tt(a2[:, :n], ps3[1][:, :n], g3[1][:, :n], ALU.mult)
                    P.tt(a1[:, :n], a1[:, :n], a2[:, :n], ALU.add)
                    P.tt(a2[:, :n], ps3[2][:, :n], g3[2][:, :n], ALU.mult)
                    P.tt(mo[:, :n], a1[:, :n], a2[:, :n], ALU.add)
                    P.dma("sp", MIXT[m * 128:(m + 1) * 128, o:o + n], mo[:, :n], mo)

    def stage_proj_res(xv, nkc, wview, WB=512):
        with P.stage() as st:
            mk_stg(st, 3)
            XT = [st.tile([128, nkc, 512], BF16) for _ in range(2)]
            W = [st.tile([128, nkc, WB], BF16) for _ in range(2)]
            HR = [st.tile([128, 512], F32) for _ in range(3)]
            PS = [st.psum() for _ in range(3)]
            jobs = [(bi_, o, n, cb) for bi_, (o, n) in enumerate(cfg.sbs) for cb in range(D // WB)]
            it = [0]

            def load(i):
                bi_, o, n, cb = jobs[i]
                if cb == 0:
                    xt = XT[bi_ % 2]
                    P.dma("sp", xt[:, :, :n], xv[:, :, o:o + n], xt)
                w = W[i % 2]
                P.dma("sp", w[:, :, :], wview(cb), w, disjoint=False)

            def compute(i):
                bi_, o, n, cb = jobs[i]
                xt = XT[bi_ % 2]
                w = W[i % 2]
                for mm_ in range(WB // 128):
                    m = cb * (WB // 128) + mm_
                    ps = PS[it[0] % 3]
                    hr = HR[it[0] % 3]
                    it[0] += 1
                    P.dma("sp", hr[:, :n], HT32[m * 128:(m + 1) * 128, o:o + n], hr)
                    for kc in range(nkc):
                        P.mm(ps[:, :n], w[:, kc, mm_ * 128:(mm_ + 1) * 128], xt[:, kc, :n], start=(kc == 0),
                             stop=(kc == nkc - 1))
                    P.stt(hr[:, :n], hr[:, :n], ALPHA, ps[:, :n], ALU.mult, ALU.add)
                    P.dma("sp", NEWT[m * 128:(m + 1) * 128, o:o + n], hr[:, :n], hr)
            pipelined(len(jobs), load, compute, depth=1)

    def stage_m2(l):
        stage_proj_res(MIXTv, KC, lambda cb: cview("wo", l * 4 + cb, KC, 512))

    def stage_ffn(l):
        moe = (l % 2 == 1)
        li = l // 2
        nexp = NE if moe else 1
        with P.stage() as st:
            XT = st.tile([128, KC, 512], BF16)
            HID = st.tile([128, FC, 512], BF16)
            W1 = [st.tile([128, KC, 256], BF16) for _ in range(2)]
            W3 = [st.tile([128, KC, 256], BF16) for _ in range(2)]
            W2 = [st.tile([128, FC, 128], BF16) for _ in range(2)]
            SIL = [st.tile([128, 512], F32) for _ in range(2)]
            HR = [st.tile([128, 512], F32) for _ in range(2)]
            PA = [st.psum() for _ in range(2)]
            PB = [st.psum() for _ in range(2)]
            PO = [st.psum() for _ in range(2)]
            mk_stg(st, 2)
            if moe:
                ACC = st.tile([128, KC, 512], F32)
                GBC = st.tile([128, NE, 512], BF16)
                X32 = st.tile([128, KC, 128], F32)
                RWT = st.tile([128, KC, NE], F32)
                LG = st.tile([128, 8], F32)
                MX = st.tile([128, 8], F32)
                EX = st.tile([128, 8], F32)
                MK = st.tile([128, 8], F32)
                SC = st.tile([128, 4], F32)
                DG = st.tile([128, 128], F32)
                PR_ = st.psum()
                P.dma("sp", RWT[:, :, :], r_w.view("l (kc p) e -> l p kc e", p=128)[li], RWT)
            it = 0
            wi = 0
            w2i = 0
            for bi_, (o, n) in enumerate(cfg.sbs):
                P.dma("sp", XT[:, :, :n], HTbv[:, :, o:o + n], XT)
                if moe:
                    for t0 in range(0, n, 128):
                        tn = min(128, n - t0)
                        P.dma("sp", X32[:, :, :tn], HT32v[:, :, o + t0:o + t0 + tn], X32)
                        for kc in range(KC):
                            P.mm(PR_[:tn, 0:8], X32[:, kc, :tn], RWT[:, kc, :], start=(kc == 0), stop=(kc == KC - 1))
                        P.copy(LG[:tn, :], PR_[:tn, 0:8])
                        P.op("dve", lambda e: e.max(out=MX.t[:tn, :], in_=LG.t[:tn, :]), [LG], [MX], disjoint=True)
                        P.ts(MK[:tn, :], LG[:tn, :], MX[:tn, 1:2], ALU.is_ge)
                        P.ts(SC[:tn, 0:1], MX[:tn, 0:1], -1.0, ALU.mult)
                        P.act(EX[:tn, :], LG[:tn, :], AF.Exp, bias=SC[:tn, 0:1])
                        P.tt(EX[:tn, :], EX[:tn, :], MK[:tn, :], ALU.mult)
                        P.op("dve", lambda e: e.reduce_sum(out=SC.t[:tn, 1:2], in_=EX.t[:tn, :], axis=AX.X), [EX], [SC],
                             disjoint=True)
                        P.recip(SC[:tn, 2:3], SC[:tn, 1:2])
                        P.ts(EX[:tn, :], EX[:tn, :], SC[:tn, 2:3], ALU.mult)
                        for e_ in range(NE):
                            P.ts(DG[:tn, :tn], CON[:tn, CO_ID:CO_ID + tn], EX[:tn, e_:e_ + 1], ALU.mult)
                            P.mm(PR_[:, 128:128 + tn], CON[:tn, CO_ONE:CO_ONE + 128], DG[:tn, :tn])
                            P.copy(GBC[:, e_, t0:t0 + tn], PR_[:, 128:128 + tn])
                for e_ in range(nexp):
                    if moe:
                        w1v = m_w1.view("l e (kc p) n -> l e p kc n", p=128)[li, e_]
                        w3v = m_w3.view("l e (kc p) n -> l e p kc n", p=128)[li, e_]
                        w2v = m_w2.view("l e (kc p) n -> l e p kc n", p=128)[li, e_]
                    else:
                        w1v = f_w1.view("l (kc p) n -> l p kc n", p=128)[li]
                        w3v = f_w3.view("l (kc p) n -> l p kc n", p=128)[li]
                        w2v = f_w2.view("l (kc p) n -> l p kc n", p=128)[li]
                    for cb in range(DFF // 256):
                        w1 = W1[wi % 2]
                        w3 = W3[wi % 2]
                        wi += 1
                        P.dma("sp", w1[:, :, :], cview("w1", ffn_idx(l, e_) * 22 + cb, KC, 256), w1, disjoint=False)
                        P.dma("sp", w3[:, :, :], cview("w3", ffn_idx(l, e_) * 22 + cb, KC, 256), w3, disjoint=False)
                        for j in range(2):
                            f = cb * 2 + j
                            pa, pb, sil = PA[it % 2], PB[it % 2], SIL[it % 2]
                            it += 1
                            for kc in range(KC):
                                P.mm(pa[:, :n], w1[:, kc, j * 128:(j + 1) * 128], XT[:, kc, :n], start=(kc == 0), stop=(kc == KC - 1))
                            for kc in range(KC):
                                P.mm(pb[:, :n], w3[:, kc, j * 128:(j + 1) * 128], XT[:, kc, :n], start=(kc == 0), stop=(kc == KC - 1))
                            P.act(sil[:, :n], pa[:, :n], AF.Silu)
                            if moe:
                                P.tt(sil[:, :n], sil[:, :n], pb[:, :n], ALU.mult)
                                P.tt(HID[:, f, :n], sil[:, :n], GBC[:, e_, :n], ALU.mult)
                            else:
                                P.tt(HID[:, f, :n], sil[:, :n], pb[:, :n], ALU.mult)
                    for m in range(KC):
                        w2 = W2[w2i % 2]
                        po = PO[w2i % 2]
                        hr = HR[w2i % 2]
                        w2i += 1
                        P.dma("sp", w2[:, :, :], cview("w2", ffn_idx(l, e_) * 16 + m, FC, 128), w2, disjoint=False)
                        for f in range(FC):
                            P.mm(po[:, :n], w2[:, f, :], HID[:, f, :n], start=(f == 0), stop=(f == FC - 1))
                        if moe and e_ > 0:
                            P.tt(ACC[:, m, :n], ACC[:, m, :n], po[:, :n], ALU.add)
                        elif moe:
                            P.copy(ACC[:, m, :n], po[:, :n])
                        if (not moe) or e_ == nexp - 1:
                            P.dma("sp", hr[:, :n], HT32[m * 128:(m + 1) * 128, o:o + n], hr)
                            src_ = ACC[:, m, :n] if moe else po[:, :n]
                            P.stt(hr[:, :n], hr[:, :n], ALPHA, src_, ALU.mult, ALU.add)
                            P.dma("sp", NEWT[m * 128:(m + 1) * 128, o:o + n], hr[:, :n], hr)

    def stage_out(s):
        with P.stage() as st:
            HTt = [st.tile([128, KC, 128], F32) for _ in range(2)]
            OT = [st.tile([128, D], F32) for _ in range(2)]
            PS = [st.psum() for _ in range(2)]
            for ti, (o, sz) in enumerate(cfg.tiles[1:]):
                ht = HTt[ti % 2]
                ot = OT[ti % 2]
                P.dma("sp", ht[:, :, :], HT32v[:, :, o:o + sz], ht)
                for g in range(4):
                    ps = PS[g % 2]
                    for j in range(4):
                        kc = g * 4 + j
                        P.tr(ps[:, j * 128:(j + 1) * 128], ht[:, kc, :], ident(128))
                    P.copy(ot[:, g * 512:(g + 1) * 512], ps[:, :])
                P.dma("sp", out_t[s, o - NMETA:o - NMETA + sz, :], ot[:, :], ot)

    def stage_fox(l):
        tl = cfg.tiles
        with P.stage() as st:
            FG = st.tile([12, L], F32)
            CUM = st.tile([12, L], F32)
            ONE = st.tile([12, L], F32)
            NB = st.tile([12, 2], F32)
            ENDS = st.tile([12, NTl], F32)
            Q = st.tile([128, 6, L], BF16)
            Kt = st.tile([128, 6, L], BF16)
            VP = st.tile([128, NTl, 12, 65], BF16)
            CREF = st.tile([128, 12, NTl], F32)
            CUMK = st.tile([128, NTl, 12], F32)
            BIAS = st.tile([128, NTl, 12, NTl], F32)
            VT = [st.tile([128, 6, 128], F32) for _ in range(2)]
            PT = [st.tile([128, 128], BF16) for _ in range(4)]
            YTK = [st.tile([128, FW], F32) for _ in range(2)]
            RC = [st.tile([128, 1], F32) for _ in range(2)]
            YF = [st.tile([128, 6, 128], BF16) for _ in range(2)]
            PS_S = [st.psum() for _ in range(4)]
            PS_O = [st.psum() for _ in range(2)]
            PS_T = [st.psum() for _ in range(2)]
            zq = ZT.view("(c p) l -> p c l", p=128) if False else None
            P.dma("sp", FG[:, :], ZT[C1 + 2304:C1 + 2316, :], FG)
            P.dma("sp", NB[:, 0:1], foxbf[l], NB)
            P.ts(NB[:, 1:2], NB[:, 0:1], -1.0, ALU.mult)
            P.memset(ONE[:, :], 1.0)
            P.act(FG[:, :], FG[:, :], AF.Exp, bias=NB[:, 1:2], scale=-1.0)
            P.act(FG[:, :], FG[:, :], AF.Ln, bias=1.0)
            P.ts(FG[:, :], FG[:, :], -1.0, ALU.mult)
            P.scan(CUM[:, :], ONE[:, :], FG[:, :], 0.0, ALU.mult, ALU.add)
            qv = VA(ZT.t[C1:C1 + 768, :].rearrange("(c p) l -> p c l", p=128), ZT.res)
            kv = VA(ZT.t[C1 + 768:C1 + 1536, :].rearrange("(c p) l -> p c l", p=128), ZT.res)
            vv = VA(ZT.t[C1 + 1536:C1 + 2304, :].rearrange("(c p) l -> p c l", p=128), ZT.res)
            mk_stg(st, 2)
            wload(st, Q[:, :, :], qv, 128, 6, L)
            wload(st, Kt[:, :, :], kv, 128, 6, L)
            P.memset(VP[:, :, :, :], 1.0, eng="pool")
            for j, (o, sz) in enumerate(tl):
                P.copy(ENDS[:, j:j + 1], CUM[:, o + sz // 2 - 1:o + sz // 2])
                ps = PS_T[j % 2]
                P.tr(ps[:sz, 0:12], CUM[:, o:o + sz], ident(12))
                P.copy(CUMK[:sz, j, :], ps[:sz, 0:12])
                vt = VT[j % 2]
                P.dma("sp", vt[:, :, :sz], vv[:, :, o:o + sz], vt)
                for c in range(6):
                    ps2 = PS_S[c % 3]
                    P.tr(ps2[:sz, 0:128], vt[:, c, :sz], ident(128))
                    P.copy(VP[:sz, j, 2 * c:2 * c + 2, 0:64],
                           VA(ps2.t[:sz, 0:128].rearrange("p (a b) -> p a b", a=2), ps2.res))
            psc = PS_O[0]
            for h in range(12):
                P.mm(psc[:, h * NTl:(h + 1) * NTl], CON[:12, CO_SEL + h * 128:CO_SEL + (h + 1) * 128], ENDS[:, :])
            P.copy(CREF[:, :, :], VA(psc.t[:, 0:12 * NTl].rearrange("p (a b) -> p a b", a=12), psc.res))
            for j, (o, sz) in enumerate(tl):
                for h in range(12):
                    P.ts(BIAS[:sz, j, h, :], CREF[:sz, h, :], CUMK[:sz, j, h:h + 1], ALU.subtract)
            pairs = [(i, h, j) for i in range(len(tl)) for h in range(12) for j in range(i + 1)]
            LOOK = 2
            NR = 4

            def emit_score(k):
                i, h, j = pairs[k]
                oi, si = tl[i]
                oj, sj = tl[j]
                c, pr = h // 2, (h % 2) * 64
                ps = PS_S[k % NR]
                pt = PT[k % NR]
                P.mm(ps[:sj, :si], Kt[pr:pr + 64, c, oj:oj + sj], Q[pr:pr + 64, c, oi:oi + si])
                P.act(pt[:sj, :si], ps[:sj, :si], AF.Exp, bias=BIAS[:sj, j, h, i:i + 1], scale=0.125)
                if j == i:
                    P.tt(pt[:sj, :si], pt[:sj, :si], CONB[:sj, CO_MLE:CO_MLE + si], ALU.mult, eng="pool")

            def emit_pv(k):
                i, h, j = pairs[k]
                oi, si = tl[i]
                oj, sj = tl[j]
                pt = PT[k % NR]
                po = PS_O[(i * 12 + h) % 2]
                rc = RC[(i * 12 + h) % 2]
                ytk = YTK[i % 2]
                P.mm(po[:si, 0:65], pt[:sj, :si], VP[:sj, j, h, :], start=(j == 0), stop=(j == i))
                if j == i:
                    P.recip(rc[:si, :], po[:si, 64:65])
                    P.ts(ytk[:si, h * 64:(h + 1) * 64], po[:si, 0:64], rc[:si, 0:1], ALU.mult)
                    if h == 11:
                        yf = YF[i % 2]
                        for c in range(6):
                            ps = PS_T[c % 2]
                            P.tr(ps[:, :si], ytk[:si, c * 128:(c + 1) * 128], ident(si))
                            P.copy(yf[:, c, :si], ps[:, :si], eng="act")
                        P.dma("sp", YTv[:, 6:12, oi:oi + si], yf[:, :, :si], yf)
            n_p = len(pairs)
            for k in range(n_p + LOOK):
                if k < n_p:
                    emit_score(k)
                if k - LOOK >= 0:
                    emit_pv(k - LOOK)

    def stage_mlstm(l):
        tl = cfg.tiles
        SCL = 128 ** -0.5
        with P.stage() as st:
            XP = [st.tile([128, 3 + L], F32) for _ in range(2)]
            ACC = st.tile([128, L], F32)
            QT = st.tile([128, 4, L], BF16)
            KT = st.tile([128, 4, L], BF16)
            KTOK = st.tile([128, NTl, 4, 128], BF16)
            VP = st.tile([128, NTl, 4, 129], BF16)
            CW = st.tile([128, 8, 4], F32)
            IG = st.tile([4, L], F32)
            FGm = st.tile([4, L], F32)
            ONE = st.tile([4, L], F32)
            Bc = st.tile([4, L], F32)
            WK = st.tile([4, L], F32)
            EB = st.tile([4, L], F32)
            MB = st.tile([4, 4], F32)
            ENDE = st.tile([4, NTl], F32)
            GT = st.tile([128, NTl, 8], F32)
            EBE = st.tile([128, 4, NTl], F32)
            CN = st.tile([128, 4, 129], F32)
            CNB = st.tile([128, 4, 129], BF16)
            VT = [st.tile([128, 4, 128], F32) for _ in range(2)]
            OG = [st.tile([128, 4, 128], F32) for _ in range(2)]
            GM = [st.tile([128, 128], BF16) for _ in range(2)]
            HTK = [st.tile([128, MW], F32) for _ in range(2)]
            SC = [st.tile([128, 4], F32) for _ in range(2)]
            YM = [st.tile([128, 4, 128], BF16) for _ in range(2)]
            PS_G = [st.psum() for _ in range(2)]
            PS_O = [st.psum() for _ in range(2)]
            PS_C = st.psum()
            PS_T = [st.psum() for _ in range(2)]
            PS_TB = st.psum([128, 1024], BF16)
            P.dma("sp", CW[:, :, :], convw[:, l, :, :], CW)
            P.dma("sp", MB[:, 0:2], mlb[l], MB)
            P.ts(MB[:, 2:3], MB[:, 1:2], -1.0, ALU.mult)
            P.dma("sp", IG[:, :], ZT[C2 + 2048:C2 + 2052, :], IG)
            P.dma("sp", FGm[:, :], ZT[C2 + 2052:C2 + 2056, :], FGm)
            P.memset(ONE[:, :], 1.0)
            P.act(FGm[:, :], FGm[:, :], AF.Exp, bias=MB[:, 2:3], scale=-1.0)
            P.act(FGm[:, :], FGm[:, :], AF.Ln, bias=1.0)
            P.ts(FGm[:, :], FGm[:, :], -1.0, ALU.mult)
            for j, (o, sz) in enumerate(tl):
                P.scan(Bc[:, o:o + sz], ONE[:, o:o + sz], FGm[:, o:o + sz], 0.0, ALU.mult, ALU.add)
                P.copy(ENDE[:, j:j + 1], Bc[:, o + sz - 1:o + sz])
            P.act(ENDE[:, :], ENDE[:, :], AF.Exp)
            P.tt(WK[:, :], IG[:, :], Bc[:, :], ALU.subtract)
            P.act(WK[:, :], WK[:, :], AF.Exp, bias=MB[:, 0:1])
            P.act(EB[:, :], Bc[:, :], AF.Exp)
            for h in range(4):
                P.mm(PS_C[:, h * NTl:(h + 1) * NTl], CON[:4, CO_SEL + h * 128:CO_SEL + (h + 1) * 128], ENDE[:, :])
            P.copy(EBE[:, :, :], VA(PS_C.t[:, 0:4 * NTl].rearrange("p (a b) -> p a b", a=4), PS_C.res))
            for c in range(8):
                xp = XP[c % 2]
                P.memset(xp[:, 0:3], 0.0)
                P.dma("sp", xp[:, 3:3 + L], ZT[C2 + c * 128:C2 + (c + 1) * 128, :], xp)
                P.ts(ACC[:, :], xp[:, 0:L], CW[:, c, 0:1], ALU.mult)
                for jj in range(1, 4):
                    P.stt(ACC[:, :], xp[:, jj:jj + L], CW[:, c, jj:jj + 1], ACC[:, :], ALU.mult, ALU.add)
                if c < 4:
                    P.act(QT[:, c, :], ACC[:, :], AF.Silu)
                else:
                    P.act(ACC[:, :], ACC[:, :], AF.Silu)
                    P.ts(KT[:, c - 4, :], ACC[:, :], SCL, ALU.mult)
            vv = VA(ZT.t[C2 + 1024:C2 + 1536, :].rearrange("(c p) l -> p c l", p=128), ZT.res)
            ov = VA(ZT.t[C2 + 1536:C2 + 2048, :].rearrange("(c p) l -> p c l", p=128), ZT.res)
            for j, (o, sz) in enumerate(tl):
                pt = PS_T[j % 2]
                P.tr(pt[:sz, 0:4], WK[:, o:o + sz], ident(4))
                P.tr(pt[:sz, 4:8], EB[:, o:o + sz], ident(4))
                P.copy(GT[:sz, j, :], pt[:sz, 0:8])
                vt = VT[j % 2]
                P.dma("sp", vt[:, :, :sz], vv[:, :, o:o + sz], vt)
                for h in range(4):
                    P.tr(pt[:sz, 128 * (h % 2) + 128:128 * (h % 2) + 256], vt[:, h, :sz], ident(128))
                    P.ts(VP[:sz, j, h, 0:128], pt[:sz, 128 * (h % 2) + 128:128 * (h % 2) + 256], GT[:sz, j, h:h + 1], ALU.mult)
                    P.copy(VP[:sz, j, h, 128:129], GT[:sz, j, h:h + 1])
                    P.tr(PS_TB[:sz, h * 128:(h + 1) * 128], KT[:, h, o:o + sz], CONB[:, CO_ID:CO_ID + 128])
                P.copy(KTOK[:sz, j, :, :], VA(PS_TB.t[:sz, 0:512].rearrange("p (a b) -> p a b", a=4), PS_TB.res))
            for j, (o, sz) in enumerate(tl):
                htk = HTK[j % 2]
                sc = SC[j % 2]
                og = OG[j % 2]
                P.dma("sp", og[:, :, :sz], ov[:, :, o:o + sz], og)
                P.act(og[:, :, :sz], og[:, :, :sz], AF.Sigmoid)
                for h in range(4):
                    pg = PS_G[h % 2]
                    po = PS_O[h % 2]
                    gm = GM[h % 2]
                    P.mm(pg[:sz, :sz], KT[:, h, o:o + sz], QT[:, h, o:o + sz])
                    P.tt(gm[:sz, :sz], pg[:sz, :sz], CON[:sz, CO_MLE:CO_MLE + sz], ALU.mult)
                    P.mm(po[:sz, 0:129], gm[:sz, :sz], VP[:sz, j, h, :], start=True, stop=(j == 0))
                    if j > 0:
                        P.mm(po[:sz, 0:129], QT[:, h, o:o + sz], CNB[:, h, :], start=False, stop=True)
                    P.tt(sc[:sz, 0:1], po[:sz, 128:129], GT[:sz, j, 4 + h:5 + h], ALU.mult)
                    P.ts(sc[:sz, 1:2], sc[:sz, 0:1], -1.0, ALU.mult)
                    P.tt(sc[:sz, 1:2], sc[:sz, 1:2], sc[:sz, 0:1], ALU.max)
                    P.ts(sc[:sz, 1:2], sc[:sz, 1:2], 1.0, ALU.max)
                    P.recip(sc[:sz, 2:3], sc[:sz, 1:2])
                    P.tt(sc[:sz, 3:4], sc[:sz, 2:3], GT[:sz, j, 4 + h:5 + h], ALU.mult)
                    P.ts(htk[:sz, h * 128:(h + 1) * 128], po[:sz, 0:128], sc[:sz, 3:4], ALU.mult)
                    P.mm(PS_C[:, 0:129], KTOK[:sz, j, h, :], VP[:sz, j, h, :])
                    if j == 0:
                        P.ts(CN[:, h, :], PS_C[:, 0:129], EBE[:, h, j:j + 1], ALU.mult)
                    else:
                        P.tt(CN[:, h, :], CN[:, h, :], PS_C[:, 0:129], ALU.add)
                        P.ts(CN[:, h, :], CN[:, h, :], EBE[:, h, j:j + 1], ALU.mult)
                    P.copy(CNB[:, h, :], CN[:, h, :])
                ym = YM[j % 2]
                for h in range(4):
                    pt = PS_T[h % 2]
                    P.tr(pt[:, :sz], htk[:sz, h * 128:(h + 1) * 128], ident(sz))
                    P.tt(ym[:, h, :sz], pt[:, :sz], og[:, h, :sz], ALU.mult)
                P.dma("sp", YTv[:, 12:16, o:o + sz], ym[:, :, :sz], ym)

    def stage_rwkv(l, part=0, nlev_max=99, sub=99):
        tl = cfg.tiles
        BLK = CON[:, CO_BLK:CO_BLK + 128]
        RWSv = RWS.view("q (c p) l -> p q c l", p=128)
        with P.stage() as st:
            W2 = st.tile([96, RW], BF16)
            A2 = st.tile([96, RW], BF16)
            G2 = st.tile([128, 2, RW], BF16)
            mk_stg(st, 2)
            wload(st, VA(W2.t[:, :].rearrange("p (a b) -> p a b", a=1), W2.res),
                  VA(rw2.t[l].rearrange("p (a b) -> p a b", a=1), rw2.res), 96, 1, RW)
            wload(st, VA(A2.t[:, :].rearrange("p (a b) -> p a b", a=1), A2.res),
                  VA(ra2.t[l].rearrange("p (a b) -> p a b", a=1), ra2.res), 96, 1, RW)
            wload(st, G2[:, :, :], rg2.view("l (kc p) n -> l p kc n", p=128)[l], 128, 2, RW)
            ZP = [st.tile([128, 6, 513], F32) for _ in range(3)]
            ZL = [st.tile([128, 513], F32) for _ in range(4)]
            TMP = st.tile([128, 512], F32)
            XR = st.tile([128, 6, 512], F32)
            XK = st.tile([128, 6, 512], F32)
            XV = st.tile([128, 6, 512], F32)
            LW = st.tile([128, 6, 512], F32)
            AA = st.tile([128, 6, 512], F32)
            GG = st.tile([128, 6, 512], F32)
            KK = st.tile([128, 6, 512], F32)
            BB = st.tile([128, 6, 512], F32)
            BON = st.tile([128, 6, 512], F32)
            TW = st.tile([96, 512], BF16)
            TA = st.tile([96, 512], BF16)
            SG = st.tile([128, 2, 512], BF16)
            PS = [st.psum() for _ in range(4)]
            pi = 0
            for (o, n) in cfg.sbs:
                def load_shift(zp_slice, rows0, nrows, mu_col, out):
                    pass
                for qi, (r0, X) in enumerate([(0, XR), (768, XK), (1536, XV)]):
                    zp = ZP[qi]
                    src_ = VA(ZT.t[r0:r0 + 768, :].rearrange("(c p) l -> p c l", p=128), ZT.res)
                    if o == 0:
                        P.memset(zp[:, :, 0:1], 0.0)
                        P.dma("sp", zp[:, :, 1:1 + n], src_[:, :, 0:n], zp)
                    else:
                        P.dma("sp", zp[:, :, 0:1 + n], src_[:, :, o - 1:o + n], zp, disjoint=False)
                    for c in range(6):
                        P.tt(TMP[:, :n], zp[:, c, 0:n], zp[:, c, 1:1 + n], ALU.subtract)
                        P.stt(X[:, c, :n], TMP[:, :n], VR[:, l, qi, c:c + 1], zp[:, c, 1:1 + n], ALU.mult, ALU.add)
                for qi, (r0, nr, vi, vc) in enumerate([(2304, 96, 10, 0), (2400, 96, 11, 0), (2496, 128, 12, 0), (2624, 128, 12, 1)]):
                    zl = ZL[qi]
                    if o == 0:
                        P.memset(zl[:nr, 0:1], 0.0)
                        P.dma("sp", zl[:nr, 1:1 + n], ZT[r0:r0 + nr, 0:n], zl)
                    else:
                        P.dma("sp", zl[:nr, 0:1 + n], ZT[r0:r0 + nr, o - 1:o + n], zl, disjoint=False)
                    P.tt(TMP[:nr, :n], zl[:nr, 0:n], zl[:nr, 1:1 + n], ALU.subtract)
                    P.stt(TMP[:nr, :n], TMP[:nr, :n], VR[:nr, l, vi, vc:vc + 1], zl[:nr, 1:1 + n], ALU.mult, ALU.add)
                    if qi == 0:
                        P.act(TW[:, :n], TMP[:96, :n], AF.Tanh)
                    elif qi == 1:
                        P.copy(TA[:, :n], TMP[:96, :n])
                    else:
                        P.act(SG[:, qi - 2, :n], TMP[:, :n], AF.Sigmoid)
                for c in range(6):
                    cs = slice(c * 128, (c + 1) * 128)
                    ps = PS[pi % 4]; pi += 1
                    P.mm(ps[:, :n], W2[:, cs], TW[:, :n])
                    P.act(LW[:, c, :n], ps[:, :n], AF.Sigmoid, bias=VR[:, l, 3, c:c + 1])
                    P.ts(LW[:, c, :n], LW[:, c, :n], -0.6065306597126334, ALU.mult)
                    ps = PS[pi % 4]; pi += 1
                    P.mm(ps[:, :n], A2[:, cs], TA[:, :n])
                    P.act(AA[:, c, :n], ps[:, :n], AF.Sigmoid, bias=VR[:, l, 4, c:c + 1])
                    ps = PS[pi % 4]; pi += 1
                    P.mm(ps[:, :n], G2[:, 0, cs], SG[:, 0, :n], start=True, stop=False)
                    P.mm(ps[:, :n], G2[:, 1, cs], SG[:, 1, :n], start=False, stop=True)
                    P.copy(GG[:, c, :n], ps[:, :n], eng="act")
                    P.ts(KK[:, c, :n], XK[:, c, :n], VR[:, l, 5, c:c + 1], ALU.mult)
                    P.tt(TMP[:, :n], KK[:, c, :n], KK[:, c, :n], ALU.mult)
                    ps = PS[pi % 4]; pi += 1
                    P.mm(ps[:, :n], BLK, TMP[:, :n])
                    P.act(TMP[:, :n], ps[:, :n], AF.Sqrt)
                    P.ts(TMP[:, :n], TMP[:, :n], 1e-12, ALU.max)
                    P.recip(TMP[:, :n], TMP[:, :n])
                    P.tt(KK[:, c, :n], KK[:, c, :n], TMP[:, :n], ALU.mult)
                    P.ts(TMP[:, :n], AA[:, c, :n], -1.0, ALU.add, VR[:, l, 6, c:c + 1], ALU.mult)
                    P.ts(TMP[:, :n], TMP[:, :n], 1.0, ALU.add)
                    P.tt(XK[:, c, :n], XK[:, c, :n], TMP[:, :n], ALU.mult)
                    P.tt(BB[:, c, :n], AA[:, c, :n], KK[:, c, :n], ALU.mult)
                    P.tt(TMP[:, :n], XR[:, c, :n], XK[:, c, :n], ALU.mult)
                    P.ts(TMP[:, :n], TMP[:, :n], VR[:, l, 7, c:c + 1], ALU.mult)
                    ps = PS[pi % 4]; pi += 1
                    P.mm(ps[:, :n], BLK, TMP[:, :n])
                    P.tt(BON[:, c, :n], ps[:, :n], XV[:, c, :n], ALU.mult)
                for qi, X in enumerate([XR, XK, XV, LW, KK, BB, BON, GG]):
                    P.dma("sp", RWSv[:, qi, :, o:o + n], X[:, :, :n], X)
        if part == 1:
            return
        with P.stage() as st:
            X8 = [st.tile([128, 8, 128], F32) for _ in range(2)]
            ONE = st.tile([128, 128], F32)
            CWt = st.tile([128, 128], F32)
            CWX = st.tile([128, 128], F32)
            EW = st.tile([128, 3, 128], F32)
            QS = st.tile([128, 5, 128], BF16)
            TOK = st.tile([128, 3, 128], BF16)
            SST = st.tile([128, 6, 64], F32)
            SSTB = st.tile([128, 6, 64], BF16)
            AM = [st.tile([128, 5, 128], BF16) for _ in range(2)]
            PW = [st.tile([128, 2, 128], BF16) for _ in range(2)]
            UU = st.tile([128, 128], F32)
            UB = st.tile([128, 128], BF16)
            PTB = st.psum([128, 1024], BF16)
            OTOK = st.tile([128, 128], F32)
            OT = st.tile([128, 128], F32)
            T1 = st.tile([128, 128], F32)
            T2 = st.tile([128, 128], F32)
            YR = [st.tile([128, 128], BF16) for _ in range(2)]
            PA = [st.psum() for _ in range(3)]
            PB = [st.psum() for _ in range(2)]
            PC = [st.psum() for _ in range(2)]
            P.memset(ONE[:, :], 1.0)
            P.memset(SST[:, :, :], 0.0)
            P.memset(SSTB[:, :, :], 0.0)
            MLE = CON[:, CO_MLE:CO_MLE + 128]
            MLT = CON[:, CO_MLT:CO_MLT + 128]
            MGE = CON[:, CO_MGE:CO_MGE + 128]
            it = 0
            for i, (o, sz) in enumerate(tl):
                nlev = min(int(np.ceil(np.log2(sz))), nlev_max)
                if nlev_max == 98 and i == 0:
                    continue
                for c in range(6):
                    x8 = X8[it % 2]
                    am = AM[it % 2]
                    yr = YR[it % 2]
                    it += 1
                    P.dma("sp", x8[:, :, :sz], RWSv[:, :, c, o:o + sz], x8)
                    R_, K_, V_, LW_, KK_, B_, BON_, G_ = [x8[:, q, :sz] for q in range(8)]
                    P.scan(CWt[:, :sz], ONE[:, :sz], LW_, 0.0, ALU.mult, ALU.add)
                    P.tt(CWX[:, :sz], CWt[:, :sz], LW_, ALU.subtract)
                    P.act(EW[:, 0, :sz], CWt[:, :sz], AF.Exp)
                    P.act(EW[:, 1, :sz], CWt[:, :sz], AF.Exp, scale=-1.0)
                    P.act(EW[:, 2, :sz], CWX[:, :sz], AF.Exp)
                    P.stt(QS[:, 0, :sz], KK_, -1.0, EW[:, 2, :sz], ALU.mult, ALU.mult)
                    P.tt(QS[:, 1, :sz], R_, EW[:, 0, :sz], ALU.mult)
                    P.tt(QS[:, 2, :sz], B_, EW[:, 1, :sz], ALU.mult)
                    P.tt(QS[:, 3, :sz], K_, EW[:, 1, :sz], ALU.mult)
                    P.copy(QS[:, 4, :sz], V_, eng="act")
                    IDB = CONB[:, CO_ID:CO_ID + 128]
                    P.tr(PTB[:sz, 0:128], QS[:, 4, :sz], IDB)
                    P.tr(PTB[:sz, 128:256], QS[:, 2, :sz], IDB)
                    P.tr(PTB[:sz, 256:384], QS[:, 3, :sz], IDB)
                    P.copy(TOK[:sz, :, :], VA(PTB.t[:sz, 0:384].rearrange("p (a b) -> p a b", a=3), PTB.res))
                    if sub == 0:
                        continue
                    for hh in range(2):
                        pr = slice(hh * 64, hh * 64 + 64)
                        AT = QS[pr, 0, :sz]
                        RT = QS[pr, 1, :sz]
                        BT = QS[pr, 2, :sz]
                        KT_ = QS[pr, 3, :sz]
                        p1, p2, p3 = PA[1], PA[2], PB[0]
                        P.mm(p1[:sz, 0:sz], BT, AT)
                        P.mm(p1[:sz, 128:128 + sz], BT, RT)
                        P.mm(p2[:sz, 0:sz], KT_, AT)
                        P.mm(p2[:sz, 128:128 + sz], KT_, RT)
                        P.mm(p3[:sz, 0:sz], AT, BT)
                        P.tt(am[:sz, 0, :sz], p1[:sz, 0:sz], MLT[:sz, :sz], ALU.mult)
                        P.tt(am[:sz, 1, :sz], p1[:sz, 128:128 + sz], MLE[:sz, :sz], ALU.mult)
                        P.tt(am[:sz, 2, :sz], p2[:sz, 0:sz], MLT[:sz, :sz], ALU.mult)
                        P.tt(am[:sz, 3, :sz], p2[:sz, 128:128 + sz], MLE[:sz, :sz], ALU.mult)
                        P.tt(am[:sz, 4, :sz], p3[:sz, 0:sz], MGE[:sz, :sz], ALU.mult)
                        if sub == 1:
                            continue
                        p4 = PB[1]
                        P.mm(p4[:sz, 0:64], AT, SSTB[pr, c, :], start=True, stop=False)
                        P.mm(p4[:sz, 0:64], am[:sz, 2, :sz], TOK[:sz, 0, pr], start=False, stop=True)
                        U = UU[:sz, pr]
                        Ub = UB[:sz, pr]
                        P.copy(U, p4[:sz, 0:64])
                        P.copy(Ub, p4[:sz, 0:64], eng="act")
                        Pm, PTm = am[:sz, 4, :sz], am[:sz, 0, :sz]
                        for lev in range(nlev):
                            pu = PC[lev % 2]
                            P.mm(pu[:sz, 0:64], PTm, Ub)
                            if lev < nlev - 1:
                                pw = PW[lev % 2]
                                pq, pq2 = PB[0], PB[1]
                                P.mm(pq[:sz, 0:sz], PTm, Pm)
                                P.mm(pq2[:sz, 0:sz], Pm, PTm)
                            P.tt(U, U, pu[:sz, 0:64], ALU.add)
                            P.copy(Ub, U, eng="act")
                            if lev < nlev - 1:
                                P.copy(pw[:sz, 0, :sz], pq[:sz, 0:sz])
                                P.copy(pw[:sz, 1, :sz], pq2[:sz, 0:sz], eng="act")
                                Pm, PTm = pw[:sz, 0, :sz], pw[:sz, 1, :sz]
                        if sub == 2:
                            continue
                        po = PC[0]
                        P.mm(po[:sz, 64:128], RT, SSTB[pr, c, :], start=True, stop=False)
                        P.mm(po[:sz, 64:128], am[:sz, 1, :sz], Ub, start=False, stop=False)
                        P.mm(po[:sz, 64:128], am[:sz, 3, :sz], TOK[:sz, 0, pr], start=False, stop=True)
                        P.copy(OTOK[:sz, pr], po[:sz, 64:128])
                    if sub in (1, 2, 3):
                        continue
                    psu = PA[1]
                    P.mm(psu[:, 0:128], TOK[:sz, 1, :], UB[:sz, :], start=True, stop=False)
                    P.mm(psu[:, 0:128], TOK[:sz, 2, :], TOK[:sz, 0, :], start=False, stop=True)
                    for hh in range(2):
                        pr = slice(hh * 64, hh * 64 + 64)
                        P.tt(SST[pr, c, :], SST[pr, c, :], psu[pr, pr], ALU.add)
                        P.ts(SST[pr, c, :], SST[pr, c, :], EW[pr, 0, sz - 1:sz], ALU.mult)
                        P.copy(SSTB[pr, c, :], SST[pr, c, :], eng="act")
                    if sub == 4:
                        continue
                    ptt = PA[2]
                    P.tr(ptt[:, 0:sz], OTOK[:sz, :], ident(sz))
                    P.copy(OT[:, :sz], ptt[:, 0:sz])
                    P.tt(T1[:, :sz], OT[:, :sz], OT[:, :sz], ALU.mult)
                    pmn = PB[0]
                    P.mm(pmn[:, 0:sz], BLK, OT[:, :sz])
                    P.mm(pmn[:, 128:128 + sz], BLK, T1[:, :sz])
                    P.ts(T1[:, :sz], pmn[:, 0:sz], 1.0 / 64, ALU.mult)
                    P.tt(T2[:, :sz], T1[:, :sz], T1[:, :sz], ALU.mult)
                    P.stt(T2[:, :sz], pmn[:, 128:128 + sz], 1.0 / 64, T2[:, :sz], ALU.mult, ALU.subtract)
                    P.ts(T2[:, :sz], T2[:, :sz], 64e-5, ALU.add)
                    P.act(T2[:, :sz], T2[:, :sz], AF.Sqrt)
                    P.recip(T2[:, :sz], T2[:, :sz])
                    P.tt(OT[:, :sz], OT[:, :sz], T1[:, :sz], ALU.subtract)
                    P.tt(OT[:, :sz], OT[:, :sz], T2[:, :sz], ALU.mult)
                    P.ts(OT[:, :sz], OT[:, :sz], VR[:, l, 8, c:c + 1], ALU.mult, VR[:, l, 9, c:c + 1], ALU.add)
                    P.tt(OT[:, :sz], OT[:, :sz], BON_, ALU.add)
                    P.tt(yr[:, :sz], OT[:, :sz], G_, ALU.mult)
                    P.dma("sp", YT[c * 128:(c + 1) * 128, o:o + sz], yr[:, :sz], yr)
    def run_all():
        P.no_reset = (cfg.NSEQ == 1)
        prologue()
        P.reset_all()
        def body(s):
            stage_ln0(s)
            for l in range(dep):
                stage_win(l)
                stage_fox(l)
                stage_mlstm(l)
                stage_rwkv(l)
                stage_m1(l)
                stage_m2(l)
                ln_stage(NEWTv, 2 + 4 * l + 0, 2 + 4 * l + 1)
                stage_ffn(l)
                ln_stage(NEWTv, 2 + 4 * l + 2, 2 + 4 * l + 3)
            stage_out(s)
            P.reset_all()
        if cfg.NSEQ == 1:
            body(0)
        else:
            with nc.Fori(0, cfg.NSEQ) as s:
                body(s)
        P.es.close()
    return finish(locals())


def finish(lc):
    P, cfg, dbg = lc["P"], lc["cfg"], lc["dbg"]
    run = lc.get("run_stages")
    return lc


def pack_small(p, dep):
    def fm(v):
        return np.ascontiguousarray(np.asarray(v, np.float32).reshape(KC, 128).T)
    vecD = np.zeros((128, 2 + 4 * dep, KC), np.float32)
    vecD[:, 0] = fm(p["ln_emb_g"])
    vecD[:, 1] = fm(p["ln_emb_b"])
    for l in range(dep):
        vecD[:, 2 + 4 * l + 0] = fm(p["ln1_g"][l])
        vecD[:, 2 + 4 * l + 1] = fm(p["ln1_b"][l])
        vecD[:, 2 + 4 * l + 2] = fm(p["ln2_g"][l])
        vecD[:, 2 + 4 * l + 3] = fm(p["ln2_b"][l])
    vecR = np.zeros((128, dep, 13, 6), np.float32)

    def f6(v):
        return np.asarray(v, np.float32).reshape(6, 128).T
    for l in range(dep):
        mu = np.asarray(p["rwkv_mu"][l], np.float32)
        vecR[:, l, 0] = f6(mu[0:768])
        vecR[:, l, 1] = f6(mu[768:1536])
        vecR[:, l, 2] = f6(mu[1536:2304])
        vecR[:, l, 3] = f6(p["rwkv_w0"][l])
        vecR[:, l, 4] = f6(p["rwkv_a0"][l])
        vecR[:, l, 5] = f6(p["rwkv_k_k"][l])
        vecR[:, l, 6] = f6(p["rwkv_k_a"][l])
        vecR[:, l, 7] = f6(np.asarray(p["rwkv_r_k"][l]).reshape(768))
        vecR[:, l, 8] = f6(p["rwkv_gn_g"][l])
        vecR[:, l, 9] = f6(p["rwkv_gn_b"][l])
        vecR[:96, l, 10, 0] = mu[2304:2400]
        vecR[:96, l, 11, 0] = mu[2400:2496]
        vecR[:, l, 12, 0] = mu[2496:2624]
        vecR[:, l, 12, 1] = mu[2624:2752]
    foxbf = np.ascontiguousarray(np.asarray(p["fox_b_f"], np.float32)[:dep].reshape(dep, 12, 1))
    cw = np.asarray(p["mlstm_conv_w"], np.float32)[:dep]
    convw = np.ascontiguousarray(cw.reshape(dep, 4, 8, 128).transpose(3, 0, 2, 1))
    mlb = np.zeros((dep, 4, 2), np.float32)
    mlb[:, :, 0] = np.asarray(p["mlstm_b_i"], np.float32)[:dep]
    mlb[:, :, 1] = np.asarray(p["mlstm_b_f"], np.float32)[:dep]
    return {"consts": make_consts(), "vecD": vecD, "vecR": vecR, "foxbf": foxbf, "convw": convw, "mlb": mlb}


N_CORES = 8


def kernel(**inputs):
    x = np.asarray(inputs["x"], np.float32)
    B = x.shape[0]
    nseq = B // N_CORES
    cfg = Cfg(NT=16, NSEQ=nseq, depth=4)
    Res.ALL.clear()
    lc = build(cfg)
    lc["run_all"]()
    nc = lc["nc"]
    small = pack_small(inputs, 4)
    shared = dict(small)
    shared["meta"] = np.asarray(inputs["meta_tokens"], np.float32)
    for k in ["w_in", "rwkv_w2", "rwkv_a2", "rwkv_g2", "proj_rwkv", "proj_fox", "proj_mlstm", "w_out", "ffn_w1", "ffn_w3",
              "ffn_w2", "router_w", "moe_w1", "moe_w3", "moe_w2"]:
        shared[k] = np.asarray(inputs[k], np.float32)
    in_maps = []
    for c in range(N_CORES):
        m = dict(shared)
        m["x"] = np.ascontiguousarray(x[c * nseq:(c + 1) * nseq])
        in_maps.append(m)
    res = run_bass_kernel_spmd(nc, in_maps, core_ids=list(range(N_CORES)))
    return np.concatenate([r["out"] for r in res.results], axis=0).astype(np.float32)
```
